# Optimizing a Trainium2 kernel written in Bass

```python
import jax, jax.numpy as jnp
from jax import lax
import numpy as np


D_MODEL = 1024
BATCH = 16
SEQ = 2048
DEPTH = 1

PLE_DIM = 256
W_A = D_MODEL
N_HEADS_A = 4
HEAD_DIM_A = W_A // N_HEADS_A
QKV_BLOCK = 4
CONV_A = 4
CHUNK = 64
W_B = D_MODEL
CONV_B = 31
EPS = 1e-6
D_IN = 2 * W_A + 3 * W_B + 2 * D_MODEL

kernel_name = "hybrid_mlstm_conformer_gated_block"


def rmsnorm(x, g):
    xf = x.astype(jnp.float32)
    y = xf * lax.rsqrt(jnp.mean(xf * xf, axis=-1, keepdims=True) + EPS)
    return (y * g.astype(jnp.float32)).astype(x.dtype)


def _standardize(x):
    xf = x.astype(jnp.float32)
    xc = xf - jnp.mean(xf, axis=-1, keepdims=True)
    return xc * lax.rsqrt(jnp.mean(xc * xc, axis=-1, keepdims=True) + EPS)


def causal_depthwise_conv(x, w, b):
    K, C = w.shape
    y = lax.conv_general_dilated(x, w[:, None, :].astype(x.dtype), window_strides=(1,),
                                 padding=[(K - 1, 0)], dimension_numbers=('NWC', 'WIO', 'NWC'),
                                 feature_group_count=C)
    return y + b.astype(x.dtype)


def blockdiag(x, w):
    G, bi, bo = w.shape
    y = jnp.einsum('bsgi,gio->bsgo', x.reshape(x.shape[:-1] + (G, bi)), w)
    return y.reshape(x.shape[:-1] + (G * bo,))


def mlstm_chunkwise(q, k, v, ig, lf):
    B, H, S, dh = q.shape
    nc = S // CHUNK

    def to_chunks(t):
        return jnp.moveaxis(t.reshape((B, H, nc, CHUNK) + t.shape[3:]), 2, 0)

    mask = jnp.tril(jnp.ones((CHUNK, CHUNK), dtype=bool))

    def step(carry, xs):
        C, n, m = carry
        qc, kc, vc, ic, fc = xs
        b = jnp.cumsum(fc, axis=-1)
        dlog = jnp.where(mask, b[..., :, None] - b[..., None, :] + ic[..., None, :], -jnp.inf)
        m_inter = b + m[..., None]
        m_comb = jnp.maximum(jnp.max(dlog, axis=-1), m_inter)
        s = jnp.einsum('bhjd,bhsd->bhjs', qc, kc) * jnp.exp(dlog - m_comb[..., None])
        inter = jnp.exp(m_inter - m_comb)
        num = jnp.einsum('bhjs,bhse->bhje', s, vc) + inter[..., None] * jnp.einsum('bhjd,bhde->bhje', qc, C)
        den = jnp.sum(s, axis=-1) + inter * jnp.einsum('bhjd,bhd->bhj', qc, n)
        h = num / jnp.maximum(jnp.abs(den), jnp.exp(-m_comb))[..., None]
        bL = b[..., -1]
        wlog = bL[..., None] - b + ic
        m_new = jnp.maximum(bL + m, jnp.max(wlog, axis=-1))
        w = jnp.exp(wlog - m_new[..., None])
        decay = jnp.exp(bL + m - m_new)
        wk = w[..., None] * kc
        C = decay[..., None, None] * C + jnp.einsum('bhsd,bhse->bhde', wk, vc)
        n = decay[..., None] * n + jnp.sum(wk, axis=2)
        return (C, n, m_new), h

    init = (jnp.zeros((B, H, dh, dh), jnp.float32), jnp.zeros((B, H, dh), jnp.float32),
            jnp.zeros((B, H), jnp.float32))
    _, hs = lax.scan(step, init, (to_chunks(q), to_chunks(k), to_chunks(v), to_chunks(ig), to_chunks(lf)))
    return jnp.moveaxis(hs, 0, 2).reshape(B, H, S, dh)


def setup_inputs(seed: int = 0) -> dict:
    key = jax.random.key(seed)
    ks = jax.random.split(key, 26)
    f32 = jnp.float32

    def nrm(k, shape, scale):
        return jax.random.normal(k, shape, f32) * scale

    H = N_HEADS_A
    G = W_A // QKV_BLOCK
    b_if = jnp.concatenate([
        nrm(ks[10], (DEPTH, H), 0.1),
        jnp.broadcast_to(jnp.linspace(3.0, 6.0, H, dtype=f32), (DEPTH, H)) + nrm(ks[11], (DEPTH, H), 0.01)], axis=-1)
    return {
        'x': nrm(ks[0], (BATCH, SEQ, D_MODEL), 1.0),
        'p': nrm(ks[1], (DEPTH, BATCH, SEQ, PLE_DIM), 1.0),
        'norm_g': 1.0 + nrm(ks[2], (DEPTH, D_MODEL), 0.02),
        'w_in': nrm(ks[3], (DEPTH, D_MODEL, D_IN), D_MODEL ** -0.5),
        'conv_a_w': nrm(ks[4], (DEPTH, CONV_A, W_A), CONV_A ** -0.5),
        'conv_a_b': nrm(ks[5], (DEPTH, W_A), 0.02),
        'wq': nrm(ks[6], (DEPTH, G, QKV_BLOCK, QKV_BLOCK), QKV_BLOCK ** -0.5),
        'wk': nrm(ks[7], (DEPTH, G, QKV_BLOCK, QKV_BLOCK), QKV_BLOCK ** -0.5),
        'wv': nrm(ks[8], (DEPTH, G, QKV_BLOCK, QKV_BLOCK), QKV_BLOCK ** -0.5),
        'w_if': nrm(ks[9], (DEPTH, 3 * W_A, 2 * H), (3 * W_A) ** -0.5),
        'b_if': b_if,
        'mh_norm_g': 1.0 + nrm(ks[12], (DEPTH, W_A), 0.02),
        'skip_a': 1.0 + nrm(ks[13], (DEPTH, W_A), 0.02),
        'w_proj_a': nrm(ks[14], (DEPTH, W_A, D_MODEL), W_A ** -0.5),
        'conv_b_w': nrm(ks[15], (DEPTH, CONV_B, W_B), CONV_B ** -0.5),
        'conv_b_b': nrm(ks[16], (DEPTH, W_B), 0.02),
        'ln_b_g': 1.0 + nrm(ks[17], (DEPTH, W_B), 0.02),
        'ln_b_b': nrm(ks[18], (DEPTH, W_B), 0.02),
        'w_pw2': nrm(ks[19], (DEPTH, W_B, D_MODEL), W_B ** -0.5),
        'b_pw2': nrm(ks[20], (DEPTH, D_MODEL), 0.02),
        'w_out': nrm(ks[21], (DEPTH, D_MODEL, D_MODEL), D_MODEL ** -0.5),
        'w_ple': nrm(ks[22], (DEPTH, PLE_DIM, D_MODEL), PLE_DIM ** -0.5),
        'ple_norm_g': 1.0 + nrm(ks[23], (DEPTH, D_MODEL), 0.02),
        'w_ple_gate': nrm(ks[24], (DEPTH, D_MODEL, D_MODEL), D_MODEL ** -0.5),
        'final_g': 1.0 + nrm(ks[25], (DEPTH, D_MODEL), 0.02)[0],
    }


def reference(x, p, norm_g, w_in, conv_a_w, conv_a_b, wq, wk, wv, w_if, b_if, mh_norm_g, skip_a,
              w_proj_a, conv_b_w, conv_b_b, ln_b_g, ln_b_b, w_pw2, b_pw2, w_out, w_ple, ple_norm_g,
              w_ple_gate, final_g):
    Bsz, S, _ = x.shape
    H, dh = N_HEADS_A, HEAD_DIM_A
    split_at = [W_A, 2 * W_A, 2 * W_A + W_B, 2 * W_A + 2 * W_B, 2 * W_A + 3 * W_B]
    for i in range(DEPTH):
        h = rmsnorm(x, norm_g[i])
        proj = h @ w_in[i]
        xa, za, ub, gb, zb, gates = jnp.split(proj, split_at, axis=-1)

        xc = jax.nn.silu(causal_depthwise_conv(xa, conv_a_w[i], conv_a_b[i]))
        q = blockdiag(xc, wq[i])
        k = blockdiag(xc, wk[i])
        v = blockdiag(xa, wv[i])
        gif = (jnp.concatenate([q, k, v], axis=-1) @ w_if[i] + b_if[i]).astype(jnp.float32)
        ig = jnp.transpose(gif[..., :H], (0, 2, 1))
        lf = jnp.transpose(jax.nn.log_sigmoid(gif[..., H:]), (0, 2, 1))

        def heads(t):
            return jnp.transpose(t.reshape(Bsz, S, H, dh), (0, 2, 1, 3)).astype(jnp.float32)

        ha = mlstm_chunkwise(heads(q), heads(k) * (dh ** -0.5), heads(v), ig, lf)
        ha = jnp.transpose(_standardize(ha), (0, 2, 1, 3)).reshape(Bsz, S, W_A)
        ha = (ha * mh_norm_g[i].astype(jnp.float32)).astype(x.dtype)
        ya = ((ha + skip_a[i] * xc) * jax.nn.silu(za)) @ w_proj_a[i]

        ug = ub * jax.nn.sigmoid(gb)
        c = causal_depthwise_conv(ug, conv_b_w[i], conv_b_b[i])
        c = (_standardize(c) * ln_b_g[i].astype(jnp.float32) + ln_b_b[i].astype(jnp.float32)).astype(x.dtype)
        yb = (jax.nn.silu(c) * jax.nn.silu(zb)) @ w_pw2[i] + b_pw2[i]

        g_a, g_b = jnp.split(gates, 2, axis=-1)
        merged = jax.nn.sigmoid(g_a) * ya + jax.nn.sigmoid(g_b) * yb
        x = x + merged @ w_out[i]

        ple_gate = jax.nn.sigmoid(rmsnorm(x, ple_norm_g[i]) @ w_ple_gate[i])
        x = x + (p[i] @ w_ple[i]) * ple_gate
    return rmsnorm(x, final_g)
```

```python
import math
import numpy as np
import ml_dtypes
import concourse.bass as bass
import concourse.mybir as mybir
from concourse.bass_utils import run_bass_kernel_spmd

F32 = mybir.dt.float32
BF16 = mybir.dt.bfloat16
ALU = mybir.AluOpType
AF = mybir.ActivationFunctionType
AX = mybir.AxisListType

D = 1024
SEQ = 2048
NSEQ = 2
T = 512
NT = SEQ // T
H = 4
DH = 256
EPS = 1e-6
KB = 31
KA = 4

C_NG, C_CAB, C_MHG, C_SKP, C_CBB, C_LNG, C_LNB, C_BPW, C_PNG = 0, 8, 16, 24, 32, 40, 48, 56, 64
C_CAW = 72
C_CBW = 104
C_WQ, C_WK, C_WV = 352, 384, 416
C_BIF = 448
NV = 450

G_XA, G_ZA, G_UB, G_GB, G_ZB, G_GA, G_GBM, G_PA, G_PW2, G_OUT, G_PG = range(11)


STOP = None
PHASE = ["setup"]


class _Stop(Exception):
    pass


def chk(name):
    PHASE[0] = name
    if STOP == name:
        raise _Stop()


class Eng:
    def __init__(self, name):
        self.name = name
        self.ops = []
        self.sem = None
        self.seen = {}


class Op:
    __slots__ = ("eng", "fn", "waits", "needed", "idx", "cnt", "dma", "phase")

    def __init__(self, eng, fn):
        self.eng = eng
        self.fn = fn
        self.waits = []
        self.needed = False
        self.idx = len(eng.ops)
        self.cnt = None
        self.dma = None
        self.phase = PHASE[0]


class DSem:
    def __init__(self, name, sem):
        self.name = name
        self.sem = sem
        self.count = 0


class Buf:
    __slots__ = ("name", "w", "r", "psum")

    def __init__(self, name, psum=False):
        self.name = name
        self.w = None
        self.r = {}
        self.psum = psum


def bufs(name, n, psum=False):
    return [Buf(f"{name}{i}", psum) for i in range(n)]


def tokkey(tok):
    return tok[1].eng.name if tok[0] == "op" else ("d", tok[1].name)


class K:
    def __init__(self, nc):
        self.nc = nc
        self.E = {n: Eng(n) for n in ("pe", "act", "dve", "pool", "sp")}
        for n, e in self.E.items():
            e.sem = nc.alloc_semaphore(name=f"sem_{n}")
        self.dsems = {}

    def dsem(self, name):
        if name not in self.dsems:
            self.dsems[name] = DSem(name, self.nc.alloc_semaphore(name=f"dsem_{name}"))
        return self.dsems[name]

    def _add_wait(self, op, tok, kind):
        if tok[0] == "op":
            src = tok[1]
            if src.eng is op.eng:
                if op.eng.name in ("pe", "sp"):
                    return
            if op.eng.seen.get(src.eng.name, -1) >= src.idx:
                return
            op.eng.seen[src.eng.name] = src.idx
            src.needed = True
            op.waits.append(tok)
        else:
            key = ("d", tok[1].name)
            if op.eng.seen.get(key, 0) >= tok[2]:
                return
            op.eng.seen[key] = tok[2]
            op.waits.append(tok)

    def iss(self, en, fn, rd=(), wr=(), dsem=None):
        eng = self.E[en]
        op = Op(eng, fn)
        for b in rd:
            if b.w is not None:
                self._add_wait(op, b.w, "raw")
            if b.psum:
                for kk, t in b.r.items():
                    if kk != eng.name:
                        self._add_wait(op, t, "rar")
        for b in wr:
            if b.w is not None:
                self._add_wait(op, b.w, "waw")
            for t in b.r.values():
                self._add_wait(op, t, "war")
        if dsem is not None:
            dsem.count += 16
            tok = ("dma", dsem, dsem.count)
            op.dma = dsem
        else:
            tok = ("op", op)
        for b in rd:
            b.r[tokkey(tok)] = tok
        for b in wr:
            b.w = tok
            b.r = {}
        eng.ops.append(op)
        return op

    def handoff(self, old, new):
        toks = {}
        for b in old:
            if b.w is not None:
                toks[("w",) + (tokkey(b.w),)] = b.w
            for k, t in b.r.items():
                kk = ("r", k)
                if kk in toks:
                    a = toks[kk]
                    if a[0] == "op":
                        if t[1].idx > a[1].idx:
                            toks[kk] = t
                    elif t[2] > a[2]:
                        toks[kk] = t
                else:
                    toks[kk] = t
        merged = {}
        for (_, k), t in toks.items():
            if k in merged:
                a = merged[k]
                if a[0] == "op":
                    if t[1].idx > a[1].idx:
                        merged[k] = t
                elif t[2] > a[2]:
                    merged[k] = t
            else:
                merged[k] = t
        for b in new:
            b.w = None
            b.r = dict(merged)

    def mm(self, out, lhsT, rhs, start, stop, rd, wr):
        return self.iss("pe", lambda h: h.matmul(out, lhsT, rhs, start=start, stop=stop), rd, wr)

    def tr(self, out, in_, ident, rd, wr):
        return self.iss("pe", lambda h: h.transpose(out, in_, ident), rd, wr)

    def act(self, out, in_, func, rd, wr, bias=None, scale=None, accum=None):
        kw = {}
        if bias is not None:
            kw["bias"] = bias
        if scale is not None:
            kw["scale"] = scale
        if accum is not None:
            kw["accum_out"] = accum
        return self.iss("act", lambda h: h.activation(out, in_, func, **kw), rd, wr)

    def tt(self, en, out, in0, in1, op, rd, wr):
        return self.iss(en, lambda h: h.tensor_tensor(out, in0, in1, op), rd, wr)

    def ts(self, en, out, in0, s1, s2, op0, op1, rd, wr):
        if s2 is None:
            return self.iss(en, lambda h: h.tensor_scalar(out, in0, s1, None, op0), rd, wr)
        return self.iss(en, lambda h: h.tensor_scalar(out, in0, s1, s2, op0, op1), rd, wr)

    def stt(self, out, in0, scalar, in1, op0, op1, rd, wr):
        return self.iss("dve", lambda h: h.scalar_tensor_tensor(out, in0, scalar, in1, op0, op1), rd, wr)

    def cp(self, en, out, in_, rd, wr):
        if en == "act":
            return self.iss("act", lambda h: h.activation(out, in_, AF.Copy), rd, wr)
        return self.iss(en, lambda h: h.tensor_copy(out, in_), rd, wr)

    def memset(self, en, ap, val, wr):
        return self.iss(en, lambda h: h.memset(ap, val), (), wr)

    def dma(self, out, in_, rd, wr, dsem, en="sp"):
        return self.iss(en, lambda h: h.dma_start(out=out, in_=in_), rd, wr, dsem=dsem)

    def emit(self, final_waits):
        nc = self.nc
        for e in self.E.values():
            c = 0
            for op in e.ops:
                if op.needed:
                    c += 1
                    op.cnt = c

        def run(eng, h, extra=()):
            for op in eng.ops:
                for tok in op.waits:
                    if tok[0] == "op":
                        h.wait_ge(tok[1].eng.sem, tok[1].cnt)
                    else:
                        h.wait_ge(tok[1].sem, tok[2])
                ins = op.fn(h)
                if op.dma is not None:
                    ins.then_inc(op.dma.sem, 16)
                elif op.needed:
                    ins.then_inc(eng.sem, 1)
            for ds in extra:
                h.wait_ge(ds.sem, ds.count)

        E = self.E
        with nc.Block() as block:
            @block.tensor
            def _(h):
                run(E["pe"], h)

            @block.scalar
            def _(h):
                run(E["act"], h)

            @block.vector
            def _(h):
                run(E["dve"], h)

            @block.gpsimd
            def _(h):
                run(E["pool"], h, final_waits)

            @block.sync
            def _(h):
                run(E["sp"], h)


def build_program(n_seq=NSEQ, n_tiles=NT, debug=False):
    nc = bass.Bass("TRN2", target_bir_lowering=False)
    k = K(nc)
    NTOK = n_seq * SEQ

    def din(name, shape):
        return nc.dram_tensor(name, list(shape), F32, kind="ExternalInput").ap()

    x_d = din("x", [NTOK, D])
    p_d = din("p", [NTOK, 256])
    w_in_d = din("w_in", [D, 7 * D])
    w_pa_d = din("w_proj_a", [D, D])
    w_pw2_d = din("w_pw2", [D, D])
    w_out_d = din("w_out", [D, D])
    w_pg_d = din("w_ple_gate", [D, D])
    w_ple_d = din("w_ple", [256, D])
    vec_d = din("vecs", [128, NV])
    cmat_d = din("cmat", [128, 288])
    fg_d = din("fg_bc", [128, D])
    wif_d = din("w_if_t", [128, 24 * 8])
    out_d = nc.dram_tensor("out", [NTOK, D], F32, kind="ExternalOutput").ap()
    wsc_d = nc.dram_tensor("wsc", [11, 128, 8 * D], BF16).ap()
    wsc_ple_d = nc.dram_tensor("wsc_ple", [128, 2 * D], BF16).ap()
    dsc_d = nc.dram_tensor("dsc", [8, 128, KB * 128], BF16).ap()

    def sb(name, shape, dt):
        return nc.alloc_sbuf_tensor("s_" + name, list(shape), dt).ap()

    def ps(name, shape, dt):
        return nc.alloc_psum_tensor("p_" + name, list(shape), dt).ap()

    vec = sb("vec", [128, NV], F32)
    cmat = sb("cmat", [128, 288], F32)
    fgbc = sb("fgbc", [128, D], F32)
    wif32 = sb("wif32", [128, 192], F32)
    wifb = sb("wifb", [128, 24, 8], BF16)
    identb = sb("identb", [128, 128], BF16)
    onesb = sb("onesb", [128, 128], BF16)
    ones32 = sb("ones32", [128, 512], F32)
    diag4 = sb("diag4", [128, KA * 8, 128], BF16)
    bd = sb("bd", [128, 3 * 8, 128], BF16)
    negbf = sb("negbf", [128, 1], F32)
    B_const = Buf("const")
    ident32 = cmat[:, 0:128]
    trimask = cmat[:, 128:256]
    bmask = cmat[:, 256:288]

    ds_c = k.dsem("const")
    k.dma(vec, vec_d, (), [B_const], ds_c)
    k.dma(cmat, cmat_d, (), [B_const], ds_c)
    k.dma(fgbc, fg_d, (), [B_const], ds_c)
    k.dma(wif32, wif_d, (), [B_const], ds_c)
    B_c2 = Buf("const2")
    k.cp("dve", wifb.rearrange("p a b -> p (a b)"), wif32, [B_const], [B_c2])
    k.cp("dve", identb, ident32, [B_const], [B_c2])
    k.memset("dve", onesb, 1.0, [B_c2])
    k.memset("dve", ones32, 1.0, [B_c2])
    k.memset("dve", bd.rearrange("p a b -> p (a b)"), 0.0, [B_c2])
    k.ts("dve", negbf[0:4, :], vec[0:4, C_BIF + 1:C_BIF + 2], -1.0, None, ALU.mult, None, [B_const], [B_c2])
    for kk in range(KA):
        for c in range(8):
            col = C_CAW + kk * 8 + c
            k.ts("dve", diag4[:, kk * 8 + c, :], identb, vec[:, col:col + 1], None, ALU.mult, None,
                 [B_const, B_c2], [B_c2])
    for wi, cb in enumerate((C_WQ, C_WK, C_WV)):
        for c in range(8):
            dst = bd[:, wi * 8 + c, :].rearrange("p (g o) -> p g o", o=4)
            for o in range(4):
                col = cb + c * 4 + o
                k.ts("dve", dst[:, :, o], bmask, vec[:, col:col + 1], None, ALU.mult, None,
                     [B_const, B_c2], [B_c2])
    CONSTS = [B_const, B_c2]

    PSA = [ps(f"psa{i}", [128, 512], F32) for i in range(8)]
    PSAb = bufs("psa", 8, True)
    pctr = {"f": 0}
    pinned = set()

    def psf_i():
        while True:
            i = pctr["f"] % 8
            pctr["f"] += 1
            if i not in pinned:
                return i

    def psf():
        i = psf_i()
        return PSA[i], PSAb[i]

    def psb():
        i = psf_i()
        return PSA[i].bitcast(BF16), PSAb[i]

    def pspin():
        i = psf_i()
        pinned.add(i)
        return i

    NSLOT = 3
    wslot = [sb(f"wslot{i}", [128, 8, D], BF16) for i in range(NSLOT)]
    wslotb = bufs("wslot", NSLOT)
    wple = sb("wple", [128, 2, D], BF16)
    wpleb = Buf("wple")
    NST = 2
    stage = [sb(f"stage{i}", [128, 512], F32) for i in range(NST)]
    stageb = bufs("stage", NST)
    x_tok = sb("x_tok", [128, 4, D], F32)
    x_b = bufs("x_tok", 4)
    p_tok = sb("p_tok", [128, 4, 256], F32)
    p_b = Buf("p_tok")
    p_bf = sb("p_bf", [128, 4, 256], BF16)
    p_bfb = Buf("p_bf")
    sb_xbfull = sb("xbfull", [128, 4, D], BF16)
    xbfb = bufs("xbf", 4)
    t1v = sb_xbfull.rearrange("p s f -> p (s f)").rearrange("p (c t) -> p c t", c=8)
    hT = sb("hT", [128, 8, T], BF16)
    hTb = bufs("hT", 8)
    actB = sb("actB", [128, 8, T], BF16)
    actBb = bufs("actB", 8)
    pT = sb("pT", [128, 2, T], BF16)
    pTb = bufs("pT", 2)
    XN = 20608
    arena = sb("arena", [128, XN], BF16)
    UGW = T + KB - 1
    XAW = T + 4
    ug = arena[:, 0:8 * UGW].rearrange("p (c t) -> p c t", c=8)
    o_c = 8 * UGW + (8 * UGW) % 2
    c_sb = arena[:, o_c:o_c + 2 * 8 * T].bitcast(F32).rearrange("p (c t) -> p c t", c=8)
    o_d = o_c + 2 * 8 * T
    dbuf = [arena[:, o_d + i * KB * 128:o_d + (i + 1) * KB * 128].rearrange("p (k j) -> p k j", k=KB)
            for i in range(2)]
    assert o_d + 2 * KB * 128 <= XN
    xaT = arena[:, 0:8 * XAW].rearrange("p (c t) -> p c t", c=8)
    o1 = 4128
    xcT = arena[:, o1:o1 + 8 * T].rearrange("p (c t) -> p c t", c=8)
    o2 = o1 + 8 * T
    qT = arena[:, o2:o2 + 8 * T].rearrange("p (c t) -> p c t", c=8)
    o3 = o2 + 8 * T
    kT = arena[:, o3:o3 + 8 * T].rearrange("p (c t) -> p c t", c=8)
    o4 = o3 + 8 * T
    ha_tok = arena[:, o4:o4 + 4 * D].rearrange("p (s f) -> p s f", s=4)
    assert o4 + 4 * D <= XN
    actA = qT
    t1 = arena[:, o3:o3 + 2 * 8 * T].bitcast(F32).rearrange("p (c t) -> p c t", c=8)
    mergedT = arena[:, 0:8 * T].rearrange("p (c t) -> p c t", c=8)
    ugb = bufs("ug", 8)
    ughb = Buf("ugh")
    c_sbb = bufs("c_sb", 8)
    dbufb = bufs("dbuf", 2)
    xaTb = bufs("xaT", 8)
    xahb = Buf("xah")
    xcTb = bufs("xcT", 8)
    qTb = bufs("qT", 8)
    kTb = bufs("kT", 8)
    hatb = bufs("ha_tok", 4)
    actAb = bufs("actA", 8)
    t1b = bufs("t1", 8)
    mTb = bufs("mergedT", 8)
    out_tok = arena[:, o_c:o_c + 2 * 8 * T].bitcast(F32).rearrange("p (s f) -> p s f", s=4)
    outb = bufs("out_tok", 4)
    PHASE_B = ugb + [ughb] + c_sbb + dbufb
    PHASE_A = xaTb + [xahb] + xcTb + qTb + kTb + hatb
    ugh = sb("ugh", [128, 8, KB - 1], BF16)
    xah = sb("xah", [128, 8, KA - 1], BF16)
    ughsb = Buf("ugh_s")
    xahsb = Buf("xah_s")

    tmpf = [sb(f"tmpf{i}", [128, T], F32) for i in range(3)]
    tmpfb = bufs("tmpf", 3)
    tmpb = [sb(f"tmpb{i}", [128, T], BF16) for i in range(4)]
    tmpbb = bufs("tmpb", 4)
    tctr = {"f": 0, "b": 0}

    def tf():
        i = tctr["f"] % 3
        tctr["f"] += 1
        return tmpf[i], tmpfb[i]

    def tb():
        i = tctr["b"] % 4
        tctr["b"] += 1
        return tmpb[i], tmpbb[i]

    ssq = sb("ssq", [128, 12], F32)
    ssqb = Buf("ssq")
    rr = sb("rr", [128, 12], F32)
    rrb = Buf("rr")
    g_A = sb("g_A", [128, T], F32)
    g_l = sb("g_l", [128, T], F32)
    g_L = sb("g_L", [128, T], F32)
    g_w = sb("g_w", [128, T], F32)
    g_cl = sb("g_cl", [128, T], F32)
    g_small = sb("g_small", [128, 64], F32)
    gb_ = Buf("gates")
    gt = sb("gt", [128, 32], F32)
    gtb = Buf("gt")
    dec = sb("dec", [128, 5, 4], F32)
    decb = Buf("dec")
    Chat = sb("Chat", [128, 8, 257], F32)
    Chatb = bufs("Chat", 8)
    Cb = sb("Cb", [128, 8, 257], BF16)
    Cbb = bufs("Cb", 8)
    NMB = 3
    wk_tok = [sb(f"wk_tok{i}", [128, 256], BF16) for i in range(NMB)]
    wk_tokb = bufs("wk_tok", NMB)
    v_ext = [sb(f"v_ext{i}", [128, 257], BF16) for i in range(NMB)]
    v_extb = bufs("v_ext", NMB)
    S_sb = [sb(f"S_sb{i}", [128, 128], BF16) for i in range(NMB)]
    S_sbb = bufs("S_sb", NMB)
    sm = [sb(f"sm{i}", [128, 16], F32) for i in range(NMB)]
    smb = bufs("sm", NMB)

    for i in range(NMB):
        k.memset("pool", v_ext[i][:, 256:257], 1.0, [v_extb[i]])

    ds_w = [k.dsem(f"w{i}") for i in range(NSLOT)]
    ds_wple = k.dsem("wple")
    ds_wc = [k.dsem(f"wc{i}") for i in range(NSLOT)]
    ds_wplec = k.dsem("wplec")
    ds_st = [k.dsem(f"st{i}") for i in range(NST)]
    ds_sc = [k.dsem(f"sc{i}") for i in range(12)]
    ds_x = [k.dsem(f"x{i}") for i in range(4)]
    ds_p = k.dsem("p")
    ds_db = [k.dsem(f"db{i}") for i in range(2)]
    ds_dsc = [k.dsem(f"dsc{i}") for i in range(8)]
    dscb = bufs("dsc", 8)
    ds_o = [k.dsem(f"o{i}") for i in range(4)]
    scb = bufs("scratch", 12)
    converted = set()
    sctr = {"slot": 0, "st": 0, "cast": 0}

    def w_src(g):
        if g < 7:
            return w_in_d[:, g * D:(g + 1) * D]
        return {G_PA: w_pa_d, G_PW2: w_pw2_d, G_OUT: w_out_d, G_PG: w_pg_d}[g]

    USES_PER_TILE = [G_UB, G_GB, G_ZB, G_XA, G_PW2, G_GBM, G_ZA, G_PA, G_GA, G_OUT, G_PG]
    uses = USES_PER_TILE * (n_seq * n_tiles)
    fills = []
    wst = {"consumed": 0}

    def issue_fill():
        u = len(fills)
        if u >= len(uses):
            return
        g = uses[u]
        i = u % NSLOT
        sl, slb = wslot[i], wslotb[i]
        if g in converted:
            for q in range(4):
                k.dma(sl[q * 32:(q + 1) * 32].rearrange("p a b -> p (a b)"),
                      wsc_d[g, q * 32:(q + 1) * 32, :], [scb[g]], [slb], ds_w[i])
        else:
            src = w_src(g)
            for kc in range(8):
                k.dma(sl[:, kc, :], src[kc * 128:(kc + 1) * 128, :], (), [slb], ds_wc[i], en="pool")
            k.dma(wsc_d[g], sl.rearrange("p a b -> p (a b)"), [slb], [scb[g]], ds_sc[g])
            converted.add(g)
        fills.append((sl, slb))

    def fill_slot(g):
        u = wst["consumed"]
        assert uses[u] == g, (u, uses[u], g)
        while len(fills) <= u:
            issue_fill()
        wst["consumed"] += 1
        return fills[u]

    def wdone():
        while len(fills) < min(len(uses), wst["consumed"] + NSLOT) and uses[len(fills)] not in converted:
            issue_fill()

    def fill_wple():
        if "ple" in converted:
            k.dma(wple.rearrange("p a b -> p (a b)"), wsc_ple_d, [scb[11]], [wpleb], ds_wple)
        else:
            for kc in range(2):
                k.dma(wple[:, kc, :], w_ple_d[kc * 128:(kc + 1) * 128, :], (), [wpleb], ds_wplec, en="pool")
            k.dma(wsc_ple_d, wple.rearrange("p a b -> p (a b)"), [wpleb], [scb[11]], ds_sc[11])
            converted.add("ple")

    def vcol(base, c):
        return vec[:, base + c:base + c + 1]

    def rms_stats(col0):
        for s in range(4):
            k.act(sb_xbfull[:, s, :], x_tok[:, s, :], AF.Square, [x_b[s]], [xbfb[s], ssqb],
                  accum=ssq[:, col0 + s:col0 + s + 1])
        k.ts("dve", rr[:, col0:col0 + 4], ssq[:, col0:col0 + 4], 1.0 / D, EPS, ALU.mult, ALU.add,
             [ssqb], [rrb])
        k.act(rr[:, col0:col0 + 4], rr[:, col0:col0 + 4], AF.Ln, [rrb], [rrb])
        k.act(rr[:, col0:col0 + 4], rr[:, col0:col0 + 4], AF.Exp, [rrb], [rrb], scale=-0.5)

    dbg = {}
    first_out = {"done": False}

    def tile_body(seq, ti):
        t0 = seq * SEQ + ti * T
        first = ti == 0
        for s in range(4):
            k.dma(x_tok[:, s, :], x_d[t0 + s * 128:t0 + (s + 1) * 128, :], (), [x_b[s]], ds_x[s])
        k.dma(p_tok, p_d[t0:t0 + T, :].rearrange("(s p) f -> p s f", p=128), (), [p_b], ds_p)

        rms_stats(0)
        xbt = sb_xbfull
        for s in range(4):
            k.ts("dve", xbt[:, s, :], x_tok[:, s, :], rr[:, s:s + 1], None, ALU.mult, None,
                 [x_b[s], rrb], [xbfb[s]])
        for c in range(8):
            pt, ptb = psb()
            for s in range(4):
                k.tr(pt[:, s * 128:(s + 1) * 128], xbt[:, s, c * 128:(c + 1) * 128], identb,
                     [xbfb[s]] + CONSTS, [ptb])
            k.act(hT[:, c, :], pt[:, 0:T], AF.Copy, [ptb] + CONSTS, [hTb[c]], scale=vcol(C_NG, c))

        chk("hT")
        k.handoff(PHASE_A + actAb + t1b + mTb + outb, PHASE_B)
        if first:
            k.memset("pool", ugh.rearrange("p a b -> p (a b)"), 0.0, [ughsb])
        k.cp("pool", ug[:, :, 0:KB - 1], ugh, [ughsb], [ughb])
        wu, wub = fill_slot(G_UB)
        wg, wgb = fill_slot(G_GB)
        for m in range(8):
            pu, pub = psf()
            pg, pgb = psf()
            for kc in range(8):
                k.mm(pu, wu[:, kc, m * 128:(m + 1) * 128], hT[:, kc, :], kc == 0, kc == 7,
                     [wub, hTb[kc]], [pub])
            for kc in range(8):
                k.mm(pg, wg[:, kc, m * 128:(m + 1) * 128], hT[:, kc, :], kc == 0, kc == 7,
                     [wgb, hTb[kc]], [pgb])
            sg, sgb = tf()
            k.act(sg, pg, AF.Sigmoid, [pgb], [sgb])
            k.tt("dve", ug[:, m, KB - 1:], pu, sg, ALU.mult, [pub, sgb], [ugb[m]])
        wdone()
        k.cp("pool", ugh, ug[:, :, T:T + KB - 1], ugb, [ughsb])

        chk("ug")
        wz, wzb = fill_slot(G_ZB)
        pi0, pi1 = pspin(), pspin()
        psum_s, psum_sb = PSA[pi0], PSAb[pi0]
        psum_q, psum_qb = PSA[pi1], PSAb[pi1]
        for m in range(8):
            db, dbb = dbuf[m % 2], dbufb[m % 2]
            if ("diag", m) in converted:
                k.dma(db.rearrange("p a b -> p (a b)"), dsc_d[m], [dscb[m]], [dbb], ds_db[m % 2])
            else:
                for kk in range(KB):
                    col = C_CBW + m * KB + kk
                    if kk % 2 == 0:
                        k.ts("dve", db[:, kk, :], identb, vec[:, col:col + 1], None, ALU.mult, None,
                             CONSTS, [dbb])
                    else:
                        k.act(db[:, kk, :], identb, AF.Copy, CONSTS, [dbb], scale=vec[:, col:col + 1])
                k.dma(dsc_d[m], db.rearrange("p a b -> p (a b)"), [dbb], [dscb[m]], ds_dsc[m])
                converted.add(("diag", m))
            pc, pcb = psf()
            for kk in range(KB):
                k.mm(pc, db[:, kk, :], ug[:, m, kk:kk + T], kk == 0, kk == KB - 1,
                     [dbb, ugb[m], ughb], [pcb])
            k.act(c_sb[:, m, :], pc, AF.Identity, [pcb] + CONSTS, [c_sbb[m]], bias=vcol(C_CBB, m))
            cb_, cbb_ = tb()
            k.cp("dve", cb_, c_sb[:, m, :], [c_sbb[m]], [cbb_])
            cq, cqb = tb()
            k.act(cq, c_sb[:, m, :], AF.Square, [c_sbb[m]], [cqb])
            k.mm(psum_s, onesb, cb_, m == 0, m == 7, [cbb_] + CONSTS, [psum_sb])
            k.mm(psum_q, onesb, cq, m == 0, m == 7, [cqb] + CONSTS, [psum_qb])
        chk("c_sb")
        mean, meanb = g_w, Buf("ln_mean")
        msq, msqb = g_l, Buf("ln_msq")
        rstd, rstdb = g_cl, Buf("ln_rstd")
        k.handoff([gb_], [meanb, msqb, rstdb])
        pinned.discard(pi0)
        pinned.discard(pi1)
        k.act(mean, psum_s, AF.Copy, [psum_sb], [meanb], scale=1.0 / D)
        k.tt("dve", msq, mean, mean, ALU.mult, [meanb], [msqb])
        k.stt(rstd, psum_q, 1.0 / D, msq, ALU.mult, ALU.subtract, [psum_qb, msqb], [rstdb])
        k.ts("dve", rstd, rstd, EPS, None, ALU.add, None, [rstdb], [rstdb])
        k.act(rstd, rstd, AF.Ln, [rstdb], [rstdb])
        k.act(rstd, rstd, AF.Exp, [rstdb], [rstdb], scale=-0.5)
        LNB_ = [meanb, msqb, rstdb]
        for m in range(8):
            pz, pzb = psf()
            for kc in range(8):
                k.mm(pz, wz[:, kc, m * 128:(m + 1) * 128], hT[:, kc, :], kc == 0, kc == 7,
                     [wzb, hTb[kc]], [pzb])
            sz, szb = tb()
            k.act(sz, pz, AF.Silu, [pzb], [szb])
            cn, cnb = tf()
            k.tt("dve", cn, c_sb[:, m, :], mean, ALU.subtract, [c_sbb[m], meanb], [cnb])
            k.tt("dve", cn, cn, rstd, ALU.mult, [cnb, rstdb], [cnb])
            sc, scb_ = tb()
            k.act(sc, cn, AF.Silu, [cnb] + CONSTS, [scb_], bias=vcol(C_LNB, m), scale=vcol(C_LNG, m))
            k.tt("dve", actB[:, m, :], sc, sz, ALU.mult, [scb_, szb], [actBb[m]])

        wdone()
        k.handoff(LNB_, [gb_])
        chk("actB")
        k.handoff(PHASE_B, PHASE_A)
        if first:
            k.memset("pool", xah.rearrange("p a b -> p (a b)"), 0.0, [xahsb])
        k.cp("pool", xaT[:, :, 1:4], xah, [xahsb], [xahb])
        wx, wxb = fill_slot(G_XA)
        for m in range(8):
            px, pxb = psf()
            for kc in range(8):
                k.mm(px, wx[:, kc, m * 128:(m + 1) * 128], hT[:, kc, :], kc == 0, kc == 7,
                     [wxb, hTb[kc]], [pxb])
            k.cp("act", xaT[:, m, 4:], px, [pxb], [xaTb[m]])
        wdone()
        k.cp("pool", xah, xaT[:, :, T + 1:T + 4], xaTb, [xahsb])
        pi0, pi1 = pspin(), pspin()
        pig, pigb = PSA[pi0], PSAb[pi0]
        pfg, pfgb = PSA[pi1], PSAb[pi1]
        for m in range(8):
            pc, pcb = psf()
            for kk in range(KA):
                k.mm(pc, diag4[:, kk * 8 + m, :], xaT[:, m, 1 + kk:1 + kk + T], kk == 0, kk == KA - 1,
                     [xaTb[m], xahb] + CONSTS, [pcb])
            k.act(xcT[:, m, :], pc, AF.Silu, [pcb] + CONSTS, [xcTb[m]], bias=vcol(C_CAB, m))
        for m in range(8):
            pq, pqb = psf()
            k.mm(pq, bd[:, 0 * 8 + m, :], xcT[:, m, :], True, True, [xcTb[m]] + CONSTS, [pqb])
            k.cp("act", qT[:, m, :], pq, [pqb], [qTb[m]])
            pk, pkb = psf()
            k.mm(pk, bd[:, 1 * 8 + m, :], xcT[:, m, :], True, True, [xcTb[m]] + CONSTS, [pkb])
            k.cp("dve", kT[:, m, :], pk, [pkb], [kTb[m]])
            pv, pvb = psf()
            k.mm(pv, bd[:, 2 * 8 + m, :], xaT[:, m, 4:], True, True, [xaTb[m]] + CONSTS, [pvb])
            vt, vtb = tb()
            k.cp("act", vt, pv, [pvb], [vtb])
            for gi, (pp, ppb) in enumerate(((pig, pigb), (pfg, pfgb))):
                lo = gi * 4
                k.mm(pp[0:4, :], wifb[:, m, lo:lo + 4], qT[:, m, :], m == 0, False,
                     [qTb[m]] + CONSTS, [ppb])
                k.mm(pp[0:4, :], wifb[:, 8 + m, lo:lo + 4], kT[:, m, :], False, False,
                     [kTb[m]] + CONSTS, [ppb])
                k.mm(pp[0:4, :], wifb[:, 16 + m, lo:lo + 4], vt, False, m == 7,
                     [vtb] + CONSTS, [ppb])

        chk("qk")
        pinned.discard(pi0)
        pinned.discard(pi1)
        G = [gb_]
        A_ = g_A[0:4, :]
        l_ = g_l[0:4, :]
        L_ = g_L[0:4, :]
        cm = g_small[0:4, 0:4]
        mus = g_small[0:4, 4:9]
        negmu = g_small[0:4, 9:14]
        dcy = g_small[0:4, 14:18]
        Lc = g_small[0:4, 18:19]
        Dexp = g_small[0:4, 20:36]
        if first:
            k.memset("dve", g_small[0:4, :], 0.0, G)
            k.memset("dve", dec.rearrange("p a b -> p (a b)"), 1.0, [decb])
        else:
            k.cp("dve", dec[:, 0, :], dec[:, 4, :], [decb], [decb])
        k.act(A_, pig[0:4, :], AF.Identity, [pigb] + CONSTS, G, bias=vec[0:4, C_BIF:C_BIF + 1])
        k.act(l_, pfg[0:4, :], AF.Exp, [pfgb] + CONSTS, G, bias=negbf[0:4, :], scale=-1.0)
        k.act(l_, l_, AF.Ln, G, G, bias=1.0)
        k.iss("dve", lambda h: h.tensor_tensor_scan(L_, ones32[0:4, 0:T], l_, Lc, ALU.mult, ALU.add),
              G + CONSTS, G)
        k.cp("dve", Lc, L_[:, T - 1:T], G, G)
        k.tt("dve", A_, A_, L_, ALU.add, G, G)
        k.iss("dve", lambda h: h.tensor_reduce(cm, A_.rearrange("p (c t) -> p c t", c=4), AX.X, ALU.max),
              G, G)
        if not first:
            k.cp("dve", mus[:, 0:1], mus[:, 4:5], G, G)
        k.iss("dve", lambda h: h.tensor_tensor_scan(mus[:, 1:5], cm, cm, mus[:, 0:1], ALU.max, ALU.max),
              G, G)
        k.ts("dve", negmu, mus, -1.0, None, ALU.mult, None, G, G)
        k.tt("dve", dcy, mus[:, 0:4], mus[:, 1:5], ALU.subtract, G, G)
        k.act(dcy, dcy, AF.Exp, G, G)
        negmu16 = g_small[0:4, 36:41]
        k.ts("dve", negmu16, negmu, -math.log(16.0), None, ALU.add, None, G, G)
        w_ = g_w[0:4, :]
        cl_ = g_cl[0:4, :]
        for c in range(4):
            k.act(w_[:, c * 128:(c + 1) * 128], A_[:, c * 128:(c + 1) * 128], AF.Exp, G, G,
                  bias=negmu16[:, c:c + 1])
            k.act(cl_[:, c * 128:(c + 1) * 128], L_[:, c * 128:(c + 1) * 128], AF.Exp, G, G,
                  bias=negmu[:, c:c + 1])
            k.ts("dve", Dexp[:, c * 4:(c + 1) * 4], ident32[0:4, 0:4], dcy[:, c:c + 1], None, ALU.mult, None,
                 G + CONSTS, G)
        pgt, pgtb = psf()
        for c in range(4):
            k.mm(pgt[:, c * 8:c * 8 + 4], w_[:, c * 128:(c + 1) * 128], ident32[0:4, 0:4], True, True,
                 G + CONSTS, [pgtb])
            k.mm(pgt[:, c * 8 + 4:c * 8 + 8], cl_[:, c * 128:(c + 1) * 128], ident32[0:4, 0:4], True, True,
                 G + CONSTS, [pgtb])
        k.mm(pgt[:, 32:48], ones32[0:4, 0:128], Dexp, True, True, G + CONSTS, [pgtb])
        k.cp("dve", gt, pgt[:, 0:32], [pgtb], [gtb])
        k.cp("dve", dec[:, 1:5, :].rearrange("p a b -> p (a b)"), pgt[:, 32:48], [pgtb], [decb])

        chk("gates")
        if first:
            k.memset("pool", Chat.rearrange("p a b -> p (a b)"), 0.0, Chatb)
            k.memset("pool", Cb.rearrange("p a b -> p (a b)"), 0.0, Cbb)
        chk("m0a")
        steps = [(c, h) for c in range(4) for h in range(H)]
        ctx = {}

        def stage_a(i):
            c, h = steps[i]
            j = i % NMB
            tsl = slice(c * 128, (c + 1) * 128)
            wcol = gt[:, c * 8 + h:c * 8 + h + 1]
            pkv, pkvb = psf()
            for dc in range(2):
                m = 2 * h + dc
                k.mm(pkv[:, dc * 128:(dc + 1) * 128], xcT[:, m, tsl], bd[:, 8 + m, :], True, True,
                     [xcTb[m]] + CONSTS, [pkvb])
                k.mm(pkv[:, 256 + dc * 128:256 + (dc + 1) * 128],
                     xaT[:, m, 4 + c * 128:4 + (c + 1) * 128], bd[:, 16 + m, :], True, True,
                     [xaTb[m]] + CONSTS, [pkvb])
            k.act(wk_tok[j], pkv[:, 0:256], AF.Copy, [pkvb, gtb], [wk_tokb[j]], scale=wcol)
            k.cp("act", v_ext[j][:, 0:256], pkv[:, 256:512], [pkvb], [v_extb[j]])
            pS, pSb = psf()
            for dc in range(2):
                m = 2 * h + dc
                k.mm(pS[:, 0:128], kT[:, m, tsl], qT[:, m, tsl], dc == 0, dc == 1,
                     [kTb[m], qTb[m]], [pSb])
            k.stt(S_sb[j], pS[:, 0:128], wcol, trimask, ALU.mult, ALU.mult, [pSb, gtb] + CONSTS, [S_sbb[j]])

        def stage_b(i):
            c, h = steps[i]
            j = i % NMB
            tsl = slice(c * 128, (c + 1) * 128)
            clcol = gt[:, c * 8 + 4 + h:c * 8 + 4 + h + 1]
            pidx = pspin()
            pin, pinb = PSA[pidx], PSAb[pidx]
            ctx[i] = (pin, pinb, pidx)
            k.mm(pin[:, 0:257], S_sb[j], v_ext[j], True, False, [S_sbb[j], v_extb[j]], [pinb])
            for dc in range(2):
                m = 2 * h + dc
                k.mm(pin[:, 0:257], qT[:, m, tsl], Cb[:, m, :], False, dc == 1,
                     [qTb[m], Cbb[m]], [pinb])
            for dc in range(2):
                m = 2 * h + dc
                pu_, pub_ = psf()
                k.mm(pu_[:, 0:257], wk_tok[j][:, dc * 128:(dc + 1) * 128], v_ext[j], True, True,
                     [wk_tokb[j], v_extb[j]], [pub_])
                k.stt(Chat[:, m, :], Chat[:, m, :], dec[:, c, h:h + 1], pu_[:, 0:257], ALU.mult, ALU.add,
                      [Chatb[m], decb, pub_], [Chatb[m]])
                k.act(Cb[:, m, :], Chat[:, m, :], AF.Copy, [Chatb[m], decb], [Cbb[m]],
                      scale=dec[:, c + 1, h:h + 1])
            s_ = sm[j]
            k.act(s_[:, 0:1], pin[:, 256:257], AF.Abs, [pinb], [smb[j]])
            k.iss("dve", lambda hd, o=s_[:, 2:8], i_=pin[:, 0:256]: hd.bn_stats(o, i_), [pinb], [smb[j]])
            k.iss("dve", lambda hd, o=s_[:, 8:10], i_=s_[:, 2:8]: hd.bn_aggr(o, i_), [smb[j]], [smb[j]])
            k.ts("dve", s_[:, 0:1], s_[:, 0:1], clcol, None, ALU.max, None, [smb[j], gtb], [smb[j]])
            k.iss("dve", lambda hd, o=s_[:, 1:2], i_=s_[:, 0:1]: hd.reciprocal(o, i_), [smb[j]], [smb[j]])
            k.tt("dve", s_[:, 10:11], s_[:, 1:2], s_[:, 1:2], ALU.mult, [smb[j]], [smb[j]])
            k.ts("dve", s_[:, 10:11], s_[:, 10:11], s_[:, 9:10], EPS, ALU.mult, ALU.add, [smb[j]], [smb[j]])
            k.act(s_[:, 10:11], s_[:, 10:11], AF.Ln, [smb[j]], [smb[j]])
            k.act(s_[:, 10:11], s_[:, 10:11], AF.Exp, [smb[j]], [smb[j]], scale=-0.5)

        def stage_c(i):
            c, h = steps[i]
            j = i % NMB
            pin, pinb, pidx = ctx[i]
            s_ = sm[j]
            k.tt("dve", s_[:, 11:12], s_[:, 10:11], s_[:, 1:2], ALU.mult, [smb[j]], [smb[j]])
            k.ts("dve", ha_tok[:, c, h * DH:(h + 1) * DH], pin[:, 0:256], s_[:, 8:9], s_[:, 11:12],
                 ALU.subtract, ALU.mult, [pinb, smb[j]], [hatb[c]])
            pinned.discard(pidx)

        wpw, wpwb = fill_slot(G_PW2)
        wgm, wgmb = fill_slot(G_GBM)

        def filler(m):
            pa, pab = psf()
            pg, pgb = psf()
            for kc in range(8):
                k.mm(pa, wpw[:, kc, m * 128:(m + 1) * 128], actB[:, kc, :], kc == 0, kc == 7,
                     [wpwb, actBb[kc]], [pab])
            for kc in range(8):
                k.mm(pg, wgm[:, kc, m * 128:(m + 1) * 128], hT[:, kc, :], kc == 0, kc == 7,
                     [wgmb, hTb[kc]], [pgb])
            sg, sgb = tf()
            k.act(sg, pg, AF.Sigmoid, [pgb], [sgb])
            k.stt(t1v[:, m, :], pa, vcol(C_BPW, m), sg, ALU.add, ALU.mult, [pab, sgb] + CONSTS, [xbfb[m // 2]])

        stage_a(0)
        for i in range(16):
            if i + 1 < 16:
                stage_a(i + 1)
            stage_b(i)
            if i >= 1:
                stage_c(i - 1)
            if i % 2 == 1:
                filler(i // 2)
        stage_c(15)
        wdone()

        chk("ha")
        k.handoff(qTb, actAb)
        wza, wzab = fill_slot(G_ZA)
        for m in range(8):
            pt, ptb = psb()
            for s in range(4):
                k.tr(pt[:, s * 128:(s + 1) * 128], ha_tok[:, s, m * 128:(m + 1) * 128], identb,
                     [hatb[s]] + CONSTS, [ptb])
            pz, pzb = psf()
            for kc in range(8):
                k.mm(pz, wza[:, kc, m * 128:(m + 1) * 128], hT[:, kc, :], kc == 0, kc == 7,
                     [wzab, hTb[kc]], [pzb])
            sz, szb = tb()
            k.act(sz, pz, AF.Silu, [pzb], [szb])
            sk, skb = tf()
            k.ts("dve", sk, xcT[:, m, :], vcol(C_SKP, m), None, ALU.mult, None, [xcTb[m]] + CONSTS, [skb])
            k.stt(sk, pt[:, 0:T], vcol(C_MHG, m), sk, ALU.mult, ALU.add, [ptb, skb] + CONSTS, [skb])
            k.tt("dve", actA[:, m, :], sk, sz, ALU.mult, [skb, szb], [actAb[m]])

        wdone()
        chk("actA")
        k.handoff(xaTb + [xahb], mTb)
        wpa, wpab = fill_slot(G_PA)
        wga, wgab = fill_slot(G_GA)
        for m in range(8):
            pa, pab = psf()
            pg, pgb = psf()
            for kc in range(8):
                k.mm(pa, wpa[:, kc, m * 128:(m + 1) * 128], actA[:, kc, :], kc == 0, kc == 7,
                     [wpab, actAb[kc]], [pab])
            for kc in range(8):
                k.mm(pg, wga[:, kc, m * 128:(m + 1) * 128], hT[:, kc, :], kc == 0, kc == 7,
                     [wgab, hTb[kc]], [pgb])
            sg, sgb = tf()
            k.act(sg, pg, AF.Sigmoid, [pgb], [sgb])
            k.tt("dve", sg, pa, sg, ALU.mult, [pab, sgb], [sgb])
            k.tt("dve", mergedT[:, m, :], sg, t1v[:, m, :], ALU.add, [sgb, xbfb[m // 2]], [mTb[m]])

        wdone()
        chk("merged")
        wo, wob = fill_slot(G_OUT)
        for s in range(4):
            for hf in range(2):
                po, pob = psf()
                for kc in range(8):
                    k.mm(po, mergedT[:, kc, s * 128:(s + 1) * 128], wo[:, kc, hf * 512:(hf + 1) * 512],
                         kc == 0, kc == 7, [mTb[kc], wob], [pob])
                k.tt("dve", x_tok[:, s, hf * 512:(hf + 1) * 512], x_tok[:, s, hf * 512:(hf + 1) * 512], po,
                     ALU.add, [x_b[s], pob], [x_b[s]])

        wdone()
        chk("x1")
        rms_stats(4)
        for s in range(4):
            k.ts("dve", xbt[:, s, :], x_tok[:, s, :], rr[:, 4 + s:5 + s], None, ALU.mult, None,
                 [x_b[s], rrb], [xbfb[s]])
        for c in range(8):
            pt, ptb = psb()
            for s in range(4):
                k.tr(pt[:, s * 128:(s + 1) * 128], xbt[:, s, c * 128:(c + 1) * 128], identb,
                     [xbfb[s]] + CONSTS, [ptb])
            k.act(hT[:, c, :], pt[:, 0:T], AF.Copy, [ptb] + CONSTS, [hTb[c]], scale=vcol(C_PNG, c))
        k.cp("pool", p_bf.rearrange("p a b -> p (a b)"), p_tok.rearrange("p a b -> p (a b)"), [p_b], [p_bfb])
        for pc_ in range(2):
            pt, ptb = psb()
            for s in range(4):
                k.tr(pt[:, s * 128:(s + 1) * 128], p_bf[:, s, pc_ * 128:(pc_ + 1) * 128], identb,
                     [p_bfb] + CONSTS, [ptb])
            k.cp("act", pT[:, pc_, :], pt[:, 0:T], [ptb], [pTb[pc_]])
        wpg, wpgb = fill_slot(G_PG)
        for s in range(4):
            for hf in range(2):
                pg, pgb = psf()
                pp, ppb = psf()
                for kc in range(8):
                    k.mm(pg, hT[:, kc, s * 128:(s + 1) * 128], wpg[:, kc, hf * 512:(hf + 1) * 512],
                         kc == 0, kc == 7, [hTb[kc], wpgb], [pgb])
                for kc in range(2):
                    k.mm(pp, pT[:, kc, s * 128:(s + 1) * 128], wple[:, kc, hf * 512:(hf + 1) * 512],
                         kc == 0, kc == 1, [pTb[kc], wpleb], [ppb])
                sg, sgb = tf()
                k.act(sg, pg, AF.Sigmoid, [pgb], [sgb])
                k.tt("dve", sg, sg, pp, ALU.mult, [sgb, ppb], [sgb])
                k.tt("dve", x_tok[:, s, hf * 512:(hf + 1) * 512], x_tok[:, s, hf * 512:(hf + 1) * 512], sg,
                     ALU.add, [x_b[s], sgb], [x_b[s]])

        wdone()
        while len(fills) < min(len(uses), wst["consumed"] + NSLOT):
            issue_fill()
        if not (seq == n_seq - 1 and ti == n_tiles - 1):
            fill_wple()
        chk("x2")
        rms_stats(8)
        k.handoff(xcTb + actAb + qTb + kTb, outb)
        for s in range(4):
            k.stt(out_tok[:, s, :], x_tok[:, s, :], rr[:, 8 + s:9 + s], fgbc, ALU.mult, ALU.mult,
                  [x_b[s], rrb] + CONSTS, [outb[s]])
            if STOP != "nostore":
                k.dma(out_d[t0 + s * 128:t0 + (s + 1) * 128, :], out_tok[:, s, :], [outb[s]], (), ds_o[s], en="pool")

    def convert_all():
        for g in USES_PER_TILE:
            src = w_src(g)
            for kc in range(8):
                k.dma(wsc_d[g][:, kc * D:(kc + 1) * D], src[kc * 128:(kc + 1) * 128, :], (), [scb[g]], ds_sc[g],
                      en="pool")
            converted.add(g)
        for kc in range(2):
            k.dma(wsc_ple_d[:, kc * D:(kc + 1) * D], w_ple_d[kc * 128:(kc + 1) * 128, :], (), [scb[11]], ds_sc[11],
                  en="pool")
        converted.add("ple")

    convert_all()
    fill_wple()
    try:
        for seq in range(n_seq):
            for ti in range(n_tiles):
                tile_body(seq, ti)
    except _Stop:
        pass

    k.emit(ds_o)
    return nc


_NC_CACHE = {}


def _host_consts(inp):
    f = np.float32
    vec = np.zeros((128, NV), f)

    def chunks(v):
        return np.ascontiguousarray(np.asarray(v, f).reshape(8, 128).T)

    vec[:, C_NG:C_NG + 8] = chunks(inp["norm_g"][0])
    vec[:, C_CAB:C_CAB + 8] = chunks(inp["conv_a_b"][0])
    vec[:, C_MHG:C_MHG + 8] = chunks(inp["mh_norm_g"][0])
    vec[:, C_SKP:C_SKP + 8] = chunks(inp["skip_a"][0])
    vec[:, C_CBB:C_CBB + 8] = chunks(inp["conv_b_b"][0])
    vec[:, C_LNG:C_LNG + 8] = chunks(inp["ln_b_g"][0])
    vec[:, C_LNB:C_LNB + 8] = chunks(inp["ln_b_b"][0])
    vec[:, C_BPW:C_BPW + 8] = chunks(inp["b_pw2"][0])
    vec[:, C_PNG:C_PNG + 8] = chunks(inp["ple_norm_g"][0])
    caw = np.asarray(inp["conv_a_w"][0], f)
    for kk in range(KA):
        vec[:, C_CAW + kk * 8:C_CAW + kk * 8 + 8] = chunks(caw[kk])
    cbw = np.asarray(inp["conv_b_w"][0], f)
    for c in range(8):
        vec[:, C_CBW + c * KB:C_CBW + (c + 1) * KB] = cbw[:, c * 128:(c + 1) * 128].T
    for base, nm in ((C_WQ, "wq"), (C_WK, "wk"), (C_WV, "wv")):
        w = np.asarray(inp[nm][0], f)
        for c in range(8):
            vec[:, base + c * 4:base + c * 4 + 4] = w[c * 32:(c + 1) * 32].reshape(128, 4)
    bif = np.asarray(inp["b_if"][0], f)
    vec[0:4, C_BIF] = bif[0:4]
    vec[0:4, C_BIF + 1] = bif[4:8]
    cmat = np.zeros((128, 288), f)
    cmat[:, 0:128] = np.eye(128, dtype=f)
    cmat[:, 128:256] = np.triu(np.ones((128, 128), f))
    cmat[:, 256:288] = (np.arange(128)[:, None] // 4 == np.arange(32)[None, :]).astype(f)
    fg = np.ascontiguousarray(np.broadcast_to(np.asarray(inp["final_g"], f)[None, :], (128, D)))
    wif = np.asarray(inp["w_if"][0], f).reshape(24, 128, 8).transpose(1, 0, 2).reshape(128, 192)
    return vec, cmat, fg, np.ascontiguousarray(wif)


def kernel(**inputs):
    n = 8
    x = np.asarray(inputs["x"], np.float32)
    p = np.asarray(inputs["p"], np.float32)[0]
    vec, cmat, fg, wif = _host_consts(inputs)
    if "nc" not in _NC_CACHE:
        _NC_CACHE["nc"] = build_program()
    nc = _NC_CACHE["nc"]
    shared = {
        "w_in": np.ascontiguousarray(np.asarray(inputs["w_in"], np.float32)[0]),
        "w_proj_a": np.ascontiguousarray(np.asarray(inputs["w_proj_a"], np.float32)[0]),
        "w_pw2": np.ascontiguousarray(np.asarray(inputs["w_pw2"], np.float32)[0]),
        "w_out": np.ascontiguousarray(np.asarray(inputs["w_out"], np.float32)[0]),
        "w_ple_gate": np.ascontiguousarray(np.asarray(inputs["w_ple_gate"], np.float32)[0]),
        "w_ple": np.ascontiguousarray(np.asarray(inputs["w_ple"], np.float32)[0]),
        "vecs": vec, "cmat": cmat, "fg_bc": fg, "w_if_t": wif,
    }
    in_maps = []
    for c in range(n):
        m = dict(shared)
        m["x"] = np.ascontiguousarray(x[2 * c:2 * c + 2].reshape(NSEQ * SEQ, D))
        m["p"] = np.ascontiguousarray(p[2 * c:2 * c + 2].reshape(NSEQ * SEQ, 256))
        in_maps.append(m)
    res = run_bass_kernel_spmd(nc, in_maps, core_ids=list(range(n)))
    out = np.stack([np.asarray(r["out"], np.float32).reshape(NSEQ, SEQ, D) for r in res.results], 0)
    return out.reshape(16, SEQ, D)
```

```python
import math
import numpy as np
import ml_dtypes
import concourse.bass as bass
import concourse.mybir as mybir
from concourse.bass_utils import run_bass_kernel_spmd

F32 = mybir.dt.float32
BF16 = mybir.dt.bfloat16
ALU = mybir.AluOpType
AF = mybir.ActivationFunctionType
AX = mybir.AxisListType

D = 1024
SEQ = 2048
NSEQ = 2
T = 512
NT = SEQ // T
H = 4
DH = 256
EPS = 1e-6
KB = 31
KA = 4

C_NG, C_CAB, C_MHG, C_SKP, C_CBB, C_LNG, C_LNB, C_BPW, C_PNG = 0, 8, 16, 24, 32, 40, 48, 56, 64
C_CAW = 72
C_CBW = 104
C_WQ, C_WK, C_WV = 352, 384, 416
C_BIF = 448
NV = 450

G_XA, G_ZA, G_UB, G_GB, G_ZB, G_GA, G_GBM, G_PA, G_PW2, G_OUT, G_PG = range(11)


STOP = None
PHASE = ["setup"]


class _Stop(Exception):
    pass


def chk(name):
    PHASE[0] = name
    if STOP == name:
        raise _Stop()


class Eng:
    def __init__(self, name):
        self.name = name
        self.ops = []
        self.sem = None
        self.seen = {}


class Op:
    __slots__ = ("eng", "fn", "waits", "needed", "idx", "cnt", "dma", "phase")

    def __init__(self, eng, fn):
        self.eng = eng
        self.fn = fn
        self.waits = []
        self.needed = False
        self.idx = len(eng.ops)
        self.cnt = None
        self.dma = None
        self.phase = PHASE[0]


class DSem:
    def __init__(self, name, sem):
        self.name = name
        self.sem = sem
        self.count = 0


class Buf:
    __slots__ = ("name", "w", "r", "psum")

    def __init__(self, name, psum=False):
        self.name = name
        self.w = None
        self.r = {}
        self.psum = psum


def bufs(name, n, psum=False):
    return [Buf(f"{name}{i}", psum) for i in range(n)]


def tokkey(tok):
    return tok[1].eng.name if tok[0] == "op" else ("d", tok[1].name)


class K:
    def __init__(self, nc):
        self.nc = nc
        self.E = {n: Eng(n) for n in ("pe", "act", "dve", "pool", "sp")}
        for n, e in self.E.items():
            e.sem = nc.alloc_semaphore(name=f"sem_{n}")
        self.dsems = {}

    def dsem(self, name):
        if name not in self.dsems:
            self.dsems[name] = DSem(name, self.nc.alloc_semaphore(name=f"dsem_{name}"))
        return self.dsems[name]

    def _add_wait(self, op, tok, kind):
        if tok[0] == "op":
            src = tok[1]
            if src.eng is op.eng:
                if op.eng.name in ("pe", "sp"):
                    return
            if op.eng.seen.get(src.eng.name, -1) >= src.idx:
                return
            op.eng.seen[src.eng.name] = src.idx
            src.needed = True
            op.waits.append(tok)
        else:
            key = ("d", tok[1].name)
            if op.eng.seen.get(key, 0) >= tok[2]:
                return
            op.eng.seen[key] = tok[2]
            op.waits.append(tok)

    def iss(self, en, fn, rd=(), wr=(), dsem=None):
        eng = self.E[en]
        op = Op(eng, fn)
        for b in rd:
            if b.w is not None:
                self._add_wait(op, b.w, "raw")
            if b.psum:
                for kk, t in b.r.items():
                    if kk != eng.name:
                        self._add_wait(op, t, "rar")
        for b in wr:
            if b.w is not None:
                self._add_wait(op, b.w, "waw")
            for t in b.r.values():
                self._add_wait(op, t, "war")
        if dsem is not None:
            dsem.count += 16
            tok = ("dma", dsem, dsem.count)
            op.dma = dsem
        else:
            tok = ("op", op)
        for b in rd:
            b.r[tokkey(tok)] = tok
        for b in wr:
            b.w = tok
            b.r = {}
        eng.ops.append(op)
        return op

    def handoff(self, old, new):
        toks = {}
        for b in old:
            if b.w is not None:
                toks[("w",) + (tokkey(b.w),)] = b.w
            for k, t in b.r.items():
                kk = ("r", k)
                if kk in toks:
                    a = toks[kk]
                    if a[0] == "op":
                        if t[1].idx > a[1].idx:
                            toks[kk] = t
                    elif t[2] > a[2]:
                        toks[kk] = t
                else:
                    toks[kk] = t
        merged = {}
        for (_, k), t in toks.items():
            if k in merged:
                a = merged[k]
                if a[0] == "op":
                    if t[1].idx > a[1].idx:
                        merged[k] = t
                elif t[2] > a[2]:
                    merged[k] = t
            else:
                merged[k] = t
        for b in new:
            b.w = None
            b.r = dict(merged)

    def mm(self, out, lhsT, rhs, start, stop, rd, wr):
        return self.iss("pe", lambda h: h.matmul(out, lhsT, rhs, start=start, stop=stop), rd, wr)

    def tr(self, out, in_, ident, rd, wr):
        return self.iss("pe", lambda h: h.transpose(out, in_, ident), rd, wr)

    def act(self, out, in_, func, rd, wr, bias=None, scale=None, accum=None):
        kw = {}
        if bias is not None:
            kw["bias"] = bias
        if scale is not None:
            kw["scale"] = scale
        if accum is not None:
            kw["accum_out"] = accum
        return self.iss("act", lambda h: h.activation(out, in_, func, **kw), rd, wr)

    def tt(self, en, out, in0, in1, op, rd, wr):
        return self.iss(en, lambda h: h.tensor_tensor(out, in0, in1, op), rd, wr)

    def ts(self, en, out, in0, s1, s2, op0, op1, rd, wr):
        if s2 is None:
            return self.iss(en, lambda h: h.tensor_scalar(out, in0, s1, None, op0), rd, wr)
        return self.iss(en, lambda h: h.tensor_scalar(out, in0, s1, s2, op0, op1), rd, wr)

    def stt(self, out, in0, scalar, in1, op0, op1, rd, wr):
        return self.iss("dve", lambda h: h.scalar_tensor_tensor(out, in0, scalar, in1, op0, op1), rd, wr)

    def cp(self, en, out, in_, rd, wr):
        if en == "act":
            return self.iss("act", lambda h: h.activation(out, in_, AF.Copy), rd, wr)
        return self.iss(en, lambda h: h.tensor_copy(out, in_), rd, wr)

    def memset(self, en, ap, val, wr):
        return self.iss(en, lambda h: h.memset(ap, val), (), wr)

    def dma(self, out, in_, rd, wr, dsem, en="sp"):
        return self.iss(en, lambda h: h.dma_start(out=out, in_=in_), rd, wr, dsem=dsem)

    def emit(self, final_waits):
        nc = self.nc
        for e in self.E.values():
            c = 0
            for op in e.ops:
                if op.needed:
                    c += 1
                    op.cnt = c

        def run(eng, h, extra=()):
            for op in eng.ops:
                for tok in op.waits:
                    if tok[0] == "op":
                        h.wait_ge(tok[1].eng.sem, tok[1].cnt)
                    else:
                        h.wait_ge(tok[1].sem, tok[2])
                ins = op.fn(h)
                if op.dma is not None:
                    ins.then_inc(op.dma.sem, 16)
                elif op.needed:
                    ins.then_inc(eng.sem, 1)
            for ds in extra:
                h.wait_ge(ds.sem, ds.count)

        E = self.E
        with nc.Block() as block:
            @block.tensor
            def _(h):
                run(E["pe"], h)

            @block.scalar
            def _(h):
                run(E["act"], h)

            @block.vector
            def _(h):
                run(E["dve"], h)

            @block.gpsimd
            def _(h):
                run(E["pool"], h, final_waits)

            @block.sync
            def _(h):
                run(E["sp"], h)


def build_program(n_seq=NSEQ, n_tiles=NT, debug=False):
    nc = bass.Bass("TRN2", target_bir_lowering=False)
    k = K(nc)
    NTOK = n_seq * SEQ

    def din(name, shape):
        return nc.dram_tensor(name, list(shape), F32, kind="ExternalInput").ap()

    x_d = din("x", [NTOK, D])
    p_d = din("p", [NTOK, 256])
    w_in_d = din("w_in", [D, 7 * D])
    w_pa_d = din("w_proj_a", [D, D])
    w_pw2_d = din("w_pw2", [D, D])
    w_out_d = din("w_out", [D, D])
    w_pg_d = din("w_ple_gate", [D, D])
    w_ple_d = din("w_ple", [256, D])
    vec_d = din("vecs", [128, NV])
    cmat_d = din("cmat", [128, 288])
    fg_d = din("fg_bc", [128, D])
    wif_d = din("w_if_t", [128, 24 * 8])
    out_d = nc.dram_tensor("out", [NTOK, D], F32, kind="ExternalOutput").ap()
    wsc_d = nc.dram_tensor("wsc", [11, 128, 8 * D], BF16).ap()
    wsc_ple_d = nc.dram_tensor("wsc_ple", [128, 2 * D], BF16).ap()
    dsc_d = nc.dram_tensor("dsc", [8, 128, KB * 128], BF16).ap()

    def sb(name, shape, dt):
        return nc.alloc_sbuf_tensor("s_" + name, list(shape), dt).ap()

    def ps(name, shape, dt):
        return nc.alloc_psum_tensor("p_" + name, list(shape), dt).ap()

    vec = sb("vec", [128, NV], F32)
    cmat = sb("cmat", [128, 288], F32)
    fgbc = sb("fgbc", [128, D], F32)
    wif32 = sb("wif32", [128, 192], F32)
    wifb = sb("wifb", [128, 24, 8], BF16)
    identb = sb("identb", [128, 128], BF16)
    onesb = sb("onesb", [128, 128], BF16)
    ones32 = sb("ones32", [128, 512], F32)
    diag4 = sb("diag4", [128, KA * 8, 128], BF16)
    bd = sb("bd", [128, 3 * 8, 128], BF16)
    negbf = sb("negbf", [128, 1], F32)
    B_const = Buf("const")
    ident32 = cmat[:, 0:128]
    trimask = cmat[:, 128:256]
    bmask = cmat[:, 256:288]

    ds_c = k.dsem("const")
    k.dma(vec, vec_d, (), [B_const], ds_c)
    k.dma(cmat, cmat_d, (), [B_const], ds_c)
    k.dma(fgbc, fg_d, (), [B_const], ds_c)
    k.dma(wif32, wif_d, (), [B_const], ds_c)
    B_c2 = Buf("const2")
    k.cp("dve", wifb.rearrange("p a b -> p (a b)"), wif32, [B_const], [B_c2])
    k.cp("dve", identb, ident32, [B_const], [B_c2])
    k.memset("dve", onesb, 1.0, [B_c2])
    k.memset("dve", ones32, 1.0, [B_c2])
    k.memset("dve", bd.rearrange("p a b -> p (a b)"), 0.0, [B_c2])
    k.ts("dve", negbf[0:4, :], vec[0:4, C_BIF + 1:C_BIF + 2], -1.0, None, ALU.mult, None, [B_const], [B_c2])
    for kk in range(KA):
        for c in range(8):
            col = C_CAW + kk * 8 + c
            k.ts("dve", diag4[:, kk * 8 + c, :], identb, vec[:, col:col + 1], None, ALU.mult, None,
                 [B_const, B_c2], [B_c2])
    for wi, cb in enumerate((C_WQ, C_WK, C_WV)):
        for c in range(8):
            dst = bd[:, wi * 8 + c, :].rearrange("p (g o) -> p g o", o=4)
            for o in range(4):
                col = cb + c * 4 + o
                k.ts("dve", dst[:, :, o], bmask, vec[:, col:col + 1], None, ALU.mult, None,
                     [B_const, B_c2], [B_c2])
    CONSTS = [B_const, B_c2]

    PSA = [ps(f"psa{i}", [128, 512], F32) for i in range(8)]
    PSAb = bufs("psa", 8, True)
    pctr = {"f": 0}
    pinned = set()

    def psf_i():
        while True:
            i = pctr["f"] % 8
            pctr["f"] += 1
            if i not in pinned:
                return i

    def psf():
        i = psf_i()
        return PSA[i], PSAb[i]

    def psb():
        i = psf_i()
        return PSA[i].bitcast(BF16), PSAb[i]

    def pspin():
        i = psf_i()
        pinned.add(i)
        return i

    NSLOT = 3
    wslot = [sb(f"wslot{i}", [128, 8, D], BF16) for i in range(NSLOT)]
    wslotb = bufs("wslot", NSLOT)
    wple = sb("wple", [128, 2, D], BF16)
    wpleb = Buf("wple")
    NST = 2
    stage = [sb(f"stage{i}", [128, 512], F32) for i in range(NST)]
    stageb = bufs("stage", NST)
    x_tok = sb("x_tok", [128, 4, D], F32)
    x_b = bufs("x_tok", 4)
    p_tok = sb("p_tok", [128, 4, 256], F32)
    p_b = Buf("p_tok")
    p_bf = sb("p_bf", [128, 4, 256], BF16)
    p_bfb = Buf("p_bf")
    sb_xbfull = sb("xbfull", [128, 4, D], BF16)
    xbfb = bufs("xbf", 4)
    t1v = sb_xbfull.rearrange("p s f -> p (s f)").rearrange("p (c t) -> p c t", c=8)
    hT = sb("hT", [128, 8, T], BF16)
    hTb = bufs("hT", 8)
    actB = sb("actB", [128, 8, T], BF16)
    actBb = bufs("actB", 8)
    pT = sb("pT", [128, 2, T], BF16)
    pTb = bufs("pT", 2)
    XN = 20608
    arena = sb("arena", [128, XN], BF16)
    UGW = T + KB - 1
    XAW = T + 4
    ug = arena[:, 0:8 * UGW].rearrange("p (c t) -> p c t", c=8)
    o_c = 8 * UGW + (8 * UGW) % 2
    c_sb = arena[:, o_c:o_c + 2 * 8 * T].bitcast(F32).rearrange("p (c t) -> p c t", c=8)
    o_d = o_c + 2 * 8 * T
    dbuf = [arena[:, o_d + i * KB * 128:o_d + (i + 1) * KB * 128].rearrange("p (k j) -> p k j", k=KB)
            for i in range(2)]
    assert o_d + 2 * KB * 128 <= XN
    xaT = arena[:, 0:8 * XAW].rearrange("p (c t) -> p c t", c=8)
    o1 = 4128
    xcT = arena[:, o1:o1 + 8 * T].rearrange("p (c t) -> p c t", c=8)
    o2 = o1 + 8 * T
    qT = arena[:, o2:o2 + 8 * T].rearrange("p (c t) -> p c t", c=8)
    o3 = o2 + 8 * T
    kT = arena[:, o3:o3 + 8 * T].rearrange("p (c t) -> p c t", c=8)
    o4 = o3 + 8 * T
    ha_tok = arena[:, o4:o4 + 4 * D].rearrange("p (s f) -> p s f", s=4)
    assert o4 + 4 * D <= XN
    actA = qT
    t1 = arena[:, o3:o3 + 2 * 8 * T].bitcast(F32).rearrange("p (c t) -> p c t", c=8)
    mergedT = arena[:, 0:8 * T].rearrange("p (c t) -> p c t", c=8)
    ugb = bufs("ug", 8)
    ughb = Buf("ugh")
    c_sbb = bufs("c_sb", 8)
    dbufb = bufs("dbuf", 2)
    xaTb = bufs("xaT", 8)
    xahb = Buf("xah")
    xcTb = bufs("xcT", 8)
    qTb = bufs("qT", 8)
    kTb = bufs("kT", 8)
    hatb = bufs("ha_tok", 4)
    actAb = bufs("actA", 8)
    t1b = bufs("t1", 8)
    mTb = bufs("mergedT", 8)
    out_tok = arena[:, o_c:o_c + 2 * 8 * T].bitcast(F32).rearrange("p (s f) -> p s f", s=4)
    outb = bufs("out_tok", 4)
    PHASE_B = ugb + [ughb] + c_sbb + dbufb
    PHASE_A = xaTb + [xahb] + xcTb + qTb + kTb + hatb
    ugh = sb("ugh", [128, 8, KB - 1], BF16)
    xah = sb("xah", [128, 8, KA - 1], BF16)
    ughsb = Buf("ugh_s")
    xahsb = Buf("xah_s")

    tmpf = [sb(f"tmpf{i}", [128, T], F32) for i in range(3)]
    tmpfb = bufs("tmpf", 3)
    tmpb = [sb(f"tmpb{i}", [128, T], BF16) for i in range(4)]
    tmpbb = bufs("tmpb", 4)
    tctr = {"f": 0, "b": 0}

    def tf():
        i = tctr["f"] % 3
        tctr["f"] += 1
        return tmpf[i], tmpfb[i]

    def tb():
        i = tctr["b"] % 4
        tctr["b"] += 1
        return tmpb[i], tmpbb[i]

    ssq = sb("ssq", [128, 12], F32)
    ssqb = Buf("ssq")
    rr = sb("rr", [128, 12], F32)
    rrb = Buf("rr")
    g_A = sb("g_A", [128, T], F32)
    g_l = sb("g_l", [128, T], F32)
    g_L = sb("g_L", [128, T], F32)
    g_w = sb("g_w", [128, T], F32)
    g_cl = sb("g_cl", [128, T], F32)
    g_small = sb("g_small", [128, 64], F32)
    gb_ = Buf("gates")
    gt = sb("gt", [128, 32], F32)
    gtb = Buf("gt")
    dec = sb("dec", [128, 5, 4], F32)
    decb = Buf("dec")
    Chat = sb("Chat", [128, 8, 257], F32)
    Chatb = bufs("Chat", 8)
    Cb = sb("Cb", [128, 8, 257], BF16)
    Cbb = bufs("Cb", 8)
    NMB = 3
    wk_tok = [sb(f"wk_tok{i}", [128, 256], BF16) for i in range(NMB)]
    wk_tokb = bufs("wk_tok", NMB)
    v_ext = [sb(f"v_ext{i}", [128, 257], BF16) for i in range(NMB)]
    v_extb = bufs("v_ext", NMB)
    S_sb = [sb(f"S_sb{i}", [128, 128], BF16) for i in range(NMB)]
    S_sbb = bufs("S_sb", NMB)
    sm = [sb(f"sm{i}", [128, 16], F32) for i in range(NMB)]
    smb = bufs("sm", NMB)

    for i in range(NMB):
        k.memset("pool", v_ext[i][:, 256:257], 1.0, [v_extb[i]])

    ds_w = [k.dsem(f"w{i}") for i in range(NSLOT)]
    ds_wple = k.dsem("wple")
    ds_wc = [k.dsem(f"wc{i}") for i in range(NSLOT)]
    ds_wplec = k.dsem("wplec")
    ds_st = [k.dsem(f"st{i}") for i in range(NST)]
    ds_sc = [k.dsem(f"sc{i}") for i in range(12)]
    ds_x = [k.dsem(f"x{i}") for i in range(4)]
    ds_p = k.dsem("p")
    ds_db = [k.dsem(f"db{i}") for i in range(2)]
    ds_dsc = [k.dsem(f"dsc{i}") for i in range(8)]
    dscb = bufs("dsc", 8)
    ds_o = [k.dsem(f"o{i}") for i in range(4)]
    scb = bufs("scratch", 12)
    converted = set()
    sctr = {"slot": 0, "st": 0, "cast": 0}

    def w_src(g):
        if g < 7:
            return w_in_d[:, g * D:(g + 1) * D]
        return {G_PA: w_pa_d, G_PW2: w_pw2_d, G_OUT: w_out_d, G_PG: w_pg_d}[g]

    USES_PER_TILE = [G_UB, G_GB, G_ZB, G_XA, G_PW2, G_GBM, G_ZA, G_PA, G_GA, G_OUT, G_PG]
    uses = USES_PER_TILE * (n_seq * n_tiles)
    fills = []
    wst = {"consumed": 0}

    def issue_fill():
        u = len(fills)
        if u >= len(uses):
            return
        g = uses[u]
        i = u % NSLOT
        sl, slb = wslot[i], wslotb[i]
        assert g in converted, g
        if g in converted:
            for q in range(4):
                k.dma(sl[q * 32:(q + 1) * 32].rearrange("p a b -> p (a b)"),
                      wsc_d[g, q * 32:(q + 1) * 32, :], [scb[g]], [slb], ds_w[i])
        else:
            src = w_src(g)
            for kc in range(8):
                k.dma(sl[:, kc, :], src[kc * 128:(kc + 1) * 128, :], (), [slb], ds_wc[i], en="pool")
            k.dma(wsc_d[g], sl.rearrange("p a b -> p (a b)"), [slb], [scb[g]], ds_sc[g])
            converted.add(g)
        fills.append((sl, slb))

    def fill_slot(g):
        u = wst["consumed"]
        assert uses[u] == g, (u, uses[u], g)
        while len(fills) <= u:
            issue_fill()
        wst["consumed"] += 1
        return fills[u]

    def wdone():
        conv_issue(2)
        while len(fills) < min(len(uses), wst["consumed"] + NSLOT) and uses[len(fills)] not in converted:
            issue_fill()

    def fill_wple():
        assert "ple" in converted
        if "ple" in converted:
            k.dma(wple.rearrange("p a b -> p (a b)"), wsc_ple_d, [scb[11]], [wpleb], ds_wple)
        else:
            for kc in range(2):
                k.dma(wple[:, kc, :], w_ple_d[kc * 128:(kc + 1) * 128, :], (), [wpleb], ds_wplec, en="pool")
            k.dma(wsc_ple_d, wple.rearrange("p a b -> p (a b)"), [wpleb], [scb[11]], ds_sc[11])
            converted.add("ple")

    def vcol(base, c):
        return vec[:, base + c:base + c + 1]

    def rms_stats(col0):
        for s in range(4):
            k.act(sb_xbfull[:, s, :], x_tok[:, s, :], AF.Square, [x_b[s]], [xbfb[s], ssqb],
                  accum=ssq[:, col0 + s:col0 + s + 1])
        k.ts("dve", rr[:, col0:col0 + 4], ssq[:, col0:col0 + 4], 1.0 / D, EPS, ALU.mult, ALU.add,
             [ssqb], [rrb])
        k.act(rr[:, col0:col0 + 4], rr[:, col0:col0 + 4], AF.Ln, [rrb], [rrb])
        k.act(rr[:, col0:col0 + 4], rr[:, col0:col0 + 4], AF.Exp, [rrb], [rrb], scale=-0.5)

    dbg = {}
    first_out = {"done": False}

    def tile_body(seq, ti):
        t0 = seq * SEQ + ti * T
        first = ti == 0
        for s in range(4):
            k.dma(x_tok[:, s, :], x_d[t0 + s * 128:t0 + (s + 1) * 128, :], (), [x_b[s]], ds_x[s])
        k.dma(p_tok, p_d[t0:t0 + T, :].rearrange("(s p) f -> p s f", p=128), (), [p_b], ds_p)

        rms_stats(0)
        xbt = sb_xbfull
        for s in range(4):
            k.ts("dve", xbt[:, s, :], x_tok[:, s, :], rr[:, s:s + 1], None, ALU.mult, None,
                 [x_b[s], rrb], [xbfb[s]])
        for c in range(8):
            pt, ptb = psb()
            for s in range(4):
                k.tr(pt[:, s * 128:(s + 1) * 128], xbt[:, s, c * 128:(c + 1) * 128], identb,
                     [xbfb[s]] + CONSTS, [ptb])
            k.act(hT[:, c, :], pt[:, 0:T], AF.Copy, [ptb] + CONSTS, [hTb[c]], scale=vcol(C_NG, c))

        chk("hT")
        k.handoff(PHASE_A + actAb + t1b + mTb + outb, PHASE_B)
        if first:
            k.memset("pool", ugh.rearrange("p a b -> p (a b)"), 0.0, [ughsb])
        k.cp("pool", ug[:, :, 0:KB - 1], ugh, [ughsb], [ughb])
        wu, wub = fill_slot(G_UB)
        wg, wgb = fill_slot(G_GB)
        for m in range(8):
            pu, pub = psf()
            pg, pgb = psf()
            for kc in range(8):
                k.mm(pu, wu[:, kc, m * 128:(m + 1) * 128], hT[:, kc, :], kc == 0, kc == 7,
                     [wub, hTb[kc]], [pub])
            for kc in range(8):
                k.mm(pg, wg[:, kc, m * 128:(m + 1) * 128], hT[:, kc, :], kc == 0, kc == 7,
                     [wgb, hTb[kc]], [pgb])
            sg, sgb = tf()
            k.act(sg, pg, AF.Sigmoid, [pgb], [sgb])
            k.tt("dve", ug[:, m, KB - 1:], pu, sg, ALU.mult, [pub, sgb], [ugb[m]])
        wdone()
        k.cp("pool", ugh, ug[:, :, T:T + KB - 1], ugb, [ughsb])

        chk("ug")
        wz, wzb = fill_slot(G_ZB)
        pi0, pi1 = pspin(), pspin()
        psum_s, psum_sb = PSA[pi0], PSAb[pi0]
        psum_q, psum_qb = PSA[pi1], PSAb[pi1]
        for m in range(8):
            db, dbb = dbuf[m % 2], dbufb[m % 2]
            if ("diag", m) in converted:
                k.dma(db.rearrange("p a b -> p (a b)"), dsc_d[m], [dscb[m]], [dbb], ds_db[m % 2])
            else:
                for kk in range(KB):
                    col = C_CBW + m * KB + kk
                    if kk % 2 == 0:
                        k.ts("dve", db[:, kk, :], identb, vec[:, col:col + 1], None, ALU.mult, None,
                             CONSTS, [dbb])
                    else:
                        k.act(db[:, kk, :], identb, AF.Copy, CONSTS, [dbb], scale=vec[:, col:col + 1])
                k.dma(dsc_d[m], db.rearrange("p a b -> p (a b)"), [dbb], [dscb[m]], ds_dsc[m])
                converted.add(("diag", m))
            pc, pcb = psf()
            for kk in range(KB):
                k.mm(pc, db[:, kk, :], ug[:, m, kk:kk + T], kk == 0, kk == KB - 1,
                     [dbb, ugb[m], ughb], [pcb])
            k.act(c_sb[:, m, :], pc, AF.Identity, [pcb] + CONSTS, [c_sbb[m]], bias=vcol(C_CBB, m))
            cb_, cbb_ = tb()
            k.cp("dve", cb_, c_sb[:, m, :], [c_sbb[m]], [cbb_])
            cq, cqb = tb()
            k.act(cq, c_sb[:, m, :], AF.Square, [c_sbb[m]], [cqb])
            k.mm(psum_s, onesb, cb_, m == 0, m == 7, [cbb_] + CONSTS, [psum_sb])
            k.mm(psum_q, onesb, cq, m == 0, m == 7, [cqb] + CONSTS, [psum_qb])
        chk("c_sb")
        mean, meanb = g_w, Buf("ln_mean")
        msq, msqb = g_l, Buf("ln_msq")
        rstd, rstdb = g_cl, Buf("ln_rstd")
        k.handoff([gb_], [meanb, msqb, rstdb])
        pinned.discard(pi0)
        pinned.discard(pi1)
        k.act(mean, psum_s, AF.Copy, [psum_sb], [meanb], scale=1.0 / D)
        k.tt("dve", msq, mean, mean, ALU.mult, [meanb], [msqb])
        k.stt(rstd, psum_q, 1.0 / D, msq, ALU.mult, ALU.subtract, [psum_qb, msqb], [rstdb])
        k.ts("dve", rstd, rstd, EPS, None, ALU.add, None, [rstdb], [rstdb])
        k.act(rstd, rstd, AF.Ln, [rstdb], [rstdb])
        k.act(rstd, rstd, AF.Exp, [rstdb], [rstdb], scale=-0.5)
        LNB_ = [meanb, msqb, rstdb]
        for m in range(8):
            pz, pzb = psf()
            for kc in range(8):
                k.mm(pz, wz[:, kc, m * 128:(m + 1) * 128], hT[:, kc, :], kc == 0, kc == 7,
                     [wzb, hTb[kc]], [pzb])
            sz, szb = tb()
            k.act(sz, pz, AF.Silu, [pzb], [szb])
            cn, cnb = tf()
            k.tt("dve", cn, c_sb[:, m, :], mean, ALU.subtract, [c_sbb[m], meanb], [cnb])
            k.tt("dve", cn, cn, rstd, ALU.mult, [cnb, rstdb], [cnb])
            sc, scb_ = tb()
            k.act(sc, cn, AF.Silu, [cnb] + CONSTS, [scb_], bias=vcol(C_LNB, m), scale=vcol(C_LNG, m))
            k.tt("dve", actB[:, m, :], sc, sz, ALU.mult, [scb_, szb], [actBb[m]])

        wdone()
        k.handoff(LNB_, [gb_])
        chk("actB")
        k.handoff(PHASE_B, PHASE_A)
        if first:
            k.memset("pool", xah.rearrange("p a b -> p (a b)"), 0.0, [xahsb])
        k.cp("pool", xaT[:, :, 1:4], xah, [xahsb], [xahb])
        wx, wxb = fill_slot(G_XA)
        for m in range(8):
            px, pxb = psf()
            for kc in range(8):
                k.mm(px, wx[:, kc, m * 128:(m + 1) * 128], hT[:, kc, :], kc == 0, kc == 7,
                     [wxb, hTb[kc]], [pxb])
            k.cp("act", xaT[:, m, 4:], px, [pxb], [xaTb[m]])
        wdone()
        k.cp("pool", xah, xaT[:, :, T + 1:T + 4], xaTb, [xahsb])
        pi0, pi1 = pspin(), pspin()
        pig, pigb = PSA[pi0], PSAb[pi0]
        pfg, pfgb = PSA[pi1], PSAb[pi1]
        for m in range(8):
            pc, pcb = psf()
            for kk in range(KA):
                k.mm(pc, diag4[:, kk * 8 + m, :], xaT[:, m, 1 + kk:1 + kk + T], kk == 0, kk == KA - 1,
                     [xaTb[m], xahb] + CONSTS, [pcb])
            k.act(xcT[:, m, :], pc, AF.Silu, [pcb] + CONSTS, [xcTb[m]], bias=vcol(C_CAB, m))
        for m in range(8):
            pq, pqb = psf()
            k.mm(pq, bd[:, 0 * 8 + m, :], xcT[:, m, :], True, True, [xcTb[m]] + CONSTS, [pqb])
            k.cp("act", qT[:, m, :], pq, [pqb], [qTb[m]])
            pk, pkb = psf()
            k.mm(pk, bd[:, 1 * 8 + m, :], xcT[:, m, :], True, True, [xcTb[m]] + CONSTS, [pkb])
            k.cp("dve", kT[:, m, :], pk, [pkb], [kTb[m]])
            pv, pvb = psf()
            k.mm(pv, bd[:, 2 * 8 + m, :], xaT[:, m, 4:], True, True, [xaTb[m]] + CONSTS, [pvb])
            vt, vtb = tb()
            k.cp("act", vt, pv, [pvb], [vtb])
            for gi, (pp, ppb) in enumerate(((pig, pigb), (pfg, pfgb))):
                lo = gi * 4
                k.mm(pp[0:4, :], wifb[:, m, lo:lo + 4], qT[:, m, :], m == 0, False,
                     [qTb[m]] + CONSTS, [ppb])
                k.mm(pp[0:4, :], wifb[:, 8 + m, lo:lo + 4], kT[:, m, :], False, False,
                     [kTb[m]] + CONSTS, [ppb])
                k.mm(pp[0:4, :], wifb[:, 16 + m, lo:lo + 4], vt, False, m == 7,
                     [vtb] + CONSTS, [ppb])

        chk("qk")
        pinned.discard(pi0)
        pinned.discard(pi1)
        G = [gb_]
        A_ = g_A[0:4, :]
        l_ = g_l[0:4, :]
        L_ = g_L[0:4, :]
        cm = g_small[0:4, 0:4]
        mus = g_small[0:4, 4:9]
        negmu = g_small[0:4, 9:14]
        dcy = g_small[0:4, 14:18]
        Lc = g_small[0:4, 18:19]
        Dexp = g_small[0:4, 20:36]
        if first:
            k.memset("dve", g_small[0:4, :], 0.0, G)
            k.memset("dve", dec.rearrange("p a b -> p (a b)"), 1.0, [decb])
        else:
            k.cp("dve", dec[:, 0, :], dec[:, 4, :], [decb], [decb])
        k.act(A_, pig[0:4, :], AF.Identity, [pigb] + CONSTS, G, bias=vec[0:4, C_BIF:C_BIF + 1])
        k.act(l_, pfg[0:4, :], AF.Exp, [pfgb] + CONSTS, G, bias=negbf[0:4, :], scale=-1.0)
        k.act(l_, l_, AF.Ln, G, G, bias=1.0)
        k.iss("dve", lambda h: h.tensor_tensor_scan(L_, ones32[0:4, 0:T], l_, Lc, ALU.mult, ALU.add),
              G + CONSTS, G)
        k.cp("dve", Lc, L_[:, T - 1:T], G, G)
        k.tt("dve", A_, A_, L_, ALU.add, G, G)
        k.iss("dve", lambda h: h.tensor_reduce(cm, A_.rearrange("p (c t) -> p c t", c=4), AX.X, ALU.max),
              G, G)
        if not first:
            k.cp("dve", mus[:, 0:1], mus[:, 4:5], G, G)
        k.iss("dve", lambda h: h.tensor_tensor_scan(mus[:, 1:5], cm, cm, mus[:, 0:1], ALU.max, ALU.max),
              G, G)
        k.ts("dve", negmu, mus, -1.0, None, ALU.mult, None, G, G)
        k.tt("dve", dcy, mus[:, 0:4], mus[:, 1:5], ALU.subtract, G, G)
        k.act(dcy, dcy, AF.Exp, G, G)
        negmu16 = g_small[0:4, 36:41]
        k.ts("dve", negmu16, negmu, -math.log(16.0), None, ALU.add, None, G, G)
        w_ = g_w[0:4, :]
        cl_ = g_cl[0:4, :]
        for c in range(4):
            k.act(w_[:, c * 128:(c + 1) * 128], A_[:, c * 128:(c + 1) * 128], AF.Exp, G, G,
                  bias=negmu16[:, c:c + 1])
            k.act(cl_[:, c * 128:(c + 1) * 128], L_[:, c * 128:(c + 1) * 128], AF.Exp, G, G,
                  bias=negmu[:, c:c + 1])
            k.ts("dve", Dexp[:, c * 4:(c + 1) * 4], ident32[0:4, 0:4], dcy[:, c:c + 1], None, ALU.mult, None,
                 G + CONSTS, G)
        pgt, pgtb = psf()
        for c in range(4):
            k.mm(pgt[:, c * 8:c * 8 + 4], w_[:, c * 128:(c + 1) * 128], ident32[0:4, 0:4], True, True,
                 G + CONSTS, [pgtb])
            k.mm(pgt[:, c * 8 + 4:c * 8 + 8], cl_[:, c * 128:(c + 1) * 128], ident32[0:4, 0:4], True, True,
                 G + CONSTS, [pgtb])
        k.mm(pgt[:, 32:48], ones32[0:4, 0:128], Dexp, True, True, G + CONSTS, [pgtb])
        k.cp("dve", gt, pgt[:, 0:32], [pgtb], [gtb])
        k.cp("dve", dec[:, 1:5, :].rearrange("p a b -> p (a b)"), pgt[:, 32:48], [pgtb], [decb])

        chk("gates")
        if first:
            k.memset("pool", Chat.rearrange("p a b -> p (a b)"), 0.0, Chatb)
            k.memset("pool", Cb.rearrange("p a b -> p (a b)"), 0.0, Cbb)
        chk("m0a")
        steps = [(c, h) for c in range(4) for h in range(H)]
        ctx = {}

        def stage_a(i):
            c, h = steps[i]
            j = i % NMB
            tsl = slice(c * 128, (c + 1) * 128)
            wcol = gt[:, c * 8 + h:c * 8 + h + 1]
            pkv, pkvb = psf()
            for dc in range(2):
                m = 2 * h + dc
                k.mm(pkv[:, dc * 128:(dc + 1) * 128], xcT[:, m, tsl], bd[:, 8 + m, :], True, True,
                     [xcTb[m]] + CONSTS, [pkvb])
                k.mm(pkv[:, 256 + dc * 128:256 + (dc + 1) * 128],
                     xaT[:, m, 4 + c * 128:4 + (c + 1) * 128], bd[:, 16 + m, :], True, True,
                     [xaTb[m]] + CONSTS, [pkvb])
            k.act(wk_tok[j], pkv[:, 0:256], AF.Copy, [pkvb, gtb], [wk_tokb[j]], scale=wcol)
            k.cp("act", v_ext[j][:, 0:256], pkv[:, 256:512], [pkvb], [v_extb[j]])
            pS, pSb = psf()
            for dc in range(2):
                m = 2 * h + dc
                k.mm(pS[:, 0:128], kT[:, m, tsl], qT[:, m, tsl], dc == 0, dc == 1,
                     [kTb[m], qTb[m]], [pSb])
            k.stt(S_sb[j], pS[:, 0:128], wcol, trimask, ALU.mult, ALU.mult, [pSb, gtb] + CONSTS, [S_sbb[j]])

        def stage_b(i):
            c, h = steps[i]
            j = i % NMB
            tsl = slice(c * 128, (c + 1) * 128)
            clcol = gt[:, c * 8 + 4 + h:c * 8 + 4 + h + 1]
            pidx = pspin()
            pin, pinb = PSA[pidx], PSAb[pidx]
            ctx[i] = (pin, pinb, pidx)
            k.mm(pin[:, 0:257], S_sb[j], v_ext[j], True, False, [S_sbb[j], v_extb[j]], [pinb])
            for dc in range(2):
                m = 2 * h + dc
                k.mm(pin[:, 0:257], qT[:, m, tsl], Cb[:, m, :], False, dc == 1,
                     [qTb[m], Cbb[m]], [pinb])
            for dc in range(2):
                m = 2 * h + dc
                pu_, pub_ = psf()
                k.mm(pu_[:, 0:257], wk_tok[j][:, dc * 128:(dc + 1) * 128], v_ext[j], True, True,
                     [wk_tokb[j], v_extb[j]], [pub_])
                k.stt(Chat[:, m, :], Chat[:, m, :], dec[:, c, h:h + 1], pu_[:, 0:257], ALU.mult, ALU.add,
                      [Chatb[m], decb, pub_], [Chatb[m]])
                k.act(Cb[:, m, :], Chat[:, m, :], AF.Copy, [Chatb[m], decb], [Cbb[m]],
                      scale=dec[:, c + 1, h:h + 1])
            s_ = sm[j]
            k.act(s_[:, 0:1], pin[:, 256:257], AF.Abs, [pinb], [smb[j]])
            k.iss("dve", lambda hd, o=s_[:, 2:8], i_=pin[:, 0:256]: hd.bn_stats(o, i_), [pinb], [smb[j]])
            k.iss("dve", lambda hd, o=s_[:, 8:10], i_=s_[:, 2:8]: hd.bn_aggr(o, i_), [smb[j]], [smb[j]])
            k.ts("dve", s_[:, 0:1], s_[:, 0:1], clcol, None, ALU.max, None, [smb[j], gtb], [smb[j]])
            k.iss("dve", lambda hd, o=s_[:, 1:2], i_=s_[:, 0:1]: hd.reciprocal(o, i_), [smb[j]], [smb[j]])
            k.tt("dve", s_[:, 10:11], s_[:, 1:2], s_[:, 1:2], ALU.mult, [smb[j]], [smb[j]])
            k.ts("dve", s_[:, 10:11], s_[:, 10:11], s_[:, 9:10], EPS, ALU.mult, ALU.add, [smb[j]], [smb[j]])
            k.act(s_[:, 10:11], s_[:, 10:11], AF.Ln, [smb[j]], [smb[j]])
            k.act(s_[:, 10:11], s_[:, 10:11], AF.Exp, [smb[j]], [smb[j]], scale=-0.5)

        def stage_c(i):
            c, h = steps[i]
            j = i % NMB
            pin, pinb, pidx = ctx[i]
            s_ = sm[j]
            k.tt("dve", s_[:, 11:12], s_[:, 10:11], s_[:, 1:2], ALU.mult, [smb[j]], [smb[j]])
            k.ts("dve", ha_tok[:, c, h * DH:(h + 1) * DH], pin[:, 0:256], s_[:, 8:9], s_[:, 11:12],
                 ALU.subtract, ALU.mult, [pinb, smb[j]], [hatb[c]])
            pinned.discard(pidx)

        wpw, wpwb = fill_slot(G_PW2)
        wgm, wgmb = fill_slot(G_GBM)

        def filler(m):
            pa, pab = psf()
            pg, pgb = psf()
            for kc in range(8):
                k.mm(pa, wpw[:, kc, m * 128:(m + 1) * 128], actB[:, kc, :], kc == 0, kc == 7,
                     [wpwb, actBb[kc]], [pab])
            for kc in range(8):
                k.mm(pg, wgm[:, kc, m * 128:(m + 1) * 128], hT[:, kc, :], kc == 0, kc == 7,
                     [wgmb, hTb[kc]], [pgb])
            sg, sgb = tf()
            k.act(sg, pg, AF.Sigmoid, [pgb], [sgb])
            k.stt(t1v[:, m, :], pa, vcol(C_BPW, m), sg, ALU.add, ALU.mult, [pab, sgb] + CONSTS, [xbfb[m // 2]])

        stage_a(0)
        for i in range(16):
            if i + 1 < 16:
                stage_a(i + 1)
            stage_b(i)
            if i >= 1:
                stage_c(i - 1)
            if i % 2 == 1:
                filler(i // 2)
        stage_c(15)
        wdone()

        chk("ha")
        k.handoff(qTb, actAb)
        wza, wzab = fill_slot(G_ZA)
        for m in range(8):
            pt, ptb = psb()
            for s in range(4):
                k.tr(pt[:, s * 128:(s + 1) * 128], ha_tok[:, s, m * 128:(m + 1) * 128], identb,
                     [hatb[s]] + CONSTS, [ptb])
            pz, pzb = psf()
            for kc in range(8):
                k.mm(pz, wza[:, kc, m * 128:(m + 1) * 128], hT[:, kc, :], kc == 0, kc == 7,
                     [wzab, hTb[kc]], [pzb])
            sz, szb = tb()
            k.act(sz, pz, AF.Silu, [pzb], [szb])
            sk, skb = tf()
            k.ts("dve", sk, xcT[:, m, :], vcol(C_SKP, m), None, ALU.mult, None, [xcTb[m]] + CONSTS, [skb])
            k.stt(sk, pt[:, 0:T], vcol(C_MHG, m), sk, ALU.mult, ALU.add, [ptb, skb] + CONSTS, [skb])
            k.tt("dve", actA[:, m, :], sk, sz, ALU.mult, [skb, szb], [actAb[m]])

        wdone()
        chk("actA")
        k.handoff(xaTb + [xahb], mTb)
        wpa, wpab = fill_slot(G_PA)
        wga, wgab = fill_slot(G_GA)
        for m in range(8):
            pa, pab = psf()
            pg, pgb = psf()
            for kc in range(8):
                k.mm(pa, wpa[:, kc, m * 128:(m + 1) * 128], actA[:, kc, :], kc == 0, kc == 7,
                     [wpab, actAb[kc]], [pab])
            for kc in range(8):
                k.mm(pg, wga[:, kc, m * 128:(m + 1) * 128], hT[:, kc, :], kc == 0, kc == 7,
                     [wgab, hTb[kc]], [pgb])
            sg, sgb = tf()
            k.act(sg, pg, AF.Sigmoid, [pgb], [sgb])
            k.tt("dve", sg, pa, sg, ALU.mult, [pab, sgb], [sgb])
            k.tt("dve", mergedT[:, m, :], sg, t1v[:, m, :], ALU.add, [sgb, xbfb[m // 2]], [mTb[m]])

        wdone()
        chk("merged")
        wo, wob = fill_slot(G_OUT)
        for s in range(4):
            for hf in range(2):
                po, pob = psf()
                for kc in range(8):
                    k.mm(po, mergedT[:, kc, s * 128:(s + 1) * 128], wo[:, kc, hf * 512:(hf + 1) * 512],
                         kc == 0, kc == 7, [mTb[kc], wob], [pob])
                k.tt("dve", x_tok[:, s, hf * 512:(hf + 1) * 512], x_tok[:, s, hf * 512:(hf + 1) * 512], po,
                     ALU.add, [x_b[s], pob], [x_b[s]])

        wdone()
        chk("x1")
        rms_stats(4)
        for s in range(4):
            k.ts("dve", xbt[:, s, :], x_tok[:, s, :], rr[:, 4 + s:5 + s], None, ALU.mult, None,
                 [x_b[s], rrb], [xbfb[s]])
        for c in range(8):
            pt, ptb = psb()
            for s in range(4):
                k.tr(pt[:, s * 128:(s + 1) * 128], xbt[:, s, c * 128:(c + 1) * 128], identb,
                     [xbfb[s]] + CONSTS, [ptb])
            k.act(hT[:, c, :], pt[:, 0:T], AF.Copy, [ptb] + CONSTS, [hTb[c]], scale=vcol(C_PNG, c))
        k.cp("pool", p_bf.rearrange("p a b -> p (a b)"), p_tok.rearrange("p a b -> p (a b)"), [p_b], [p_bfb])
        for pc_ in range(2):
            pt, ptb = psb()
            for s in range(4):
                k.tr(pt[:, s * 128:(s + 1) * 128], p_bf[:, s, pc_ * 128:(pc_ + 1) * 128], identb,
                     [p_bfb] + CONSTS, [ptb])
            k.cp("act", pT[:, pc_, :], pt[:, 0:T], [ptb], [pTb[pc_]])
        wpg, wpgb = fill_slot(G_PG)
        for s in range(4):
            for hf in range(2):
                pg, pgb = psf()
                pp, ppb = psf()
                for kc in range(8):
                    k.mm(pg, hT[:, kc, s * 128:(s + 1) * 128], wpg[:, kc, hf * 512:(hf + 1) * 512],
                         kc == 0, kc == 7, [hTb[kc], wpgb], [pgb])
                for kc in range(2):
                    k.mm(pp, pT[:, kc, s * 128:(s + 1) * 128], wple[:, kc, hf * 512:(hf + 1) * 512],
                         kc == 0, kc == 1, [pTb[kc], wpleb], [ppb])
                sg, sgb = tf()
                k.act(sg, pg, AF.Sigmoid, [pgb], [sgb])
                k.tt("dve", sg, sg, pp, ALU.mult, [sgb, ppb], [sgb])
                k.tt("dve", x_tok[:, s, hf * 512:(hf + 1) * 512], x_tok[:, s, hf * 512:(hf + 1) * 512], sg,
                     ALU.add, [x_b[s], sgb], [x_b[s]])

        wdone()
        while len(fills) < min(len(uses), wst["consumed"] + NSLOT):
            issue_fill()
        if not (seq == n_seq - 1 and ti == n_tiles - 1):
            fill_wple()
        chk("x2")
        rms_stats(8)
        k.handoff(xcTb + actAb + qTb + kTb, outb)
        for s in range(4):
            k.stt(out_tok[:, s, :], x_tok[:, s, :], rr[:, 8 + s:9 + s], fgbc, ALU.mult, ALU.mult,
                  [x_b[s], rrb] + CONSTS, [outb[s]])
            if STOP != "nostore":
                k.dma(out_d[t0 + s * 128:t0 + (s + 1) * 128, :], out_tok[:, s, :], [outb[s]], (), ds_o[s], en="pool")

    CONV_ORDER = ["ple"] + USES_PER_TILE
    cptr = {"i": 0}

    def conv_issue(n):
        for _ in range(n):
            i = cptr["i"]
            if i >= len(CONV_ORDER):
                return
            g = CONV_ORDER[i]
            cptr["i"] += 1
            gate = []
            if i >= 2:
                pg_ = CONV_ORDER[i - 2]
                gate = [scb[11 if pg_ == "ple" else pg_]]
            if g == "ple":
                for kc in range(2):
                    k.dma(wsc_ple_d[:, kc * D:(kc + 1) * D], w_ple_d[kc * 128:(kc + 1) * 128, :], gate, [scb[11]],
                          ds_sc[11], en="pool")
            else:
                src = w_src(g)
                for kc in range(8):
                    k.dma(wsc_d[g][:, kc * D:(kc + 1) * D], src[kc * 128:(kc + 1) * 128, :], gate, [scb[g]],
                          ds_sc[g], en="pool")
            converted.add(g)

    conv_issue(5)
    fill_wple()
    try:
        for seq in range(n_seq):
            for ti in range(n_tiles):
                tile_body(seq, ti)
    except _Stop:
        pass

    k.emit(ds_o)
    return nc


_NC_CACHE = {}


def _host_consts(inp):
    f = np.float32
    vec = np.zeros((128, NV), f)

    def chunks(v):
        return np.ascontiguousarray(np.asarray(v, f).reshape(8, 128).T)

    vec[:, C_NG:C_NG + 8] = chunks(inp["norm_g"][0])
    vec[:, C_CAB:C_CAB + 8] = chunks(inp["conv_a_b"][0])
    vec[:, C_MHG:C_MHG + 8] = chunks(inp["mh_norm_g"][0])
    vec[:, C_SKP:C_SKP + 8] = chunks(inp["skip_a"][0])
    vec[:, C_CBB:C_CBB + 8] = chunks(inp["conv_b_b"][0])
    vec[:, C_LNG:C_LNG + 8] = chunks(inp["ln_b_g"][0])
    vec[:, C_LNB:C_LNB + 8] = chunks(inp["ln_b_b"][0])
    vec[:, C_BPW:C_BPW + 8] = chunks(inp["b_pw2"][0])
    vec[:, C_PNG:C_PNG + 8] = chunks(inp["ple_norm_g"][0])
    caw = np.asarray(inp["conv_a_w"][0], f)
    for kk in range(KA):
        vec[:, C_CAW + kk * 8:C_CAW + kk * 8 + 8] = chunks(caw[kk])
    cbw = np.asarray(inp["conv_b_w"][0], f)
    for c in range(8):
        vec[:, C_CBW + c * KB:C_CBW + (c + 1) * KB] = cbw[:, c * 128:(c + 1) * 128].T
    for base, nm in ((C_WQ, "wq"), (C_WK, "wk"), (C_WV, "wv")):
        w = np.asarray(inp[nm][0], f)
        for c in range(8):
            vec[:, base + c * 4:base + c * 4 + 4] = w[c * 32:(c + 1) * 32].reshape(128, 4)
    bif = np.asarray(inp["b_if"][0], f)
    vec[0:4, C_BIF] = bif[0:4]
    vec[0:4, C_BIF + 1] = bif[4:8]
    cmat = np.zeros((128, 288), f)
    cmat[:, 0:128] = np.eye(128, dtype=f)
    cmat[:, 128:256] = np.triu(np.ones((128, 128), f))
    cmat[:, 256:288] = (np.arange(128)[:, None] // 4 == np.arange(32)[None, :]).astype(f)
    fg = np.ascontiguousarray(np.broadcast_to(np.asarray(inp["final_g"], f)[None, :], (128, D)))
    wif = np.asarray(inp["w_if"][0], f).reshape(24, 128, 8).transpose(1, 0, 2).reshape(128, 192)
    return vec, cmat, fg, np.ascontiguousarray(wif)


def kernel(**inputs):
    n = 8
    x = np.asarray(inputs["x"], np.float32)
    p = np.asarray(inputs["p"], np.float32)[0]
    vec, cmat, fg, wif = _host_consts(inputs)
    if "nc" not in _NC_CACHE:
        _NC_CACHE["nc"] = build_program()
    nc = _NC_CACHE["nc"]
    shared = {
        "w_in": np.ascontiguousarray(np.asarray(inputs["w_in"], np.float32)[0]),
        "w_proj_a": np.ascontiguousarray(np.asarray(inputs["w_proj_a"], np.float32)[0]),
        "w_pw2": np.ascontiguousarray(np.asarray(inputs["w_pw2"], np.float32)[0]),
        "w_out": np.ascontiguousarray(np.asarray(inputs["w_out"], np.float32)[0]),
        "w_ple_gate": np.ascontiguousarray(np.asarray(inputs["w_ple_gate"], np.float32)[0]),
        "w_ple": np.ascontiguousarray(np.asarray(inputs["w_ple"], np.float32)[0]),
        "vecs": vec, "cmat": cmat, "fg_bc": fg, "w_if_t": wif,
    }
    in_maps = []
    for c in range(n):
        m = dict(shared)
        m["x"] = np.ascontiguousarray(x[2 * c:2 * c + 2].reshape(NSEQ * SEQ, D))
        m["p"] = np.ascontiguousarray(p[2 * c:2 * c + 2].reshape(NSEQ * SEQ, 256))
        in_maps.append(m)
    res = run_bass_kernel_spmd(nc, in_maps, core_ids=list(range(n)))
    out = np.stack([np.asarray(r["out"], np.float32).reshape(NSEQ, SEQ, D) for r in res.results], 0)
    return out.reshape(16, SEQ, D)
```

```python
import math
import numpy as np
import ml_dtypes
import concourse.bass as bass
import concourse.mybir as mybir
from concourse.bass_utils import run_bass_kernel_spmd

F32 = mybir.dt.float32
BF16 = mybir.dt.bfloat16
ALU = mybir.AluOpType
AF = mybir.ActivationFunctionType
AX = mybir.AxisListType

D = 1024
SEQ = 2048
NSEQ = 2
T = 512
NT = SEQ // T
H = 4
DH = 256
EPS = 1e-6
KB = 31
KA = 4

C_NG, C_CAB, C_MHG, C_SKP, C_CBB, C_LNG, C_LNB, C_BPW, C_PNG = 0, 8, 16, 24, 32, 40, 48, 56, 64
C_CAW = 72
C_CBW = 104
C_WQ, C_WK, C_WV = 352, 384, 416
C_BIF = 448
NV = 450

G_XA, G_ZA, G_UB, G_GB, G_ZB, G_GA, G_GBM, G_PA, G_PW2, G_OUT, G_PG = range(11)


STOP = None
PHASE = ["setup"]


class _Stop(Exception):
    pass


def chk(name):
    PHASE[0] = name
    if STOP == name:
        raise _Stop()


class Eng:
    def __init__(self, name):
        self.name = name
        self.ops = []
        self.sem = None
        self.seen = {}


class Op:
    __slots__ = ("eng", "fn", "waits", "needed", "idx", "cnt", "dma", "phase")

    def __init__(self, eng, fn):
        self.eng = eng
        self.fn = fn
        self.waits = []
        self.needed = False
        self.idx = len(eng.ops)
        self.cnt = None
        self.dma = None
        self.phase = PHASE[0]


class DSem:
    def __init__(self, name, sem):
        self.name = name
        self.sem = sem
        self.count = 0


class Buf:
    __slots__ = ("name", "w", "r", "psum")

    def __init__(self, name, psum=False):
        self.name = name
        self.w = None
        self.r = {}
        self.psum = psum


def bufs(name, n, psum=False):
    return [Buf(f"{name}{i}", psum) for i in range(n)]


def tokkey(tok):
    return tok[1].eng.name if tok[0] == "op" else ("d", tok[1].name)


class K:
    def __init__(self, nc):
        self.nc = nc
        self.E = {n: Eng(n) for n in ("pe", "act", "dve", "pool", "sp")}
        for n, e in self.E.items():
            e.sem = nc.alloc_semaphore(name=f"sem_{n}")
        self.dsems = {}

    def dsem(self, name):
        if name not in self.dsems:
            self.dsems[name] = DSem(name, self.nc.alloc_semaphore(name=f"dsem_{name}"))
        return self.dsems[name]

    def _add_wait(self, op, tok, kind):
        if tok[0] == "op":
            src = tok[1]
            if src.eng is op.eng:
                if op.eng.name in ("pe", "sp"):
                    return
            if op.eng.seen.get(src.eng.name, -1) >= src.idx:
                return
            op.eng.seen[src.eng.name] = src.idx
            src.needed = True
            op.waits.append(tok)
        else:
            key = ("d", tok[1].name)
            if op.eng.seen.get(key, 0) >= tok[2]:
                return
            op.eng.seen[key] = tok[2]
            op.waits.append(tok)

    def iss(self, en, fn, rd=(), wr=(), dsem=None):
        eng = self.E[en]
        op = Op(eng, fn)
        for b in rd:
            if b.w is not None:
                self._add_wait(op, b.w, "raw")
            if b.psum:
                for kk, t in b.r.items():
                    if kk != eng.name:
                        self._add_wait(op, t, "rar")
        for b in wr:
            if b.w is not None:
                self._add_wait(op, b.w, "waw")
            for t in b.r.values():
                self._add_wait(op, t, "war")
        if dsem is not None:
            dsem.count += 16
            tok = ("dma", dsem, dsem.count)
            op.dma = dsem
        else:
            tok = ("op", op)
        for b in rd:
            b.r[tokkey(tok)] = tok
        for b in wr:
            b.w = tok
            b.r = {}
        eng.ops.append(op)
        return op

    def handoff(self, old, new):
        toks = {}
        for b in old:
            if b.w is not None:
                toks[("w",) + (tokkey(b.w),)] = b.w
            for k, t in b.r.items():
                kk = ("r", k)
                if kk in toks:
                    a = toks[kk]
                    if a[0] == "op":
                        if t[1].idx > a[1].idx:
                            toks[kk] = t
                    elif t[2] > a[2]:
                        toks[kk] = t
                else:
                    toks[kk] = t
        merged = {}
        for (_, k), t in toks.items():
            if k in merged:
                a = merged[k]
                if a[0] == "op":
                    if t[1].idx > a[1].idx:
                        merged[k] = t
                elif t[2] > a[2]:
                    merged[k] = t
            else:
                merged[k] = t
        for b in new:
            b.w = None
            b.r = dict(merged)

    def mm(self, out, lhsT, rhs, start, stop, rd, wr):
        return self.iss("pe", lambda h: h.matmul(out, lhsT, rhs, start=start, stop=stop), rd, wr)

    def tr(self, out, in_, ident, rd, wr):
        return self.iss("pe", lambda h: h.transpose(out, in_, ident), rd, wr)

    def act(self, out, in_, func, rd, wr, bias=None, scale=None, accum=None):
        kw = {}
        if bias is not None:
            kw["bias"] = bias
        if scale is not None:
            kw["scale"] = scale
        if accum is not None:
            kw["accum_out"] = accum
        return self.iss("act", lambda h: h.activation(out, in_, func, **kw), rd, wr)

    def tt(self, en, out, in0, in1, op, rd, wr):
        return self.iss(en, lambda h: h.tensor_tensor(out, in0, in1, op), rd, wr)

    def ts(self, en, out, in0, s1, s2, op0, op1, rd, wr):
        if s2 is None:
            return self.iss(en, lambda h: h.tensor_scalar(out, in0, s1, None, op0), rd, wr)
        return self.iss(en, lambda h: h.tensor_scalar(out, in0, s1, s2, op0, op1), rd, wr)

    def stt(self, out, in0, scalar, in1, op0, op1, rd, wr):
        return self.iss("dve", lambda h: h.scalar_tensor_tensor(out, in0, scalar, in1, op0, op1), rd, wr)

    def cp(self, en, out, in_, rd, wr):
        if en == "act":
            return self.iss("act", lambda h: h.activation(out, in_, AF.Copy), rd, wr)
        return self.iss(en, lambda h: h.tensor_copy(out, in_), rd, wr)

    def memset(self, en, ap, val, wr):
        return self.iss(en, lambda h: h.memset(ap, val), (), wr)

    def dma(self, out, in_, rd, wr, dsem, en="sp"):
        return self.iss(en, lambda h: h.dma_start(out=out, in_=in_), rd, wr, dsem=dsem)

    def emit(self, final_waits):
        nc = self.nc
        for e in self.E.values():
            c = 0
            for op in e.ops:
                if op.needed:
                    c += 1
                    op.cnt = c

        def run(eng, h, extra=()):
            for op in eng.ops:
                for tok in op.waits:
                    if tok[0] == "op":
                        h.wait_ge(tok[1].eng.sem, tok[1].cnt)
                    else:
                        h.wait_ge(tok[1].sem, tok[2])
                ins = op.fn(h)
                if op.dma is not None:
                    ins.then_inc(op.dma.sem, 16)
                elif op.needed:
                    ins.then_inc(eng.sem, 1)
            for ds in extra:
                h.wait_ge(ds.sem, ds.count)

        E = self.E
        with nc.Block() as block:
            @block.tensor
            def _(h):
                run(E["pe"], h)

            @block.scalar
            def _(h):
                run(E["act"], h)

            @block.vector
            def _(h):
                run(E["dve"], h)

            @block.gpsimd
            def _(h):
                run(E["pool"], h, final_waits)

            @block.sync
            def _(h):
                run(E["sp"], h)


def build_program(n_seq=NSEQ, n_tiles=NT, debug=False):
    nc = bass.Bass("TRN2", target_bir_lowering=False)
    k = K(nc)
    NTOK = n_seq * SEQ

    def din(name, shape):
        return nc.dram_tensor(name, list(shape), F32, kind="ExternalInput").ap()

    x_d = din("x", [NTOK, D])
    p_d = din("p", [NTOK, 256])
    w_in_d = din("w_in", [D, 7 * D])
    w_pa_d = din("w_proj_a", [D, D])
    w_pw2_d = din("w_pw2", [D, D])
    w_out_d = din("w_out", [D, D])
    w_pg_d = din("w_ple_gate", [D, D])
    w_ple_d = din("w_ple", [256, D])
    vec_d = din("vecs", [128, NV])
    cmat_d = din("cmat", [128, 288])
    fg_d = din("fg_bc", [128, D])
    wif_d = din("w_if_t", [128, 24 * 8])
    out_d = nc.dram_tensor("out", [NTOK, D], F32, kind="ExternalOutput").ap()
    wsc_d = nc.dram_tensor("wsc", [11, 128, 8 * D], BF16).ap()
    wsc_ple_d = nc.dram_tensor("wsc_ple", [128, 2 * D], BF16).ap()
    dsc_d = nc.dram_tensor("dsc", [8, 128, KB * 128], BF16).ap()

    def sb(name, shape, dt):
        return nc.alloc_sbuf_tensor("s_" + name, list(shape), dt).ap()

    def ps(name, shape, dt):
        return nc.alloc_psum_tensor("p_" + name, list(shape), dt).ap()

    vec = sb("vec", [128, NV], F32)
    cmat = sb("cmat", [128, 288], F32)
    fgbc = sb("fgbc", [128, D], F32)
    wif32 = sb("wif32", [128, 192], F32)
    wifb = sb("wifb", [128, 24, 8], BF16)
    identb = sb("identb", [128, 128], BF16)
    onesb = sb("onesb", [128, 128], BF16)
    ones32 = sb("ones32", [128, 512], F32)
    diag4 = sb("diag4", [128, KA * 8, 128], BF16)
    bd = sb("bd", [128, 3 * 8, 128], BF16)
    negbf = sb("negbf", [128, 1], F32)
    B_const = Buf("const")
    ident32 = cmat[:, 0:128]
    trimask = cmat[:, 128:256]
    bmask = cmat[:, 256:288]

    ds_c = k.dsem("const")
    k.dma(vec, vec_d, (), [B_const], ds_c)
    k.dma(cmat, cmat_d, (), [B_const], ds_c)
    k.dma(fgbc, fg_d, (), [B_const], ds_c)
    k.dma(wif32, wif_d, (), [B_const], ds_c)
    B_c2 = Buf("const2")
    k.cp("dve", wifb.rearrange("p a b -> p (a b)"), wif32, [B_const], [B_c2])
    k.cp("dve", identb, ident32, [B_const], [B_c2])
    k.memset("dve", onesb, 1.0, [B_c2])
    k.memset("dve", ones32, 1.0, [B_c2])
    k.memset("dve", bd.rearrange("p a b -> p (a b)"), 0.0, [B_c2])
    k.ts("dve", negbf[0:4, :], vec[0:4, C_BIF + 1:C_BIF + 2], -1.0, None, ALU.mult, None, [B_const], [B_c2])
    for kk in range(KA):
        for c in range(8):
            col = C_CAW + kk * 8 + c
            k.ts("dve", diag4[:, kk * 8 + c, :], identb, vec[:, col:col + 1], None, ALU.mult, None,
                 [B_const, B_c2], [B_c2])
    for wi, cb in enumerate((C_WQ, C_WK, C_WV)):
        for c in range(8):
            dst = bd[:, wi * 8 + c, :].rearrange("p (g o) -> p g o", o=4)
            for o in range(4):
                col = cb + c * 4 + o
                k.ts("dve", dst[:, :, o], bmask, vec[:, col:col + 1], None, ALU.mult, None,
                     [B_const, B_c2], [B_c2])
    CONSTS = [B_const, B_c2]

    PSA = [ps(f"psa{i}", [128, 512], F32) for i in range(8)]
    PSAb = bufs("psa", 8, True)
    pctr = {"f": 0}
    pinned = set()

    def psf_i():
        while True:
            i = pctr["f"] % 8
            pctr["f"] += 1
            if i not in pinned:
                return i

    def psf():
        i = psf_i()
        return PSA[i], PSAb[i]

    def psb():
        i = psf_i()
        return PSA[i].bitcast(BF16), PSAb[i]

    def pspin():
        i = psf_i()
        pinned.add(i)
        return i

    NSLOT = 3
    wslot = [sb(f"wslot{i}", [128, 8, D], BF16) for i in range(NSLOT)]
    wslotb = bufs("wslot", NSLOT)
    wple = sb("wple", [128, 2, D], BF16)
    wpleb = Buf("wple")
    NST = 2
    stage = [sb(f"stage{i}", [128, 512], F32) for i in range(NST)]
    stageb = bufs("stage", NST)
    x_tok = sb("x_tok", [128, 4, D], F32)
    x_b = bufs("x_tok", 4)
    p_tok = sb("p_tok", [128, 4, 256], F32)
    p_b = Buf("p_tok")
    p_bf = sb("p_bf", [128, 4, 256], BF16)
    p_bfb = Buf("p_bf")
    sb_xbfull = sb("xbfull", [128, 4, D], BF16)
    xbfb = bufs("xbf", 4)
    t1v = sb_xbfull.rearrange("p s f -> p (s f)").rearrange("p (c t) -> p c t", c=8)
    hT = sb("hT", [128, 8, T], BF16)
    hTb = bufs("hT", 8)
    actB = sb("actB", [128, 8, T], BF16)
    actBb = bufs("actB", 8)
    pT = sb("pT", [128, 2, T], BF16)
    pTb = bufs("pT", 2)
    XN = 20608
    arena = sb("arena", [128, XN], BF16)
    UGW = T + KB - 1
    XAW = T + 4
    ug = arena[:, 0:8 * UGW].rearrange("p (c t) -> p c t", c=8)
    o_c = 8 * UGW + (8 * UGW) % 2
    c_sb = arena[:, o_c:o_c + 2 * 8 * T].bitcast(F32).rearrange("p (c t) -> p c t", c=8)
    o_d = o_c + 2 * 8 * T
    dbuf = [arena[:, o_d + i * KB * 128:o_d + (i + 1) * KB * 128].rearrange("p (k j) -> p k j", k=KB)
            for i in range(2)]
    assert o_d + 2 * KB * 128 <= XN
    xaT = arena[:, 0:8 * XAW].rearrange("p (c t) -> p c t", c=8)
    o1 = 4128
    xcT = arena[:, o1:o1 + 8 * T].rearrange("p (c t) -> p c t", c=8)
    o2 = o1 + 8 * T
    qT = arena[:, o2:o2 + 8 * T].rearrange("p (c t) -> p c t", c=8)
    o3 = o2 + 8 * T
    kT = arena[:, o3:o3 + 8 * T].rearrange("p (c t) -> p c t", c=8)
    o4 = o3 + 8 * T
    ha_tok = arena[:, o4:o4 + 4 * D].rearrange("p (s f) -> p s f", s=4)
    assert o4 + 4 * D <= XN
    actA = qT
    t1 = arena[:, o3:o3 + 2 * 8 * T].bitcast(F32).rearrange("p (c t) -> p c t", c=8)
    mergedT = arena[:, 0:8 * T].rearrange("p (c t) -> p c t", c=8)
    ugb = bufs("ug", 8)
    ughb = Buf("ugh")
    c_sbb = bufs("c_sb", 8)
    dbufb = bufs("dbuf", 2)
    xaTb = bufs("xaT", 8)
    xahb = Buf("xah")
    xcTb = bufs("xcT", 8)
    qTb = bufs("qT", 8)
    kTb = bufs("kT", 8)
    hatb = bufs("ha_tok", 4)
    actAb = bufs("actA", 8)
    t1b = bufs("t1", 8)
    mTb = bufs("mergedT", 8)
    out_tok = arena[:, o_c:o_c + 2 * 8 * T].bitcast(F32).rearrange("p (s f) -> p s f", s=4)
    outb = bufs("out_tok", 4)
    PHASE_B = ugb + [ughb] + c_sbb + dbufb
    PHASE_A = xaTb + [xahb] + xcTb + qTb + kTb + hatb
    ugh = sb("ugh", [128, 8, KB - 1], BF16)
    xah = sb("xah", [128, 8, KA - 1], BF16)
    ughsb = Buf("ugh_s")
    xahsb = Buf("xah_s")

    tmpf = [sb(f"tmpf{i}", [128, T], F32) for i in range(3)]
    tmpfb = bufs("tmpf", 3)
    tmpb = [sb(f"tmpb{i}", [128, T], BF16) for i in range(4)]
    tmpbb = bufs("tmpb", 4)
    tctr = {"f": 0, "b": 0}

    def tf():
        i = tctr["f"] % 3
        tctr["f"] += 1
        return tmpf[i], tmpfb[i]

    def tb():
        i = tctr["b"] % 4
        tctr["b"] += 1
        return tmpb[i], tmpbb[i]

    ssq = sb("ssq", [128, 12], F32)
    ssqb = Buf("ssq")
    rr = sb("rr", [128, 12], F32)
    rrb = Buf("rr")
    g_A = sb("g_A", [128, T], F32)
    g_l = sb("g_l", [128, T], F32)
    g_L = sb("g_L", [128, T], F32)
    g_w = sb("g_w", [128, T], F32)
    g_cl = sb("g_cl", [128, T], F32)
    g_small = sb("g_small", [128, 64], F32)
    gb_ = Buf("gates")
    gt = sb("gt", [128, 32], F32)
    gtb = Buf("gt")
    dec = sb("dec", [128, 5, 4], F32)
    decb = Buf("dec")
    Chat = sb("Chat", [128, 8, 257], F32)
    Chatb = bufs("Chat", 8)
    Cb = sb("Cb", [128, 8, 257], BF16)
    Cbb = bufs("Cb", 8)
    NMB = 3
    wk_tok = [sb(f"wk_tok{i}", [128, 256], BF16) for i in range(NMB)]
    wk_tokb = bufs("wk_tok", NMB)
    v_ext = [sb(f"v_ext{i}", [128, 257], BF16) for i in range(NMB)]
    v_extb = bufs("v_ext", NMB)
    S_sb = [sb(f"S_sb{i}", [128, 128], BF16) for i in range(NMB)]
    S_sbb = bufs("S_sb", NMB)
    sm = [sb(f"sm{i}", [128, 16], F32) for i in range(NMB)]
    smb = bufs("sm", NMB)

    for i in range(NMB):
        k.memset("pool", v_ext[i][:, 256:257], 1.0, [v_extb[i]])

    ds_w = [k.dsem(f"w{i}") for i in range(NSLOT)]
    ds_wple = k.dsem("wple")
    ds_wc = [k.dsem(f"wc{i}") for i in range(NSLOT)]
    ds_wplec = k.dsem("wplec")
    ds_st = [k.dsem(f"st{i}") for i in range(NST)]
    ds_sc = [k.dsem(f"sc{i}") for i in range(12)]
    ds_x = [k.dsem(f"x{i}") for i in range(4)]
    ds_p = k.dsem("p")
    ds_db = [k.dsem(f"db{i}") for i in range(2)]
    ds_dsc = [k.dsem(f"dsc{i}") for i in range(8)]
    dscb = bufs("dsc", 8)
    ds_o = [k.dsem(f"o{i}") for i in range(4)]
    scb = bufs("scratch", 12)
    converted = set()
    sctr = {"slot": 0, "st": 0, "cast": 0}

    def w_src(g):
        if g < 7:
            return w_in_d[:, g * D:(g + 1) * D]
        return {G_PA: w_pa_d, G_PW2: w_pw2_d, G_OUT: w_out_d, G_PG: w_pg_d}[g]

    USES_PER_TILE = [G_UB, G_GB, G_ZB, G_XA, G_PW2, G_GBM, G_ZA, G_PA, G_GA, G_OUT, G_PG]
    uses = USES_PER_TILE * (n_seq * n_tiles)
    fills = []
    wst = {"consumed": 0}

    def issue_fill():
        u = len(fills)
        if u >= len(uses):
            return
        g = uses[u]
        i = u % NSLOT
        sl, slb = wslot[i], wslotb[i]
        assert g in converted, g
        if g in converted:
            for q in range(4):
                k.dma(sl[q * 32:(q + 1) * 32].rearrange("p a b -> p (a b)"),
                      wsc_d[g, q * 32:(q + 1) * 32, :], [scb[g]], [slb], ds_w[i])
        else:
            src = w_src(g)
            for kc in range(8):
                k.dma(sl[:, kc, :], src[kc * 128:(kc + 1) * 128, :], (), [slb], ds_wc[i], en="pool")
            k.dma(wsc_d[g], sl.rearrange("p a b -> p (a b)"), [slb], [scb[g]], ds_sc[g])
            converted.add(g)
        fills.append((sl, slb))

    def fill_slot(g):
        u = wst["consumed"]
        assert uses[u] == g, (u, uses[u], g)
        while len(fills) <= u:
            issue_fill()
        wst["consumed"] += 1
        return fills[u]

    def wdone():
        conv_issue(2)
        while len(fills) < min(len(uses), wst["consumed"] + NSLOT) and uses[len(fills)] not in converted:
            issue_fill()

    def fill_wple():
        assert "ple" in converted
        if "ple" in converted:
            k.dma(wple.rearrange("p a b -> p (a b)"), wsc_ple_d, [scb[11]], [wpleb], ds_wple)
        else:
            for kc in range(2):
                k.dma(wple[:, kc, :], w_ple_d[kc * 128:(kc + 1) * 128, :], (), [wpleb], ds_wplec, en="pool")
            k.dma(wsc_ple_d, wple.rearrange("p a b -> p (a b)"), [wpleb], [scb[11]], ds_sc[11])
            converted.add("ple")

    def vcol(base, c):
        return vec[:, base + c:base + c + 1]

    def rms_stats(col0):
        for s in range(4):
            k.act(sb_xbfull[:, s, :], x_tok[:, s, :], AF.Square, [x_b[s]], [xbfb[s], ssqb],
                  accum=ssq[:, col0 + s:col0 + s + 1])
        k.ts("dve", rr[:, col0:col0 + 4], ssq[:, col0:col0 + 4], 1.0 / D, EPS, ALU.mult, ALU.add,
             [ssqb], [rrb])
        k.act(rr[:, col0:col0 + 4], rr[:, col0:col0 + 4], AF.Ln, [rrb], [rrb])
        k.act(rr[:, col0:col0 + 4], rr[:, col0:col0 + 4], AF.Exp, [rrb], [rrb], scale=-0.5)

    dbg = {}
    first_out = {"done": False}

    def tile_body(seq, ti):
        t0 = seq * SEQ + ti * T
        first = ti == 0
        for s in range(4):
            k.dma(x_tok[:, s, :], x_d[t0 + s * 128:t0 + (s + 1) * 128, :], (), [x_b[s]], ds_x[s])
        k.dma(p_tok, p_d[t0:t0 + T, :].rearrange("(s p) f -> p s f", p=128), (), [p_b], ds_p)

        rms_stats(0)
        xbt = sb_xbfull
        for s in range(4):
            k.ts("dve", xbt[:, s, :], x_tok[:, s, :], rr[:, s:s + 1], None, ALU.mult, None,
                 [x_b[s], rrb], [xbfb[s]])
        for c in range(8):
            pt, ptb = psb()
            for s in range(4):
                k.tr(pt[:, s * 128:(s + 1) * 128], xbt[:, s, c * 128:(c + 1) * 128], identb,
                     [xbfb[s]] + CONSTS, [ptb])
            k.act(hT[:, c, :], pt[:, 0:T], AF.Copy, [ptb] + CONSTS, [hTb[c]], scale=vcol(C_NG, c))

        chk("hT")
        k.handoff(PHASE_A + actAb + t1b + mTb + outb, PHASE_B)
        if first:
            k.memset("pool", ugh.rearrange("p a b -> p (a b)"), 0.0, [ughsb])
        k.cp("pool", ug[:, :, 0:KB - 1], ugh, [ughsb], [ughb])
        wu, wub = fill_slot(G_UB)
        wg, wgb = fill_slot(G_GB)
        for m in range(8):
            pu, pub = psf()
            pg, pgb = psf()
            for kc in range(8):
                k.mm(pu, wu[:, kc, m * 128:(m + 1) * 128], hT[:, kc, :], kc == 0, kc == 7,
                     [wub, hTb[kc]], [pub])
            for kc in range(8):
                k.mm(pg, wg[:, kc, m * 128:(m + 1) * 128], hT[:, kc, :], kc == 0, kc == 7,
                     [wgb, hTb[kc]], [pgb])
            sg, sgb = tf()
            k.act(sg, pg, AF.Sigmoid, [pgb], [sgb])
            k.tt("dve", ug[:, m, KB - 1:], pu, sg, ALU.mult, [pub, sgb], [ugb[m]])
        wdone()
        k.cp("pool", ugh, ug[:, :, T:T + KB - 1], ugb, [ughsb])

        chk("ug")
        wz, wzb = fill_slot(G_ZB)
        pi0, pi1 = pspin(), pspin()
        psum_s, psum_sb = PSA[pi0], PSAb[pi0]
        psum_q, psum_qb = PSA[pi1], PSAb[pi1]
        for m in range(8):
            db, dbb = dbuf[m % 2], dbufb[m % 2]
            if ("diag", m) in converted:
                k.dma(db.rearrange("p a b -> p (a b)"), dsc_d[m], [dscb[m]], [dbb], ds_db[m % 2])
            else:
                for kk in range(KB):
                    col = C_CBW + m * KB + kk
                    if kk % 2 == 0:
                        k.ts("dve", db[:, kk, :], identb, vec[:, col:col + 1], None, ALU.mult, None,
                             CONSTS, [dbb])
                    else:
                        k.act(db[:, kk, :], identb, AF.Copy, CONSTS, [dbb], scale=vec[:, col:col + 1])
                k.dma(dsc_d[m], db.rearrange("p a b -> p (a b)"), [dbb], [dscb[m]], ds_dsc[m])
                converted.add(("diag", m))
            pc, pcb = psf()
            for kk in range(KB):
                k.mm(pc, db[:, kk, :], ug[:, m, kk:kk + T], kk == 0, kk == KB - 1,
                     [dbb, ugb[m], ughb], [pcb])
            k.act(c_sb[:, m, :], pc, AF.Identity, [pcb] + CONSTS, [c_sbb[m]], bias=vcol(C_CBB, m))
            cb_, cbb_ = tb()
            k.cp("dve", cb_, c_sb[:, m, :], [c_sbb[m]], [cbb_])
            cq, cqb = tb()
            k.act(cq, c_sb[:, m, :], AF.Square, [c_sbb[m]], [cqb])
            k.mm(psum_s, onesb, cb_, m == 0, m == 7, [cbb_] + CONSTS, [psum_sb])
            k.mm(psum_q, onesb, cq, m == 0, m == 7, [cqb] + CONSTS, [psum_qb])
        chk("c_sb")
        k.handoff(ugb + [ughb], xaTb + [xahb])
        if first:
            k.memset("pool", xah.rearrange("p a b -> p (a b)"), 0.0, [xahsb])
        k.cp("pool", xaT[:, :, 1:4], xah, [xahsb], [xahb])
        wx, wxb = fill_slot(G_XA)
        mean, meanb = g_w, Buf("ln_mean")
        msq, msqb = g_l, Buf("ln_msq")
        rstd, rstdb = g_cl, Buf("ln_rstd")
        k.handoff([gb_], [meanb, msqb, rstdb])
        pinned.discard(pi0)
        pinned.discard(pi1)
        k.act(mean, psum_s, AF.Copy, [psum_sb], [meanb], scale=1.0 / D)
        k.tt("dve", msq, mean, mean, ALU.mult, [meanb], [msqb])
        k.stt(rstd, psum_q, 1.0 / D, msq, ALU.mult, ALU.subtract, [psum_qb, msqb], [rstdb])
        k.ts("dve", rstd, rstd, EPS, None, ALU.add, None, [rstdb], [rstdb])
        k.act(rstd, rstd, AF.Ln, [rstdb], [rstdb])
        k.act(rstd, rstd, AF.Exp, [rstdb], [rstdb], scale=-0.5)
        LNB_ = [meanb, msqb, rstdb]
        for m in range(8):
            pz, pzb = psf()
            for kc in range(8):
                k.mm(pz, wz[:, kc, m * 128:(m + 1) * 128], hT[:, kc, :], kc == 0, kc == 7,
                     [wzb, hTb[kc]], [pzb])
            sz, szb = tb()
            k.act(sz, pz, AF.Silu, [pzb], [szb])
            cn, cnb = tf()
            k.tt("dve", cn, c_sb[:, m, :], mean, ALU.subtract, [c_sbb[m], meanb], [cnb])
            k.tt("dve", cn, cn, rstd, ALU.mult, [cnb, rstdb], [cnb])
            sc, scb_ = tb()
            k.act(sc, cn, AF.Silu, [cnb] + CONSTS, [scb_], bias=vcol(C_LNB, m), scale=vcol(C_LNG, m))
            k.tt("dve", actB[:, m, :], sc, sz, ALU.mult, [scb_, szb], [actBb[m]])
            px, pxb = psf()
            for kc in range(8):
                k.mm(px, wx[:, kc, m * 128:(m + 1) * 128], hT[:, kc, :], kc == 0, kc == 7,
                     [wxb, hTb[kc]], [pxb])
            k.cp("act", xaT[:, m, 4:], px, [pxb], [xaTb[m]])

        wdone()
        k.handoff(LNB_, [gb_])
        chk("actB")
        k.handoff(PHASE_B, [b_ for b_ in PHASE_A if b_ not in xaTb and b_ is not xahb])
        wdone()
        k.cp("pool", xah, xaT[:, :, T + 1:T + 4], xaTb, [xahsb])
        pi0, pi1 = pspin(), pspin()
        pig, pigb = PSA[pi0], PSAb[pi0]
        pfg, pfgb = PSA[pi1], PSAb[pi1]
        for m in range(8):
            pc, pcb = psf()
            for kk in range(KA):
                k.mm(pc, diag4[:, kk * 8 + m, :], xaT[:, m, 1 + kk:1 + kk + T], kk == 0, kk == KA - 1,
                     [xaTb[m], xahb] + CONSTS, [pcb])
            k.act(xcT[:, m, :], pc, AF.Silu, [pcb] + CONSTS, [xcTb[m]], bias=vcol(C_CAB, m))
        for m in range(8):
            pq, pqb = psf()
            k.mm(pq, bd[:, 0 * 8 + m, :], xcT[:, m, :], True, True, [xcTb[m]] + CONSTS, [pqb])
            k.cp("act", qT[:, m, :], pq, [pqb], [qTb[m]])
            pk, pkb = psf()
            k.mm(pk, bd[:, 1 * 8 + m, :], xcT[:, m, :], True, True, [xcTb[m]] + CONSTS, [pkb])
            k.cp("dve", kT[:, m, :], pk, [pkb], [kTb[m]])
            pv, pvb = psf()
            k.mm(pv, bd[:, 2 * 8 + m, :], xaT[:, m, 4:], True, True, [xaTb[m]] + CONSTS, [pvb])
            vt, vtb = tb()
            k.cp("act", vt, pv, [pvb], [vtb])
            for gi, (pp, ppb) in enumerate(((pig, pigb), (pfg, pfgb))):
                lo = gi * 4
                k.mm(pp[0:4, :], wifb[:, m, lo:lo + 4], qT[:, m, :], m == 0, False,
                     [qTb[m]] + CONSTS, [ppb])
                k.mm(pp[0:4, :], wifb[:, 8 + m, lo:lo + 4], kT[:, m, :], False, False,
                     [kTb[m]] + CONSTS, [ppb])
                k.mm(pp[0:4, :], wifb[:, 16 + m, lo:lo + 4], vt, False, m == 7,
                     [vtb] + CONSTS, [ppb])

        chk("qk")
        pinned.discard(pi0)
        pinned.discard(pi1)
        G = [gb_]
        A_ = g_A[0:4, :]
        l_ = g_l[0:4, :]
        L_ = g_L[0:4, :]
        cm = g_small[0:4, 0:4]
        mus = g_small[0:4, 4:9]
        negmu = g_small[0:4, 9:14]
        dcy = g_small[0:4, 14:18]
        Lc = g_small[0:4, 18:19]
        Dexp = g_small[0:4, 20:36]
        if first:
            k.memset("dve", g_small[0:4, :], 0.0, G)
            k.memset("dve", dec.rearrange("p a b -> p (a b)"), 1.0, [decb])
        else:
            k.cp("dve", dec[:, 0, :], dec[:, 4, :], [decb], [decb])
        k.act(A_, pig[0:4, :], AF.Identity, [pigb] + CONSTS, G, bias=vec[0:4, C_BIF:C_BIF + 1])
        k.act(l_, pfg[0:4, :], AF.Exp, [pfgb] + CONSTS, G, bias=negbf[0:4, :], scale=-1.0)
        k.act(l_, l_, AF.Ln, G, G, bias=1.0)
        k.iss("dve", lambda h: h.tensor_tensor_scan(L_, ones32[0:4, 0:T], l_, Lc, ALU.mult, ALU.add),
              G + CONSTS, G)
        k.cp("dve", Lc, L_[:, T - 1:T], G, G)
        k.tt("dve", A_, A_, L_, ALU.add, G, G)
        k.iss("dve", lambda h: h.tensor_reduce(cm, A_.rearrange("p (c t) -> p c t", c=4), AX.X, ALU.max),
              G, G)
        if not first:
            k.cp("dve", mus[:, 0:1], mus[:, 4:5], G, G)
        k.iss("dve", lambda h: h.tensor_tensor_scan(mus[:, 1:5], cm, cm, mus[:, 0:1], ALU.max, ALU.max),
              G, G)
        k.ts("dve", negmu, mus, -1.0, None, ALU.mult, None, G, G)
        k.tt("dve", dcy, mus[:, 0:4], mus[:, 1:5], ALU.subtract, G, G)
        k.act(dcy, dcy, AF.Exp, G, G)
        negmu16 = g_small[0:4, 36:41]
        k.ts("dve", negmu16, negmu, -math.log(16.0), None, ALU.add, None, G, G)
        w_ = g_w[0:4, :]
        cl_ = g_cl[0:4, :]
        for c in range(4):
            k.act(w_[:, c * 128:(c + 1) * 128], A_[:, c * 128:(c + 1) * 128], AF.Exp, G, G,
                  bias=negmu16[:, c:c + 1])
            k.act(cl_[:, c * 128:(c + 1) * 128], L_[:, c * 128:(c + 1) * 128], AF.Exp, G, G,
                  bias=negmu[:, c:c + 1])
            k.ts("dve", Dexp[:, c * 4:(c + 1) * 4], ident32[0:4, 0:4], dcy[:, c:c + 1], None, ALU.mult, None,
                 G + CONSTS, G)
        pgt, pgtb = psf()
        for c in range(4):
            k.mm(pgt[:, c * 8:c * 8 + 4], w_[:, c * 128:(c + 1) * 128], ident32[0:4, 0:4], True, True,
                 G + CONSTS, [pgtb])
            k.mm(pgt[:, c * 8 + 4:c * 8 + 8], cl_[:, c * 128:(c + 1) * 128], ident32[0:4, 0:4], True, True,
                 G + CONSTS, [pgtb])
        k.mm(pgt[:, 32:48], ones32[0:4, 0:128], Dexp, True, True, G + CONSTS, [pgtb])
        k.cp("dve", gt, pgt[:, 0:32], [pgtb], [gtb])
        k.cp("dve", dec[:, 1:5, :].rearrange("p a b -> p (a b)"), pgt[:, 32:48], [pgtb], [decb])

        chk("gates")
        if first:
            k.memset("pool", Chat.rearrange("p a b -> p (a b)"), 0.0, Chatb)
            k.memset("pool", Cb.rearrange("p a b -> p (a b)"), 0.0, Cbb)
        chk("m0a")
        steps = [(c, h) for c in range(4) for h in range(H)]
        ctx = {}

        def stage_a(i):
            c, h = steps[i]
            j = i % NMB
            tsl = slice(c * 128, (c + 1) * 128)
            wcol = gt[:, c * 8 + h:c * 8 + h + 1]
            pkv, pkvb = psf()
            for dc in range(2):
                m = 2 * h + dc
                k.mm(pkv[:, dc * 128:(dc + 1) * 128], xcT[:, m, tsl], bd[:, 8 + m, :], True, True,
                     [xcTb[m]] + CONSTS, [pkvb])
                k.mm(pkv[:, 256 + dc * 128:256 + (dc + 1) * 128],
                     xaT[:, m, 4 + c * 128:4 + (c + 1) * 128], bd[:, 16 + m, :], True, True,
                     [xaTb[m]] + CONSTS, [pkvb])
            k.act(wk_tok[j], pkv[:, 0:256], AF.Copy, [pkvb, gtb], [wk_tokb[j]], scale=wcol)
            k.cp("act", v_ext[j][:, 0:256], pkv[:, 256:512], [pkvb], [v_extb[j]])
            pS, pSb = psf()
            for dc in range(2):
                m = 2 * h + dc
                k.mm(pS[:, 0:128], kT[:, m, tsl], qT[:, m, tsl], dc == 0, dc == 1,
                     [kTb[m], qTb[m]], [pSb])
            k.stt(S_sb[j], pS[:, 0:128], wcol, trimask, ALU.mult, ALU.mult, [pSb, gtb] + CONSTS, [S_sbb[j]])

        def stage_b(i):
            c, h = steps[i]
            j = i % NMB
            tsl = slice(c * 128, (c + 1) * 128)
            clcol = gt[:, c * 8 + 4 + h:c * 8 + 4 + h + 1]
            pidx = pspin()
            pin, pinb = PSA[pidx], PSAb[pidx]
            ctx[i] = (pin, pinb, pidx)
            k.mm(pin[:, 0:257], S_sb[j], v_ext[j], True, False, [S_sbb[j], v_extb[j]], [pinb])
            for dc in range(2):
                m = 2 * h + dc
                k.mm(pin[:, 0:257], qT[:, m, tsl], Cb[:, m, :], False, dc == 1,
                     [qTb[m], Cbb[m]], [pinb])
            for dc in range(2):
                m = 2 * h + dc
                pu_, pub_ = psf()
                k.mm(pu_[:, 0:257], wk_tok[j][:, dc * 128:(dc + 1) * 128], v_ext[j], True, True,
                     [wk_tokb[j], v_extb[j]], [pub_])
                k.stt(Chat[:, m, :], Chat[:, m, :], dec[:, c, h:h + 1], pu_[:, 0:257], ALU.mult, ALU.add,
                      [Chatb[m], decb, pub_], [Chatb[m]])
                k.act(Cb[:, m, :], Chat[:, m, :], AF.Copy, [Chatb[m], decb], [Cbb[m]],
                      scale=dec[:, c + 1, h:h + 1])
            s_ = sm[j]
            k.act(s_[:, 0:1], pin[:, 256:257], AF.Abs, [pinb], [smb[j]])
            k.iss("dve", lambda hd, o=s_[:, 2:8], i_=pin[:, 0:256]: hd.bn_stats(o, i_), [pinb], [smb[j]])
            k.iss("dve", lambda hd, o=s_[:, 8:10], i_=s_[:, 2:8]: hd.bn_aggr(o, i_), [smb[j]], [smb[j]])
            k.ts("dve", s_[:, 0:1], s_[:, 0:1], clcol, None, ALU.max, None, [smb[j], gtb], [smb[j]])
            k.iss("dve", lambda hd, o=s_[:, 1:2], i_=s_[:, 0:1]: hd.reciprocal(o, i_), [smb[j]], [smb[j]])
            k.tt("dve", s_[:, 10:11], s_[:, 1:2], s_[:, 1:2], ALU.mult, [smb[j]], [smb[j]])
            k.ts("dve", s_[:, 10:11], s_[:, 10:11], s_[:, 9:10], EPS, ALU.mult, ALU.add, [smb[j]], [smb[j]])
            k.act(s_[:, 10:11], s_[:, 10:11], AF.Ln, [smb[j]], [smb[j]])
            k.act(s_[:, 10:11], s_[:, 10:11], AF.Exp, [smb[j]], [smb[j]], scale=-0.5)

        def stage_c(i):
            c, h = steps[i]
            j = i % NMB
            pin, pinb, pidx = ctx[i]
            s_ = sm[j]
            k.tt("dve", s_[:, 11:12], s_[:, 10:11], s_[:, 1:2], ALU.mult, [smb[j]], [smb[j]])
            k.ts("dve", ha_tok[:, c, h * DH:(h + 1) * DH], pin[:, 0:256], s_[:, 8:9], s_[:, 11:12],
                 ALU.subtract, ALU.mult, [pinb, smb[j]], [hatb[c]])
            pinned.discard(pidx)

        wpw, wpwb = fill_slot(G_PW2)
        wgm, wgmb = fill_slot(G_GBM)

        def filler(m):
            pa, pab = psf()
            pg, pgb = psf()
            for kc in range(8):
                k.mm(pa, wpw[:, kc, m * 128:(m + 1) * 128], actB[:, kc, :], kc == 0, kc == 7,
                     [wpwb, actBb[kc]], [pab])
            for kc in range(8):
                k.mm(pg, wgm[:, kc, m * 128:(m + 1) * 128], hT[:, kc, :], kc == 0, kc == 7,
                     [wgmb, hTb[kc]], [pgb])
            sg, sgb = tf()
            k.act(sg, pg, AF.Sigmoid, [pgb], [sgb])
            k.stt(t1v[:, m, :], pa, vcol(C_BPW, m), sg, ALU.add, ALU.mult, [pab, sgb] + CONSTS, [xbfb[m // 2]])

        stage_a(0)
        for i in range(16):
            if i + 1 < 16:
                stage_a(i + 1)
            stage_b(i)
            if i >= 1:
                stage_c(i - 1)
            if i % 2 == 1:
                filler(i // 2)
        stage_c(15)
        wdone()

        chk("ha")
        k.handoff(qTb, actAb)
        wza, wzab = fill_slot(G_ZA)
        for m in range(8):
            pt, ptb = psb()
            for s in range(4):
                k.tr(pt[:, s * 128:(s + 1) * 128], ha_tok[:, s, m * 128:(m + 1) * 128], identb,
                     [hatb[s]] + CONSTS, [ptb])
            pz, pzb = psf()
            for kc in range(8):
                k.mm(pz, wza[:, kc, m * 128:(m + 1) * 128], hT[:, kc, :], kc == 0, kc == 7,
                     [wzab, hTb[kc]], [pzb])
            sz, szb = tb()
            k.act(sz, pz, AF.Silu, [pzb], [szb])
            sk, skb = tf()
            k.ts("dve", sk, xcT[:, m, :], vcol(C_SKP, m), None, ALU.mult, None, [xcTb[m]] + CONSTS, [skb])
            k.stt(sk, pt[:, 0:T], vcol(C_MHG, m), sk, ALU.mult, ALU.add, [ptb, skb] + CONSTS, [skb])
            k.tt("dve", actA[:, m, :], sk, sz, ALU.mult, [skb, szb], [actAb[m]])

        wdone()
        chk("actA")
        k.handoff(xaTb + [xahb], mTb)
        wpa, wpab = fill_slot(G_PA)
        wga, wgab = fill_slot(G_GA)
        for m in range(8):
            pa, pab = psf()
            pg, pgb = psf()
            for kc in range(8):
                k.mm(pa, wpa[:, kc, m * 128:(m + 1) * 128], actA[:, kc, :], kc == 0, kc == 7,
                     [wpab, actAb[kc]], [pab])
            for kc in range(8):
                k.mm(pg, wga[:, kc, m * 128:(m + 1) * 128], hT[:, kc, :], kc == 0, kc == 7,
                     [wgab, hTb[kc]], [pgb])
            sg, sgb = tf()
            k.act(sg, pg, AF.Sigmoid, [pgb], [sgb])
            k.tt("dve", sg, pa, sg, ALU.mult, [pab, sgb], [sgb])
            k.tt("dve", mergedT[:, m, :], sg, t1v[:, m, :], ALU.add, [sgb, xbfb[m // 2]], [mTb[m]])

        wdone()
        chk("merged")
        wo, wob = fill_slot(G_OUT)
        for s in range(4):
            for hf in range(2):
                po, pob = psf()
                for kc in range(8):
                    k.mm(po, mergedT[:, kc, s * 128:(s + 1) * 128], wo[:, kc, hf * 512:(hf + 1) * 512],
                         kc == 0, kc == 7, [mTb[kc], wob], [pob])
                k.tt("dve", x_tok[:, s, hf * 512:(hf + 1) * 512], x_tok[:, s, hf * 512:(hf + 1) * 512], po,
                     ALU.add, [x_b[s], pob], [x_b[s]])

        wdone()
        chk("x1")
        rms_stats(4)
        for s in range(4):
            k.ts("dve", xbt[:, s, :], x_tok[:, s, :], rr[:, 4 + s:5 + s], None, ALU.mult, None,
                 [x_b[s], rrb], [xbfb[s]])
        for c in range(8):
            pt, ptb = psb()
            for s in range(4):
                k.tr(pt[:, s * 128:(s + 1) * 128], xbt[:, s, c * 128:(c + 1) * 128], identb,
                     [xbfb[s]] + CONSTS, [ptb])
            k.act(hT[:, c, :], pt[:, 0:T], AF.Copy, [ptb] + CONSTS, [hTb[c]], scale=vcol(C_PNG, c))
        k.cp("pool", p_bf.rearrange("p a b -> p (a b)"), p_tok.rearrange("p a b -> p (a b)"), [p_b], [p_bfb])
        for pc_ in range(2):
            pt, ptb = psb()
            for s in range(4):
                k.tr(pt[:, s * 128:(s + 1) * 128], p_bf[:, s, pc_ * 128:(pc_ + 1) * 128], identb,
                     [p_bfb] + CONSTS, [ptb])
            k.cp("act", pT[:, pc_, :], pt[:, 0:T], [ptb], [pTb[pc_]])
        wpg, wpgb = fill_slot(G_PG)
        for s in range(4):
            for hf in range(2):
                pg, pgb = psf()
                pp, ppb = psf()
                for kc in range(8):
                    k.mm(pg, hT[:, kc, s * 128:(s + 1) * 128], wpg[:, kc, hf * 512:(hf + 1) * 512],
                         kc == 0, kc == 7, [hTb[kc], wpgb], [pgb])
                for kc in range(2):
                    k.mm(pp, pT[:, kc, s * 128:(s + 1) * 128], wple[:, kc, hf * 512:(hf + 1) * 512],
                         kc == 0, kc == 1, [pTb[kc], wpleb], [ppb])
                sg, sgb = tf()
                k.act(sg, pg, AF.Sigmoid, [pgb], [sgb])
                k.tt("dve", sg, sg, pp, ALU.mult, [sgb, ppb], [sgb])
                k.tt("dve", x_tok[:, s, hf * 512:(hf + 1) * 512], x_tok[:, s, hf * 512:(hf + 1) * 512], sg,
                     ALU.add, [x_b[s], sgb], [x_b[s]])

        wdone()
        while len(fills) < min(len(uses), wst["consumed"] + NSLOT):
            issue_fill()
        if not (seq == n_seq - 1 and ti == n_tiles - 1):
            fill_wple()
        chk("x2")
        rms_stats(8)
        k.handoff(xcTb + actAb + qTb + kTb, outb)
        for s in range(4):
            k.stt(out_tok[:, s, :], x_tok[:, s, :], rr[:, 8 + s:9 + s], fgbc, ALU.mult, ALU.mult,
                  [x_b[s], rrb] + CONSTS, [outb[s]])
            if STOP != "nostore":
                k.dma(out_d[t0 + s * 128:t0 + (s + 1) * 128, :], out_tok[:, s, :], [outb[s]], (), ds_o[s], en="pool")

    CONV_ORDER = ["ple"] + USES_PER_TILE
    cptr = {"i": 0}

    def conv_issue(n):
        for _ in range(n):
            i = cptr["i"]
            if i >= len(CONV_ORDER):
                return
            g = CONV_ORDER[i]
            cptr["i"] += 1
            gate = []
            if i >= 2:
                pg_ = CONV_ORDER[i - 2]
                gate = [scb[11 if pg_ == "ple" else pg_]]
            if g == "ple":
                for kc in range(2):
                    k.dma(wsc_ple_d[:, kc * D:(kc + 1) * D], w_ple_d[kc * 128:(kc + 1) * 128, :], gate, [scb[11]],
                          ds_sc[11], en="pool")
            else:
                src = w_src(g)
                for kc in range(8):
                    k.dma(wsc_d[g][:, kc * D:(kc + 1) * D], src[kc * 128:(kc + 1) * 128, :], gate, [scb[g]],
                          ds_sc[g], en="pool")
            converted.add(g)

    conv_issue(5)
    fill_wple()
    try:
        for seq in range(n_seq):
            for ti in range(n_tiles):
                tile_body(seq, ti)
    except _Stop:
        pass

    k.emit(ds_o)
    return nc


_NC_CACHE = {}


def _host_consts(inp):
    f = np.float32
    vec = np.zeros((128, NV), f)

    def chunks(v):
        return np.ascontiguousarray(np.asarray(v, f).reshape(8, 128).T)

    vec[:, C_NG:C_NG + 8] = chunks(inp["norm_g"][0])
    vec[:, C_CAB:C_CAB + 8] = chunks(inp["conv_a_b"][0])
    vec[:, C_MHG:C_MHG + 8] = chunks(inp["mh_norm_g"][0])
    vec[:, C_SKP:C_SKP + 8] = chunks(inp["skip_a"][0])
    vec[:, C_CBB:C_CBB + 8] = chunks(inp["conv_b_b"][0])
    vec[:, C_LNG:C_LNG + 8] = chunks(inp["ln_b_g"][0])
    vec[:, C_LNB:C_LNB + 8] = chunks(inp["ln_b_b"][0])
    vec[:, C_BPW:C_BPW + 8] = chunks(inp["b_pw2"][0])
    vec[:, C_PNG:C_PNG + 8] = chunks(inp["ple_norm_g"][0])
    caw = np.asarray(inp["conv_a_w"][0], f)
    for kk in range(KA):
        vec[:, C_CAW + kk * 8:C_CAW + kk * 8 + 8] = chunks(caw[kk])
    cbw = np.asarray(inp["conv_b_w"][0], f)
    for c in range(8):
        vec[:, C_CBW + c * KB:C_CBW + (c + 1) * KB] = cbw[:, c * 128:(c + 1) * 128].T
    for base, nm in ((C_WQ, "wq"), (C_WK, "wk"), (C_WV, "wv")):
        w = np.asarray(inp[nm][0], f)
        for c in range(8):
            vec[:, base + c * 4:base + c * 4 + 4] = w[c * 32:(c + 1) * 32].reshape(128, 4)
    bif = np.asarray(inp["b_if"][0], f)
    vec[0:4, C_BIF] = bif[0:4]
    vec[0:4, C_BIF + 1] = bif[4:8]
    cmat = np.zeros((128, 288), f)
    cmat[:, 0:128] = np.eye(128, dtype=f)
    cmat[:, 128:256] = np.triu(np.ones((128, 128), f))
    cmat[:, 256:288] = (np.arange(128)[:, None] // 4 == np.arange(32)[None, :]).astype(f)
    fg = np.ascontiguousarray(np.broadcast_to(np.asarray(inp["final_g"], f)[None, :], (128, D)))
    wif = np.asarray(inp["w_if"][0], f).reshape(24, 128, 8).transpose(1, 0, 2).reshape(128, 192)
    return vec, cmat, fg, np.ascontiguousarray(wif)


def kernel(**inputs):
    n = 8
    x = np.asarray(inputs["x"], np.float32)
    p = np.asarray(inputs["p"], np.float32)[0]
    vec, cmat, fg, wif = _host_consts(inputs)
    if "nc" not in _NC_CACHE:
        _NC_CACHE["nc"] = build_program()
    nc = _NC_CACHE["nc"]
    shared = {
        "w_in": np.ascontiguousarray(np.asarray(inputs["w_in"], np.float32)[0]),
        "w_proj_a": np.ascontiguousarray(np.asarray(inputs["w_proj_a"], np.float32)[0]),
        "w_pw2": np.ascontiguousarray(np.asarray(inputs["w_pw2"], np.float32)[0]),
        "w_out": np.ascontiguousarray(np.asarray(inputs["w_out"], np.float32)[0]),
        "w_ple_gate": np.ascontiguousarray(np.asarray(inputs["w_ple_gate"], np.float32)[0]),
        "w_ple": np.ascontiguousarray(np.asarray(inputs["w_ple"], np.float32)[0]),
        "vecs": vec, "cmat": cmat, "fg_bc": fg, "w_if_t": wif,
    }
    in_maps = []
    for c in range(n):
        m = dict(shared)
        m["x"] = np.ascontiguousarray(x[2 * c:2 * c + 2].reshape(NSEQ * SEQ, D))
        m["p"] = np.ascontiguousarray(p[2 * c:2 * c + 2].reshape(NSEQ * SEQ, 256))
        in_maps.append(m)
    res = run_bass_kernel_spmd(nc, in_maps, core_ids=list(range(n)))
    out = np.stack([np.asarray(r["out"], np.float32).reshape(NSEQ, SEQ, D) for r in res.results], 0)
    return out.reshape(16, SEQ, D)
```

```python
import math
import numpy as np
import ml_dtypes
import concourse.bass as bass
import concourse.mybir as mybir
from concourse.bass_utils import run_bass_kernel_spmd

F32 = mybir.dt.float32
BF16 = mybir.dt.bfloat16
ALU = mybir.AluOpType
AF = mybir.ActivationFunctionType
AX = mybir.AxisListType

D = 1024
SEQ = 2048
NSEQ = 2
T = 512
NT = SEQ // T
H = 4
DH = 256
EPS = 1e-6
KB = 31
KA = 4

C_NG, C_CAB, C_MHG, C_SKP, C_CBB, C_LNG, C_LNB, C_BPW, C_PNG = 0, 8, 16, 24, 32, 40, 48, 56, 64
C_CAW = 72
C_CBW = 104
C_WQ, C_WK, C_WV = 352, 384, 416
C_BIF = 448
NV = 450

G_XA, G_ZA, G_UB, G_GB, G_ZB, G_GA, G_GBM, G_PA, G_PW2, G_OUT, G_PG = range(11)


STOP = None
PHASE = ["setup"]


class _Stop(Exception):
    pass


def chk(name):
    PHASE[0] = name
    if STOP == name:
        raise _Stop()


class Eng:
    def __init__(self, name):
        self.name = name
        self.ops = []
        self.sem = None
        self.seen = {}


class Op:
    __slots__ = ("eng", "fn", "waits", "needed", "idx", "cnt", "dma", "phase")

    def __init__(self, eng, fn):
        self.eng = eng
        self.fn = fn
        self.waits = []
        self.needed = False
        self.idx = len(eng.ops)
        self.cnt = None
        self.dma = None
        self.phase = PHASE[0]


class DSem:
    def __init__(self, name, sem):
        self.name = name
        self.sem = sem
        self.count = 0


class Buf:
    __slots__ = ("name", "w", "r", "psum")

    def __init__(self, name, psum=False):
        self.name = name
        self.w = None
        self.r = {}
        self.psum = psum


def bufs(name, n, psum=False):
    return [Buf(f"{name}{i}", psum) for i in range(n)]


def tokkey(tok):
    return tok[1].eng.name if tok[0] == "op" else ("d", tok[1].name)


class K:
    def __init__(self, nc):
        self.nc = nc
        self.E = {n: Eng(n) for n in ("pe", "act", "dve", "pool", "sp")}
        for n, e in self.E.items():
            e.sem = nc.alloc_semaphore(name=f"sem_{n}")
        self.dsems = {}

    def dsem(self, name):
        if name not in self.dsems:
            self.dsems[name] = DSem(name, self.nc.alloc_semaphore(name=f"dsem_{name}"))
        return self.dsems[name]

    def _add_wait(self, op, tok, kind):
        if tok[0] == "op":
            src = tok[1]
            if src.eng is op.eng:
                if op.eng.name in ("pe", "sp"):
                    return
            if op.eng.seen.get(src.eng.name, -1) >= src.idx:
                return
            op.eng.seen[src.eng.name] = src.idx
            src.needed = True
            op.waits.append(tok)
        else:
            key = ("d", tok[1].name)
            if op.eng.seen.get(key, 0) >= tok[2]:
                return
            op.eng.seen[key] = tok[2]
            op.waits.append(tok)

    def iss(self, en, fn, rd=(), wr=(), dsem=None):
        eng = self.E[en]
        op = Op(eng, fn)
        for b in rd:
            if b.w is not None:
                self._add_wait(op, b.w, "raw")
            if b.psum:
                for kk, t in b.r.items():
                    if kk != eng.name:
                        self._add_wait(op, t, "rar")
        for b in wr:
            if b.w is not None:
                self._add_wait(op, b.w, "waw")
            for t in b.r.values():
                self._add_wait(op, t, "war")
        if dsem is not None:
            dsem.count += 16
            tok = ("dma", dsem, dsem.count)
            op.dma = dsem
        else:
            tok = ("op", op)
        for b in rd:
            b.r[tokkey(tok)] = tok
        for b in wr:
            b.w = tok
            b.r = {}
        eng.ops.append(op)
        return op

    def handoff(self, old, new):
        toks = {}
        for b in old:
            if b.w is not None:
                toks[("w",) + (tokkey(b.w),)] = b.w
            for k, t in b.r.items():
                kk = ("r", k)
                if kk in toks:
                    a = toks[kk]
                    if a[0] == "op":
                        if t[1].idx > a[1].idx:
                            toks[kk] = t
                    elif t[2] > a[2]:
                        toks[kk] = t
                else:
                    toks[kk] = t
        merged = {}
        for (_, k), t in toks.items():
            if k in merged:
                a = merged[k]
                if a[0] == "op":
                    if t[1].idx > a[1].idx:
                        merged[k] = t
                elif t[2] > a[2]:
                    merged[k] = t
            else:
                merged[k] = t
        for b in new:
            b.w = None
            b.r = dict(merged)

    def mm(self, out, lhsT, rhs, start, stop, rd, wr):
        return self.iss("pe", lambda h: h.matmul(out, lhsT, rhs, start=start, stop=stop), rd, wr)

    def tr(self, out, in_, ident, rd, wr):
        return self.iss("pe", lambda h: h.transpose(out, in_, ident), rd, wr)

    def act(self, out, in_, func, rd, wr, bias=None, scale=None, accum=None):
        kw = {}
        if bias is not None:
            kw["bias"] = bias
        if scale is not None:
            kw["scale"] = scale
        if accum is not None:
            kw["accum_out"] = accum
        return self.iss("act", lambda h: h.activation(out, in_, func, **kw), rd, wr)

    def tt(self, en, out, in0, in1, op, rd, wr):
        return self.iss(en, lambda h: h.tensor_tensor(out, in0, in1, op), rd, wr)

    def ts(self, en, out, in0, s1, s2, op0, op1, rd, wr):
        if s2 is None:
            return self.iss(en, lambda h: h.tensor_scalar(out, in0, s1, None, op0), rd, wr)
        return self.iss(en, lambda h: h.tensor_scalar(out, in0, s1, s2, op0, op1), rd, wr)

    def stt(self, out, in0, scalar, in1, op0, op1, rd, wr):
        return self.iss("dve", lambda h: h.scalar_tensor_tensor(out, in0, scalar, in1, op0, op1), rd, wr)

    def cp(self, en, out, in_, rd, wr):
        if en == "act":
            return self.iss("act", lambda h: h.activation(out, in_, AF.Copy), rd, wr)
        return self.iss(en, lambda h: h.tensor_copy(out, in_), rd, wr)

    def memset(self, en, ap, val, wr):
        return self.iss(en, lambda h: h.memset(ap, val), (), wr)

    def dma(self, out, in_, rd, wr, dsem, en="sp"):
        return self.iss(en, lambda h: h.dma_start(out=out, in_=in_), rd, wr, dsem=dsem)

    def emit(self, final_waits):
        nc = self.nc
        for e in self.E.values():
            c = 0
            for op in e.ops:
                if op.needed:
                    c += 1
                    op.cnt = c

        def run(eng, h, extra=()):
            for op in eng.ops:
                for tok in op.waits:
                    if tok[0] == "op":
                        h.wait_ge(tok[1].eng.sem, tok[1].cnt)
                    else:
                        h.wait_ge(tok[1].sem, tok[2])
                ins = op.fn(h)
                if op.dma is not None:
                    ins.then_inc(op.dma.sem, 16)
                elif op.needed:
                    ins.then_inc(eng.sem, 1)
            for ds in extra:
                h.wait_ge(ds.sem, ds.count)

        E = self.E
        with nc.Block() as block:
            @block.tensor
            def _(h):
                run(E["pe"], h)

            @block.scalar
            def _(h):
                run(E["act"], h)

            @block.vector
            def _(h):
                run(E["dve"], h)

            @block.gpsimd
            def _(h):
                run(E["pool"], h, final_waits)

            @block.sync
            def _(h):
                run(E["sp"], h)


def build_program(n_seq=NSEQ, n_tiles=NT, debug=False):
    nc = bass.Bass("TRN2", target_bir_lowering=False)
    k = K(nc)
    NTOK = n_seq * SEQ

    def din(name, shape):
        return nc.dram_tensor(name, list(shape), F32, kind="ExternalInput").ap()

    x_d = din("x", [NTOK, D])
    p_d = din("p", [NTOK, 256])
    w_in_d = din("w_in", [D, 7 * D])
    w_pa_d = din("w_proj_a", [D, D])
    w_pw2_d = din("w_pw2", [D, D])
    w_out_d = din("w_out", [D, D])
    w_pg_d = din("w_ple_gate", [D, D])
    w_ple_d = din("w_ple", [256, D])
    vec_d = din("vecs", [128, NV])
    cmat_d = din("cmat", [128, 288])
    fg_d = din("fg_bc", [128, D])
    wif_d = din("w_if_t", [128, 24 * 8])
    out_d = nc.dram_tensor("out", [NTOK, D], F32, kind="ExternalOutput").ap()
    wsc_d = nc.dram_tensor("wsc", [11, 128, 8 * D], BF16).ap()
    wsc_ple_d = nc.dram_tensor("wsc_ple", [128, 2 * D], BF16).ap()
    dsc_d = nc.dram_tensor("dsc", [8, 128, KB * 128], BF16).ap()

    def sb(name, shape, dt):
        return nc.alloc_sbuf_tensor("s_" + name, list(shape), dt).ap()

    def ps(name, shape, dt):
        return nc.alloc_psum_tensor("p_" + name, list(shape), dt).ap()

    vec = sb("vec", [128, NV], F32)
    cmat = sb("cmat", [128, 288], F32)
    fgbc = sb("fgbc", [128, D], F32)
    wif32 = sb("wif32", [128, 192], F32)
    wifb = sb("wifb", [128, 24, 8], BF16)
    identb = sb("identb", [128, 128], BF16)
    onesb = sb("onesb", [128, 128], BF16)
    ones32 = sb("ones32", [128, 512], F32)
    diag4 = sb("diag4", [128, KA * 8, 128], BF16)
    bd = sb("bd", [128, 3 * 8, 128], BF16)
    negbf = sb("negbf", [128, 1], F32)
    B_const = Buf("const")
    ident32 = cmat[:, 0:128]
    trimask = cmat[:, 128:256]
    bmask = cmat[:, 256:288]

    ds_c = k.dsem("const")
    k.dma(vec, vec_d, (), [B_const], ds_c)
    k.dma(cmat, cmat_d, (), [B_const], ds_c)
    k.dma(fgbc, fg_d, (), [B_const], ds_c)
    k.dma(wif32, wif_d, (), [B_const], ds_c)
    B_c2 = Buf("const2")
    k.cp("dve", wifb.rearrange("p a b -> p (a b)"), wif32, [B_const], [B_c2])
    k.cp("dve", identb, ident32, [B_const], [B_c2])
    k.memset("dve", onesb, 1.0, [B_c2])
    k.memset("dve", ones32, 1.0, [B_c2])
    k.memset("dve", bd.rearrange("p a b -> p (a b)"), 0.0, [B_c2])
    k.ts("dve", negbf[0:4, :], vec[0:4, C_BIF + 1:C_BIF + 2], -1.0, None, ALU.mult, None, [B_const], [B_c2])
    for kk in range(KA):
        for c in range(8):
            col = C_CAW + kk * 8 + c
            k.ts("dve", diag4[:, kk * 8 + c, :], identb, vec[:, col:col + 1], None, ALU.mult, None,
                 [B_const, B_c2], [B_c2])
    for wi, cb in enumerate((C_WQ, C_WK, C_WV)):
        for c in range(8):
            dst = bd[:, wi * 8 + c, :].rearrange("p (g o) -> p g o", o=4)
            for o in range(4):
                col = cb + c * 4 + o
                k.ts("dve", dst[:, :, o], bmask, vec[:, col:col + 1], None, ALU.mult, None,
                     [B_const, B_c2], [B_c2])
    CONSTS = [B_const, B_c2]

    PSA = [ps(f"psa{i}", [128, 512], F32) for i in range(8)]
    PSAb = bufs("psa", 8, True)
    pctr = {"f": 0}
    pinned = set()

    def psf_i():
        while True:
            i = pctr["f"] % 8
            pctr["f"] += 1
            if i not in pinned:
                return i

    def psf():
        i = psf_i()
        return PSA[i], PSAb[i]

    def psb():
        i = psf_i()
        return PSA[i].bitcast(BF16), PSAb[i]

    def pspin():
        i = psf_i()
        pinned.add(i)
        return i

    NSLOT = 3
    wslot = [sb(f"wslot{i}", [128, 8, D], BF16) for i in range(NSLOT)]
    wslotb = bufs("wslot", NSLOT)
    wple = sb("wple", [128, 2, D], BF16)
    wpleb = Buf("wple")
    NST = 2
    stage = [sb(f"stage{i}", [128, 512], F32) for i in range(NST)]
    stageb = bufs("stage", NST)
    x_tok = sb("x_tok", [128, 4, D], F32)
    x_b = bufs("x_tok", 4)
    p_tok = sb("p_tok", [128, 4, 256], F32)
    p_b = Buf("p_tok")
    p_bf = sb("p_bf", [128, 4, 256], BF16)
    p_bfb = Buf("p_bf")
    sb_xbfull = sb("xbfull", [128, 4, D], BF16)
    xbfb = bufs("xbf", 4)
    t1v = sb_xbfull.rearrange("p s f -> p (s f)").rearrange("p (c t) -> p c t", c=8)
    hT = sb("hT", [128, 8, T], BF16)
    hTb = bufs("hT", 8)
    actB = sb("actB", [128, 8, T], BF16)
    actBb = bufs("actB", 8)
    pT = sb("pT", [128, 2, T], BF16)
    pTb = bufs("pT", 2)
    XN = 20608
    arena = sb("arena", [128, XN], BF16)
    UGW = T + KB - 1
    XAW = T + 4
    ug = arena[:, 0:8 * UGW].rearrange("p (c t) -> p c t", c=8)
    o_c = 8 * UGW + (8 * UGW) % 2
    c_sb = arena[:, o_c:o_c + 2 * 8 * T].bitcast(F32).rearrange("p (c t) -> p c t", c=8)
    o_d = o_c + 2 * 8 * T
    dbuf = [arena[:, o_d + i * KB * 128:o_d + (i + 1) * KB * 128].rearrange("p (k j) -> p k j", k=KB)
            for i in range(2)]
    assert o_d + 2 * KB * 128 <= XN
    xaT = arena[:, 0:8 * XAW].rearrange("p (c t) -> p c t", c=8)
    o1 = 4128
    xcT = arena[:, o1:o1 + 8 * T].rearrange("p (c t) -> p c t", c=8)
    o2 = o1 + 8 * T
    qT = arena[:, o2:o2 + 8 * T].rearrange("p (c t) -> p c t", c=8)
    o3 = o2 + 8 * T
    kT = arena[:, o3:o3 + 8 * T].rearrange("p (c t) -> p c t", c=8)
    o4 = o3 + 8 * T
    ha_tok = arena[:, o4:o4 + 4 * D].rearrange("p (s f) -> p s f", s=4)
    assert o4 + 4 * D <= XN
    actA = qT
    t1 = arena[:, o3:o3 + 2 * 8 * T].bitcast(F32).rearrange("p (c t) -> p c t", c=8)
    mergedT = arena[:, 0:8 * T].rearrange("p (c t) -> p c t", c=8)
    ugb = bufs("ug", 8)
    ughb = Buf("ugh")
    c_sbb = bufs("c_sb", 8)
    dbufb = bufs("dbuf", 2)
    xaTb = bufs("xaT", 8)
    xahb = Buf("xah")
    xcTb = bufs("xcT", 8)
    qTb = bufs("qT", 8)
    kTb = bufs("kT", 8)
    hatb = bufs("ha_tok", 4)
    actAb = bufs("actA", 8)
    t1b = bufs("t1", 8)
    mTb = bufs("mergedT", 8)
    out_tok = arena[:, o_c:o_c + 2 * 8 * T].bitcast(F32).rearrange("p (s f) -> p s f", s=4)
    outb = bufs("out_tok", 4)
    PHASE_B = ugb + [ughb] + c_sbb + dbufb
    PHASE_A = xaTb + [xahb] + xcTb + qTb + kTb + hatb
    ugh = sb("ugh", [128, 8, KB - 1], BF16)
    xah = sb("xah", [128, 8, KA - 1], BF16)
    ughsb = Buf("ugh_s")
    xahsb = Buf("xah_s")

    tmpf = [sb(f"tmpf{i}", [128, T], F32) for i in range(3)]
    tmpfb = bufs("tmpf", 3)
    tmpb = [sb(f"tmpb{i}", [128, T], BF16) for i in range(4)]
    tmpbb = bufs("tmpb", 4)
    tctr = {"f": 0, "b": 0}

    def tf():
        i = tctr["f"] % 3
        tctr["f"] += 1
        return tmpf[i], tmpfb[i]

    def tb():
        i = tctr["b"] % 4
        tctr["b"] += 1
        return tmpb[i], tmpbb[i]

    ssq = sb("ssq", [128, 12], F32)
    ssqb = Buf("ssq")
    rr = sb("rr", [128, 12], F32)
    rrb = Buf("rr")
    g_A = sb("g_A", [128, T], F32)
    g_l = sb("g_l", [128, T], F32)
    g_L = sb("g_L", [128, T], F32)
    g_w = sb("g_w", [128, T], F32)
    g_cl = sb("g_cl", [128, T], F32)
    g_small = sb("g_small", [128, 64], F32)
    gb_ = Buf("gates")
    gt = sb("gt", [128, 32], F32)
    gtb = Buf("gt")
    dec = sb("dec", [128, 5, 4], F32)
    decb = Buf("dec")
    Chat = sb("Chat", [128, 8, 257], F32)
    Chatb = bufs("Chat", 8)
    Cb = sb("Cb", [128, 8, 257], BF16)
    Cbb = bufs("Cb", 8)
    NMB = 3
    wk_tok = [sb(f"wk_tok{i}", [128, 256], BF16) for i in range(NMB)]
    wk_tokb = bufs("wk_tok", NMB)
    v_ext = [sb(f"v_ext{i}", [128, 257], BF16) for i in range(NMB)]
    v_extb = bufs("v_ext", NMB)
    S_sb = [sb(f"S_sb{i}", [128, 128], BF16) for i in range(NMB)]
    S_sbb = bufs("S_sb", NMB)
    sm = [sb(f"sm{i}", [128, 16], F32) for i in range(NMB)]
    smb = bufs("sm", NMB)

    for i in range(NMB):
        k.memset("pool", v_ext[i][:, 256:257], 1.0, [v_extb[i]])

    ds_w = [k.dsem(f"w{i}") for i in range(NSLOT)]
    ds_wple = k.dsem("wple")
    ds_wc = [k.dsem(f"wc{i}") for i in range(NSLOT)]
    ds_wplec = k.dsem("wplec")
    ds_st = [k.dsem(f"st{i}") for i in range(NST)]
    ds_sc = [k.dsem(f"sc{i}") for i in range(12)]
    ds_x = [k.dsem(f"x{i}") for i in range(4)]
    ds_p = k.dsem("p")
    ds_db = [k.dsem(f"db{i}") for i in range(2)]
    ds_dsc = [k.dsem(f"dsc{i}") for i in range(8)]
    dscb = bufs("dsc", 8)
    ds_o = [k.dsem(f"o{i}") for i in range(4)]
    scb = bufs("scratch", 12)
    converted = set()
    sctr = {"slot": 0, "st": 0, "cast": 0}

    def w_src(g):
        if g < 7:
            return w_in_d[:, g * D:(g + 1) * D]
        return {G_PA: w_pa_d, G_PW2: w_pw2_d, G_OUT: w_out_d, G_PG: w_pg_d}[g]

    USES_PER_TILE = [G_UB, G_GB, G_ZB, G_XA, G_PW2, G_GBM, G_ZA, G_PA, G_GA, G_OUT, G_PG]
    uses = USES_PER_TILE * (n_seq * n_tiles)
    fills = []
    wst = {"consumed": 0}

    def issue_fill():
        u = len(fills)
        if u >= len(uses):
            return
        g = uses[u]
        i = u % NSLOT
        sl, slb = wslot[i], wslotb[i]
        assert g in converted, g
        if g in converted:
            for q in range(4):
                k.dma(sl[q * 32:(q + 1) * 32].rearrange("p a b -> p (a b)"),
                      wsc_d[g, q * 32:(q + 1) * 32, :], [scb[g]], [slb], ds_w[i])
        else:
            src = w_src(g)
            for kc in range(8):
                k.dma(sl[:, kc, :], src[kc * 128:(kc + 1) * 128, :], (), [slb], ds_wc[i], en="pool")
            k.dma(wsc_d[g], sl.rearrange("p a b -> p (a b)"), [slb], [scb[g]], ds_sc[g])
            converted.add(g)
        fills.append((sl, slb))

    def fill_slot(g):
        u = wst["consumed"]
        assert uses[u] == g, (u, uses[u], g)
        while len(fills) <= u:
            issue_fill()
        wst["consumed"] += 1
        return fills[u]

    def wdone():
        conv_issue(2)
        while len(fills) < min(len(uses), wst["consumed"] + NSLOT) and uses[len(fills)] not in converted:
            issue_fill()

    def fill_wple():
        assert "ple" in converted
        if "ple" in converted:
            k.dma(wple.rearrange("p a b -> p (a b)"), wsc_ple_d, [scb[11]], [wpleb], ds_wple)
        else:
            for kc in range(2):
                k.dma(wple[:, kc, :], w_ple_d[kc * 128:(kc + 1) * 128, :], (), [wpleb], ds_wplec, en="pool")
            k.dma(wsc_ple_d, wple.rearrange("p a b -> p (a b)"), [wpleb], [scb[11]], ds_sc[11])
            converted.add("ple")

    def vcol(base, c):
        return vec[:, base + c:base + c + 1]

    def rms_stats(col0):
        for s in range(4):
            k.act(sb_xbfull[:, s, :], x_tok[:, s, :], AF.Square, [x_b[s]], [xbfb[s], ssqb],
                  accum=ssq[:, col0 + s:col0 + s + 1])
        k.ts("dve", rr[:, col0:col0 + 4], ssq[:, col0:col0 + 4], 1.0 / D, EPS, ALU.mult, ALU.add,
             [ssqb], [rrb])
        k.act(rr[:, col0:col0 + 4], rr[:, col0:col0 + 4], AF.Ln, [rrb], [rrb])
        k.act(rr[:, col0:col0 + 4], rr[:, col0:col0 + 4], AF.Exp, [rrb], [rrb], scale=-0.5)

    dbg = {}
    first_out = {"done": False}

    def tile_body(seq, ti):
        t0 = seq * SEQ + ti * T
        first = ti == 0
        for s in range(4):
            k.dma(x_tok[:, s, :], x_d[t0 + s * 128:t0 + (s + 1) * 128, :], (), [x_b[s]], ds_x[s])
        k.dma(p_tok, p_d[t0:t0 + T, :].rearrange("(s p) f -> p s f", p=128), (), [p_b], ds_p)

        rms_stats(0)
        xbt = sb_xbfull
        for s in range(4):
            k.ts("dve", xbt[:, s, :], x_tok[:, s, :], rr[:, s:s + 1], None, ALU.mult, None,
                 [x_b[s], rrb], [xbfb[s]])
        for c in range(8):
            pt, ptb = psb()
            for s in range(4):
                k.tr(pt[:, s * 128:(s + 1) * 128], xbt[:, s, c * 128:(c + 1) * 128], identb,
                     [xbfb[s]] + CONSTS, [ptb])
            k.act(hT[:, c, :], pt[:, 0:T], AF.Copy, [ptb] + CONSTS, [hTb[c]], scale=vcol(C_NG, c))

        chk("hT")
        k.handoff(PHASE_A + actAb + t1b + mTb + outb, PHASE_B)
        if first:
            k.memset("pool", ugh.rearrange("p a b -> p (a b)"), 0.0, [ughsb])
        k.cp("pool", ug[:, :, 0:KB - 1], ugh, [ughsb], [ughb])
        wu, wub = fill_slot(G_UB)
        wg, wgb = fill_slot(G_GB)
        for m in range(8):
            pu, pub = psf()
            pg, pgb = psf()
            for kc in range(8):
                k.mm(pu, wu[:, kc, m * 128:(m + 1) * 128], hT[:, kc, :], kc == 0, kc == 7,
                     [wub, hTb[kc]], [pub])
            for kc in range(8):
                k.mm(pg, wg[:, kc, m * 128:(m + 1) * 128], hT[:, kc, :], kc == 0, kc == 7,
                     [wgb, hTb[kc]], [pgb])
            sg, sgb = tf()
            k.act(sg, pg, AF.Sigmoid, [pgb], [sgb])
            k.tt("dve", ug[:, m, KB - 1:], pu, sg, ALU.mult, [pub, sgb], [ugb[m]])
        wdone()
        k.cp("pool", ugh, ug[:, :, T:T + KB - 1], ugb, [ughsb])

        chk("ug")
        wz, wzb = fill_slot(G_ZB)
        pi0, pi1 = pspin(), pspin()
        psum_s, psum_sb = PSA[pi0], PSAb[pi0]
        psum_q, psum_qb = PSA[pi1], PSAb[pi1]
        for m in range(8):
            db, dbb = dbuf[m % 2], dbufb[m % 2]
            if ("diag", m) in converted:
                k.dma(db.rearrange("p a b -> p (a b)"), dsc_d[m], [dscb[m]], [dbb], ds_db[m % 2])
            else:
                for kk in range(KB):
                    col = C_CBW + m * KB + kk
                    k.ts("dve", db[:, kk, :], identb, vec[:, col:col + 1], None, ALU.mult, None,
                         CONSTS, [dbb])
                k.dma(dsc_d[m], db.rearrange("p a b -> p (a b)"), [dbb], [dscb[m]], ds_dsc[m])
                converted.add(("diag", m))
            pc, pcb = psf()
            for kk in range(KB):
                k.mm(pc, db[:, kk, :], ug[:, m, kk:kk + T], kk == 0, kk == KB - 1,
                     [dbb, ugb[m], ughb], [pcb])
            k.act(c_sb[:, m, :], pc, AF.Identity, [pcb] + CONSTS, [c_sbb[m]], bias=vcol(C_CBB, m))
            cb_, cbb_ = tb()
            k.cp("dve", cb_, c_sb[:, m, :], [c_sbb[m]], [cbb_])
            cq, cqb = tb()
            k.act(cq, c_sb[:, m, :], AF.Square, [c_sbb[m]], [cqb])
            k.mm(psum_s, onesb, cb_, m == 0, m == 7, [cbb_] + CONSTS, [psum_sb])
            k.mm(psum_q, onesb, cq, m == 0, m == 7, [cqb] + CONSTS, [psum_qb])
        chk("c_sb")
        mean, meanb = g_w, Buf("ln_mean")
        msq, msqb = g_l, Buf("ln_msq")
        rstd, rstdb = g_cl, Buf("ln_rstd")
        k.handoff([gb_], [meanb, msqb, rstdb])
        pinned.discard(pi0)
        pinned.discard(pi1)
        k.act(mean, psum_s, AF.Copy, [psum_sb], [meanb], scale=1.0 / D)
        k.tt("dve", msq, mean, mean, ALU.mult, [meanb], [msqb])
        k.stt(rstd, psum_q, 1.0 / D, msq, ALU.mult, ALU.subtract, [psum_qb, msqb], [rstdb])
        k.ts("dve", rstd, rstd, EPS, None, ALU.add, None, [rstdb], [rstdb])
        k.act(rstd, rstd, AF.Ln, [rstdb], [rstdb])
        k.act(rstd, rstd, AF.Exp, [rstdb], [rstdb], scale=-0.5)
        LNB_ = [meanb, msqb, rstdb]
        for m in range(8):
            pz, pzb = psf()
            for kc in range(8):
                k.mm(pz, wz[:, kc, m * 128:(m + 1) * 128], hT[:, kc, :], kc == 0, kc == 7,
                     [wzb, hTb[kc]], [pzb])
            sz, szb = tb()
            k.act(sz, pz, AF.Silu, [pzb], [szb])
            cn, cnb = tf()
            k.tt("dve", cn, c_sb[:, m, :], mean, ALU.subtract, [c_sbb[m], meanb], [cnb])
            k.tt("dve", cn, cn, rstd, ALU.mult, [cnb, rstdb], [cnb])
            sc, scb_ = tb()
            k.act(sc, cn, AF.Silu, [cnb] + CONSTS, [scb_], bias=vcol(C_LNB, m), scale=vcol(C_LNG, m))
            k.tt("dve", actB[:, m, :], sc, sz, ALU.mult, [scb_, szb], [actBb[m]])

        wdone()
        k.handoff(LNB_, [gb_])
        chk("actB")
        k.handoff(PHASE_B, PHASE_A)
        if first:
            k.memset("pool", xah.rearrange("p a b -> p (a b)"), 0.0, [xahsb])
        k.cp("pool", xaT[:, :, 1:4], xah, [xahsb], [xahb])
        wx, wxb = fill_slot(G_XA)
        for m in range(8):
            px, pxb = psf()
            for kc in range(8):
                k.mm(px, wx[:, kc, m * 128:(m + 1) * 128], hT[:, kc, :], kc == 0, kc == 7,
                     [wxb, hTb[kc]], [pxb])
            k.cp("act", xaT[:, m, 4:], px, [pxb], [xaTb[m]])
        wdone()
        k.cp("pool", xah, xaT[:, :, T + 1:T + 4], xaTb, [xahsb])
        pi0, pi1 = pspin(), pspin()
        pig, pigb = PSA[pi0], PSAb[pi0]
        pfg, pfgb = PSA[pi1], PSAb[pi1]
        for m in range(8):
            pc, pcb = psf()
            for kk in range(KA):
                k.mm(pc, diag4[:, kk * 8 + m, :], xaT[:, m, 1 + kk:1 + kk + T], kk == 0, kk == KA - 1,
                     [xaTb[m], xahb] + CONSTS, [pcb])
            k.act(xcT[:, m, :], pc, AF.Silu, [pcb] + CONSTS, [xcTb[m]], bias=vcol(C_CAB, m))
        for m in range(8):
            pq, pqb = psf()
            k.mm(pq, bd[:, 0 * 8 + m, :], xcT[:, m, :], True, True, [xcTb[m]] + CONSTS, [pqb])
            k.cp("act", qT[:, m, :], pq, [pqb], [qTb[m]])
            pk, pkb = psf()
            k.mm(pk, bd[:, 1 * 8 + m, :], xcT[:, m, :], True, True, [xcTb[m]] + CONSTS, [pkb])
            k.cp("dve", kT[:, m, :], pk, [pkb], [kTb[m]])
            pv, pvb = psf()
            k.mm(pv, bd[:, 2 * 8 + m, :], xaT[:, m, 4:], True, True, [xaTb[m]] + CONSTS, [pvb])
            vt, vtb = tb()
            k.cp("act", vt, pv, [pvb], [vtb])
            for gi, (pp, ppb) in enumerate(((pig, pigb), (pfg, pfgb))):
                lo = gi * 4
                k.mm(pp[0:4, :], wifb[:, m, lo:lo + 4], qT[:, m, :], m == 0, False,
                     [qTb[m]] + CONSTS, [ppb])
                k.mm(pp[0:4, :], wifb[:, 8 + m, lo:lo + 4], kT[:, m, :], False, False,
                     [kTb[m]] + CONSTS, [ppb])
                k.mm(pp[0:4, :], wifb[:, 16 + m, lo:lo + 4], vt, False, m == 7,
                     [vtb] + CONSTS, [ppb])

        chk("qk")
        pinned.discard(pi0)
        pinned.discard(pi1)
        G = [gb_]
        A_ = g_A[0:4, :]
        l_ = g_l[0:4, :]
        L_ = g_L[0:4, :]
        cm = g_small[0:4, 0:4]
        mus = g_small[0:4, 4:9]
        negmu = g_small[0:4, 9:14]
        dcy = g_small[0:4, 14:18]
        Lc = g_small[0:4, 18:19]
        Dexp = g_small[0:4, 20:36]
        if first:
            k.memset("dve", g_small[0:4, :], 0.0, G)
            k.memset("dve", dec.rearrange("p a b -> p (a b)"), 1.0, [decb])
        else:
            k.cp("dve", dec[:, 0, :], dec[:, 4, :], [decb], [decb])
        k.act(A_, pig[0:4, :], AF.Identity, [pigb] + CONSTS, G, bias=vec[0:4, C_BIF:C_BIF + 1])
        k.act(l_, pfg[0:4, :], AF.Exp, [pfgb] + CONSTS, G, bias=negbf[0:4, :], scale=-1.0)
        k.act(l_, l_, AF.Ln, G, G, bias=1.0)
        k.iss("dve", lambda h: h.tensor_tensor_scan(L_, ones32[0:4, 0:T], l_, Lc, ALU.mult, ALU.add),
              G + CONSTS, G)
        k.cp("dve", Lc, L_[:, T - 1:T], G, G)
        k.tt("dve", A_, A_, L_, ALU.add, G, G)
        k.iss("dve", lambda h: h.tensor_reduce(cm, A_.rearrange("p (c t) -> p c t", c=4), AX.X, ALU.max),
              G, G)
        if not first:
            k.cp("dve", mus[:, 0:1], mus[:, 4:5], G, G)
        k.iss("dve", lambda h: h.tensor_tensor_scan(mus[:, 1:5], cm, cm, mus[:, 0:1], ALU.max, ALU.max),
              G, G)
        k.ts("dve", negmu, mus, -1.0, None, ALU.mult, None, G, G)
        k.tt("dve", dcy, mus[:, 0:4], mus[:, 1:5], ALU.subtract, G, G)
        k.act(dcy, dcy, AF.Exp, G, G)
        negmu16 = g_small[0:4, 36:41]
        k.ts("dve", negmu16, negmu, -math.log(16.0), None, ALU.add, None, G, G)
        w_ = g_w[0:4, :]
        cl_ = g_cl[0:4, :]
        for c in range(4):
            k.act(w_[:, c * 128:(c + 1) * 128], A_[:, c * 128:(c + 1) * 128], AF.Exp, G, G,
                  bias=negmu16[:, c:c + 1])
            k.act(cl_[:, c * 128:(c + 1) * 128], L_[:, c * 128:(c + 1) * 128], AF.Exp, G, G,
                  bias=negmu[:, c:c + 1])
            k.ts("dve", Dexp[:, c * 4:(c + 1) * 4], ident32[0:4, 0:4], dcy[:, c:c + 1], None, ALU.mult, None,
                 G + CONSTS, G)
        pgt, pgtb = psf()
        for c in range(4):
            k.mm(pgt[:, c * 8:c * 8 + 4], w_[:, c * 128:(c + 1) * 128], ident32[0:4, 0:4], True, True,
                 G + CONSTS, [pgtb])
            k.mm(pgt[:, c * 8 + 4:c * 8 + 8], cl_[:, c * 128:(c + 1) * 128], ident32[0:4, 0:4], True, True,
                 G + CONSTS, [pgtb])
        k.mm(pgt[:, 32:48], ones32[0:4, 0:128], Dexp, True, True, G + CONSTS, [pgtb])
        k.cp("dve", gt, pgt[:, 0:32], [pgtb], [gtb])
        k.cp("dve", dec[:, 1:5, :].rearrange("p a b -> p (a b)"), pgt[:, 32:48], [pgtb], [decb])

        chk("gates")
        if first:
            k.memset("pool", Chat.rearrange("p a b -> p (a b)"), 0.0, Chatb)
            k.memset("pool", Cb.rearrange("p a b -> p (a b)"), 0.0, Cbb)
        chk("m0a")
        steps = [(c, h) for c in range(4) for h in range(H)]
        ctx = {}

        def stage_a(i):
            c, h = steps[i]
            j = i % NMB
            tsl = slice(c * 128, (c + 1) * 128)
            wcol = gt[:, c * 8 + h:c * 8 + h + 1]
            pkv, pkvb = psf()
            for dc in range(2):
                m = 2 * h + dc
                k.mm(pkv[:, dc * 128:(dc + 1) * 128], xcT[:, m, tsl], bd[:, 8 + m, :], True, True,
                     [xcTb[m]] + CONSTS, [pkvb])
                k.mm(pkv[:, 256 + dc * 128:256 + (dc + 1) * 128],
                     xaT[:, m, 4 + c * 128:4 + (c + 1) * 128], bd[:, 16 + m, :], True, True,
                     [xaTb[m]] + CONSTS, [pkvb])
            k.act(wk_tok[j], pkv[:, 0:256], AF.Copy, [pkvb, gtb], [wk_tokb[j]], scale=wcol)
            k.cp("act", v_ext[j][:, 0:256], pkv[:, 256:512], [pkvb], [v_extb[j]])
            pS, pSb = psf()
            for dc in range(2):
                m = 2 * h + dc
                k.mm(pS[:, 0:128], kT[:, m, tsl], qT[:, m, tsl], dc == 0, dc == 1,
                     [kTb[m], qTb[m]], [pSb])
            k.stt(S_sb[j], pS[:, 0:128], wcol, trimask, ALU.mult, ALU.mult, [pSb, gtb] + CONSTS, [S_sbb[j]])

        def stage_b(i):
            c, h = steps[i]
            j = i % NMB
            tsl = slice(c * 128, (c + 1) * 128)
            clcol = gt[:, c * 8 + 4 + h:c * 8 + 4 + h + 1]
            pidx = pspin()
            pin, pinb = PSA[pidx], PSAb[pidx]
            ctx[i] = (pin, pinb, pidx)
            k.mm(pin[:, 0:257], S_sb[j], v_ext[j], True, False, [S_sbb[j], v_extb[j]], [pinb])
            for dc in range(2):
                m = 2 * h + dc
                k.mm(pin[:, 0:257], qT[:, m, tsl], Cb[:, m, :], False, dc == 1,
                     [qTb[m], Cbb[m]], [pinb])
            for dc in range(2):
                m = 2 * h + dc
                pu_, pub_ = psf()
                k.mm(pu_[:, 0:257], wk_tok[j][:, dc * 128:(dc + 1) * 128], v_ext[j], True, True,
                     [wk_tokb[j], v_extb[j]], [pub_])
                k.stt(Chat[:, m, :], Chat[:, m, :], dec[:, c, h:h + 1], pu_[:, 0:257], ALU.mult, ALU.add,
                      [Chatb[m], decb, pub_], [Chatb[m]])
                k.act(Cb[:, m, :], Chat[:, m, :], AF.Copy, [Chatb[m], decb], [Cbb[m]],
                      scale=dec[:, c + 1, h:h + 1])
            s_ = sm[j]
            k.act(s_[:, 0:1], pin[:, 256:257], AF.Abs, [pinb], [smb[j]])
            k.iss("dve", lambda hd, o=s_[:, 2:8], i_=pin[:, 0:256]: hd.bn_stats(o, i_), [pinb], [smb[j]])
            k.iss("dve", lambda hd, o=s_[:, 8:10], i_=s_[:, 2:8]: hd.bn_aggr(o, i_), [smb[j]], [smb[j]])
            k.ts("dve", s_[:, 0:1], s_[:, 0:1], clcol, None, ALU.max, None, [smb[j], gtb], [smb[j]])
            k.iss("dve", lambda hd, o=s_[:, 1:2], i_=s_[:, 0:1]: hd.reciprocal(o, i_), [smb[j]], [smb[j]])
            k.tt("dve", s_[:, 10:11], s_[:, 1:2], s_[:, 1:2], ALU.mult, [smb[j]], [smb[j]])
            k.ts("dve", s_[:, 10:11], s_[:, 10:11], s_[:, 9:10], EPS, ALU.mult, ALU.add, [smb[j]], [smb[j]])
            k.act(s_[:, 10:11], s_[:, 10:11], AF.Ln, [smb[j]], [smb[j]])
            k.act(s_[:, 10:11], s_[:, 10:11], AF.Exp, [smb[j]], [smb[j]], scale=-0.5)

        def stage_c(i):
            c, h = steps[i]
            j = i % NMB
            pin, pinb, pidx = ctx[i]
            s_ = sm[j]
            k.tt("dve", s_[:, 11:12], s_[:, 10:11], s_[:, 1:2], ALU.mult, [smb[j]], [smb[j]])
            k.ts("dve", ha_tok[:, c, h * DH:(h + 1) * DH], pin[:, 0:256], s_[:, 8:9], s_[:, 11:12],
                 ALU.subtract, ALU.mult, [pinb, smb[j]], [hatb[c]])
            pinned.discard(pidx)

        wpw, wpwb = fill_slot(G_PW2)
        wgm, wgmb = fill_slot(G_GBM)

        def filler(m):
            pa, pab = psf()
            pg, pgb = psf()
            for kc in range(8):
                k.mm(pa, wpw[:, kc, m * 128:(m + 1) * 128], actB[:, kc, :], kc == 0, kc == 7,
                     [wpwb, actBb[kc]], [pab])
            for kc in range(8):
                k.mm(pg, wgm[:, kc, m * 128:(m + 1) * 128], hT[:, kc, :], kc == 0, kc == 7,
                     [wgmb, hTb[kc]], [pgb])
            sg, sgb = tf()
            k.act(sg, pg, AF.Sigmoid, [pgb], [sgb])
            k.stt(t1v[:, m, :], pa, vcol(C_BPW, m), sg, ALU.add, ALU.mult, [pab, sgb] + CONSTS, [xbfb[m // 2]])

        stage_a(0)
        for i in range(16):
            if i + 1 < 16:
                stage_a(i + 1)
            stage_b(i)
            if i >= 1:
                stage_c(i - 1)
            if i % 2 == 1:
                filler(i // 2)
        stage_c(15)
        wdone()

        chk("ha")
        k.handoff(qTb, actAb)
        wza, wzab = fill_slot(G_ZA)
        for m in range(8):
            pt, ptb = psb()
            for s in range(4):
                k.tr(pt[:, s * 128:(s + 1) * 128], ha_tok[:, s, m * 128:(m + 1) * 128], identb,
                     [hatb[s]] + CONSTS, [ptb])
            pz, pzb = psf()
            for kc in range(8):
                k.mm(pz, wza[:, kc, m * 128:(m + 1) * 128], hT[:, kc, :], kc == 0, kc == 7,
                     [wzab, hTb[kc]], [pzb])
            sz, szb = tb()
            k.act(sz, pz, AF.Silu, [pzb], [szb])
            sk, skb = tf()
            k.ts("dve", sk, xcT[:, m, :], vcol(C_SKP, m), None, ALU.mult, None, [xcTb[m]] + CONSTS, [skb])
            k.stt(sk, pt[:, 0:T], vcol(C_MHG, m), sk, ALU.mult, ALU.add, [ptb, skb] + CONSTS, [skb])
            k.tt("dve", actA[:, m, :], sk, sz, ALU.mult, [skb, szb], [actAb[m]])

        wdone()
        chk("actA")
        k.handoff(xaTb + [xahb], mTb)
        wpa, wpab = fill_slot(G_PA)
        wga, wgab = fill_slot(G_GA)
        for m in range(8):
            pa, pab = psf()
            pg, pgb = psf()
            for kc in range(8):
                k.mm(pa, wpa[:, kc, m * 128:(m + 1) * 128], actA[:, kc, :], kc == 0, kc == 7,
                     [wpab, actAb[kc]], [pab])
            for kc in range(8):
                k.mm(pg, wga[:, kc, m * 128:(m + 1) * 128], hT[:, kc, :], kc == 0, kc == 7,
                     [wgab, hTb[kc]], [pgb])
            sg, sgb = tf()
            k.act(sg, pg, AF.Sigmoid, [pgb], [sgb])
            k.tt("dve", sg, pa, sg, ALU.mult, [pab, sgb], [sgb])
            k.tt("dve", mergedT[:, m, :], sg, t1v[:, m, :], ALU.add, [sgb, xbfb[m // 2]], [mTb[m]])

        wdone()
        chk("merged")
        wo, wob = fill_slot(G_OUT)
        for s in range(4):
            for hf in range(2):
                po, pob = psf()
                for kc in range(8):
                    k.mm(po, mergedT[:, kc, s * 128:(s + 1) * 128], wo[:, kc, hf * 512:(hf + 1) * 512],
                         kc == 0, kc == 7, [mTb[kc], wob], [pob])
                k.tt("dve", x_tok[:, s, hf * 512:(hf + 1) * 512], x_tok[:, s, hf * 512:(hf + 1) * 512], po,
                     ALU.add, [x_b[s], pob], [x_b[s]])

        wdone()
        chk("x1")
        rms_stats(4)
        for s in range(4):
            k.ts("dve", xbt[:, s, :], x_tok[:, s, :], rr[:, 4 + s:5 + s], None, ALU.mult, None,
                 [x_b[s], rrb], [xbfb[s]])
        for c in range(8):
            pt, ptb = psb()
            for s in range(4):
                k.tr(pt[:, s * 128:(s + 1) * 128], xbt[:, s, c * 128:(c + 1) * 128], identb,
                     [xbfb[s]] + CONSTS, [ptb])
            k.act(hT[:, c, :], pt[:, 0:T], AF.Copy, [ptb] + CONSTS, [hTb[c]], scale=vcol(C_PNG, c))
        k.cp("pool", p_bf.rearrange("p a b -> p (a b)"), p_tok.rearrange("p a b -> p (a b)"), [p_b], [p_bfb])
        for pc_ in range(2):
            pt, ptb = psb()
            for s in range(4):
                k.tr(pt[:, s * 128:(s + 1) * 128], p_bf[:, s, pc_ * 128:(pc_ + 1) * 128], identb,
                     [p_bfb] + CONSTS, [ptb])
            k.cp("act", pT[:, pc_, :], pt[:, 0:T], [ptb], [pTb[pc_]])
        wpg, wpgb = fill_slot(G_PG)
        for s in range(4):
            for hf in range(2):
                pg, pgb = psf()
                pp, ppb = psf()
                for kc in range(8):
                    k.mm(pg, hT[:, kc, s * 128:(s + 1) * 128], wpg[:, kc, hf * 512:(hf + 1) * 512],
                         kc == 0, kc == 7, [hTb[kc], wpgb], [pgb])
                for kc in range(2):
                    k.mm(pp, pT[:, kc, s * 128:(s + 1) * 128], wple[:, kc, hf * 512:(hf + 1) * 512],
                         kc == 0, kc == 1, [pTb[kc], wpleb], [ppb])
                sg, sgb = tf()
                k.act(sg, pg, AF.Sigmoid, [pgb], [sgb])
                k.tt("dve", sg, sg, pp, ALU.mult, [sgb, ppb], [sgb])
                k.tt("dve", x_tok[:, s, hf * 512:(hf + 1) * 512], x_tok[:, s, hf * 512:(hf + 1) * 512], sg,
                     ALU.add, [x_b[s], sgb], [x_b[s]])

        wdone()
        while len(fills) < min(len(uses), wst["consumed"] + NSLOT):
            issue_fill()
        if not (seq == n_seq - 1 and ti == n_tiles - 1):
            fill_wple()
        chk("x2")
        rms_stats(8)
        k.handoff(xcTb + actAb + qTb + kTb, outb)
        for s in range(4):
            k.stt(out_tok[:, s, :], x_tok[:, s, :], rr[:, 8 + s:9 + s], fgbc, ALU.mult, ALU.mult,
                  [x_b[s], rrb] + CONSTS, [outb[s]])
            if STOP != "nostore":
                k.dma(out_d[t0 + s * 128:t0 + (s + 1) * 128, :], out_tok[:, s, :], [outb[s]], (), ds_o[s], en="pool")

    CONV_ORDER = ["ple"] + USES_PER_TILE
    cptr = {"i": 0}

    def conv_issue(n):
        for _ in range(n):
            i = cptr["i"]
            if i >= len(CONV_ORDER):
                return
            g = CONV_ORDER[i]
            cptr["i"] += 1
            gate = []
            if i >= 2:
                pg_ = CONV_ORDER[i - 2]
                gate = [scb[11 if pg_ == "ple" else pg_]]
            if g == "ple":
                for kc in range(2):
                    k.dma(wsc_ple_d[:, kc * D:(kc + 1) * D], w_ple_d[kc * 128:(kc + 1) * 128, :], gate, [scb[11]],
                          ds_sc[11], en="pool")
            else:
                src = w_src(g)
                for kc in range(8):
                    k.dma(wsc_d[g][:, kc * D:(kc + 1) * D], src[kc * 128:(kc + 1) * 128, :], gate, [scb[g]],
                          ds_sc[g], en="pool")
            converted.add(g)

    conv_issue(5)
    fill_wple()
    try:
        for seq in range(n_seq):
            for ti in range(n_tiles):
                tile_body(seq, ti)
    except _Stop:
        pass

    k.emit(ds_o)
    return nc


_NC_CACHE = {}


def _host_consts(inp):
    f = np.float32
    vec = np.zeros((128, NV), f)

    def chunks(v):
        return np.ascontiguousarray(np.asarray(v, f).reshape(8, 128).T)

    vec[:, C_NG:C_NG + 8] = chunks(inp["norm_g"][0])
    vec[:, C_CAB:C_CAB + 8] = chunks(inp["conv_a_b"][0])
    vec[:, C_MHG:C_MHG + 8] = chunks(inp["mh_norm_g"][0])
    vec[:, C_SKP:C_SKP + 8] = chunks(inp["skip_a"][0])
    vec[:, C_CBB:C_CBB + 8] = chunks(inp["conv_b_b"][0])
    vec[:, C_LNG:C_LNG + 8] = chunks(inp["ln_b_g"][0])
    vec[:, C_LNB:C_LNB + 8] = chunks(inp["ln_b_b"][0])
    vec[:, C_BPW:C_BPW + 8] = chunks(inp["b_pw2"][0])
    vec[:, C_PNG:C_PNG + 8] = chunks(inp["ple_norm_g"][0])
    caw = np.asarray(inp["conv_a_w"][0], f)
    for kk in range(KA):
        vec[:, C_CAW + kk * 8:C_CAW + kk * 8 + 8] = chunks(caw[kk])
    cbw = np.asarray(inp["conv_b_w"][0], f)
    for c in range(8):
        vec[:, C_CBW + c * KB:C_CBW + (c + 1) * KB] = cbw[:, c * 128:(c + 1) * 128].T
    for base, nm in ((C_WQ, "wq"), (C_WK, "wk"), (C_WV, "wv")):
        w = np.asarray(inp[nm][0], f)
        for c in range(8):
            vec[:, base + c * 4:base + c * 4 + 4] = w[c * 32:(c + 1) * 32].reshape(128, 4)
    bif = np.asarray(inp["b_if"][0], f)
    vec[0:4, C_BIF] = bif[0:4]
    vec[0:4, C_BIF + 1] = bif[4:8]
    cmat = np.zeros((128, 288), f)
    cmat[:, 0:128] = np.eye(128, dtype=f)
    cmat[:, 128:256] = np.triu(np.ones((128, 128), f))
    cmat[:, 256:288] = (np.arange(128)[:, None] // 4 == np.arange(32)[None, :]).astype(f)
    fg = np.ascontiguousarray(np.broadcast_to(np.asarray(inp["final_g"], f)[None, :], (128, D)))
    wif = np.asarray(inp["w_if"][0], f).reshape(24, 128, 8).transpose(1, 0, 2).reshape(128, 192)
    return vec, cmat, fg, np.ascontiguousarray(wif)


def kernel(**inputs):
    n = 8
    x = np.asarray(inputs["x"], np.float32)
    p = np.asarray(inputs["p"], np.float32)[0]
    vec, cmat, fg, wif = _host_consts(inputs)
    if "nc" not in _NC_CACHE:
        _NC_CACHE["nc"] = build_program()
    nc = _NC_CACHE["nc"]
    shared = {
        "w_in": np.ascontiguousarray(np.asarray(inputs["w_in"], np.float32)[0]),
        "w_proj_a": np.ascontiguousarray(np.asarray(inputs["w_proj_a"], np.float32)[0]),
        "w_pw2": np.ascontiguousarray(np.asarray(inputs["w_pw2"], np.float32)[0]),
        "w_out": np.ascontiguousarray(np.asarray(inputs["w_out"], np.float32)[0]),
        "w_ple_gate": np.ascontiguousarray(np.asarray(inputs["w_ple_gate"], np.float32)[0]),
        "w_ple": np.ascontiguousarray(np.asarray(inputs["w_ple"], np.float32)[0]),
        "vecs": vec, "cmat": cmat, "fg_bc": fg, "w_if_t": wif,
    }
    in_maps = []
    for c in range(n):
        m = dict(shared)
        m["x"] = np.ascontiguousarray(x[2 * c:2 * c + 2].reshape(NSEQ * SEQ, D))
        m["p"] = np.ascontiguousarray(p[2 * c:2 * c + 2].reshape(NSEQ * SEQ, 256))
        in_maps.append(m)
    res = run_bass_kernel_spmd(nc, in_maps, core_ids=list(range(n)))
    out = np.stack([np.asarray(r["out"], np.float32).reshape(NSEQ, SEQ, D) for r in res.results], 0)
    return out.reshape(16, SEQ, D)
```
